# Optimizing a Trainium2 kernel written in Bass

```python
import math
import jax, jax.numpy as jnp
from jax import lax
import numpy as np

D_MODEL = 1024
BATCH = 8
SEQ = 2048
DEPTH = 2

A_HEADS = 4
A_DH = 64
A_DV = 2 * A_DH
A_QW = A_HEADS * 2 * A_DH
A_VW = A_HEADS * A_DV
A_BLOCK = 128
R_HEADS = 8
R_N = 64
R_W = R_HEADS * R_N
R_LORA_W = 64
R_LORA_A = 64
R_LORA_G = 128
R_COLS = 3 * R_W + R_LORA_W + R_LORA_A + R_LORA_G
RWKV_GN_EPS = 64e-5
G_HEADS = 4
G_DK = 64
G_DV = 128
G_KW = G_HEADS * G_DK
G_VW = G_HEADS * G_DV
G_LORA = 16
G_TAU = 16.0
G_CHUNK = 64
G_COLS = 2 * G_KW + G_VW + G_LORA + G_VW
A_COLS = 2 * A_QW + A_VW
GATE_COLS = 3 * D_MODEL
N_IN = A_COLS + R_COLS + G_COLS + GATE_COLS
N_GROUPS = 4
EXP_PER_GROUP = 8
N_EXPERTS = N_GROUPS * EXP_PER_GROUP
TOP_K = 2
D_EXPERT = 512
MOE_BLOCK = 128
EPS = 1e-6

kernel_name = 'hybrid_diffattn_rwkv7_gla_hmoe_adaln'


def _split(t, widths):
    cuts = [int(i) for i in np.cumsum(widths)[:-1]]
    return jnp.split(t, cuts, axis=-1)


def rms_norm(x, g, eps=EPS):
    xf = x.astype(jnp.float32)
    y = xf * lax.rsqrt(jnp.mean(xf * xf, axis=-1, keepdims=True) + eps)
    return (y * g.astype(jnp.float32)).astype(x.dtype)


def group_norm(x, g, b, eps):
    xf = x.astype(jnp.float32)
    mu = jnp.mean(xf, axis=-1, keepdims=True)
    var = jnp.mean(jnp.square(xf - mu), axis=-1, keepdims=True)
    return (xf - mu) * lax.rsqrt(var + eps) * g.astype(jnp.float32) + b.astype(jnp.float32)


def token_shift(t):
    return jnp.pad(t, ((0, 0), (1, 0), (0, 0)))[:, :-1]


def diff_attention(q, k, v, qn_g, kn_g, lam_vecs, subln_g, lambda_init):
    B, S = q.shape[:2]
    q = rms_norm(q, qn_g) * (A_DH ** -0.5)
    k = rms_norm(k, kn_g)
    q = q.transpose(0, 2, 3, 1, 4)
    k = k.transpose(0, 2, 3, 1, 4)
    v = v.transpose(0, 2, 1, 3)
    lv = lam_vecs.astype(jnp.float32)
    lam = jnp.exp(jnp.sum(lv[0] * lv[1])) - jnp.exp(jnp.sum(lv[2] * lv[3])) + lambda_init
    slopes = jnp.asarray([2.0 ** (-8.0 * (i + 1) / A_HEADS) for i in range(A_HEADS)], jnp.float32)
    kpos = jnp.arange(S)

    def block(i):
        q_blk = lax.dynamic_slice_in_dim(q, i * A_BLOCK, A_BLOCK, axis=3)
        s = jnp.einsum('bhcqd,bhckd->bhcqk', q_blk, k).astype(jnp.float32)
        qpos = i * A_BLOCK + jnp.arange(A_BLOCK)
        dist = qpos[:, None] - kpos[None, :]
        s = s - (slopes[:, None, None] * dist.astype(jnp.float32))[None, :, None]
        s = jnp.where(dist >= 0, s, -jnp.inf)
        p = jax.nn.softmax(s, axis=-1)
        a = p[:, :, 0] - lam * p[:, :, 1]
        return jnp.einsum('bhqk,bhkd->bhqd', a.astype(v.dtype), v)

    o = lax.map(block, jnp.arange(S // A_BLOCK))
    o = o.transpose(1, 0, 3, 2, 4).reshape(B, S, A_HEADS, A_DV)
    o = rms_norm(o, subln_g) * (1.0 - lambda_init)
    return o.reshape(B, S, A_VW)


def rwkv7_mix(slab, mu, w_up, w0, a_up, a0, g_up, k_k, k_a, r_k, lnx_g, lnx_b):
    B, S = slab.shape[:2]
    f32 = jnp.float32
    slab = slab + (token_shift(slab) - slab) * mu
    r, k, v, wd, ad, gd = _split(slab, [R_W, R_W, R_W, R_LORA_W, R_LORA_A, R_LORA_G])
    w = -jax.nn.softplus(-(w0 + jnp.tanh(wd) @ w_up)) - 0.5
    decay = jnp.exp(-jnp.exp(w.astype(f32)))
    a = jax.nn.sigmoid(a0 + ad @ a_up)
    g = jax.nn.sigmoid(gd) @ g_up
    hs = lambda t: t.reshape(B, S, R_HEADS, R_N).astype(f32)
    r, k, v, decay, a = hs(r), hs(k), hs(v), hs(decay), hs(a)
    kk = k * k_k.reshape(R_HEADS, R_N).astype(f32)
    kk = kk / jnp.maximum(jnp.sqrt(jnp.sum(kk * kk, axis=-1, keepdims=True)), 1e-12)
    k = k * (1.0 + (a - 1.0) * k_a.reshape(R_HEADS, R_N).astype(f32))

    def step(state, inp):
        r_t, w_t, k_t, v_t, kk_t, a_t = inp
        sa = jnp.einsum('bhvk,bhk->bhv', state, -kk_t)
        state = (state * w_t[:, :, None, :] + sa[..., None] * (kk_t * a_t)[:, :, None, :]
                 + v_t[..., None] * k_t[:, :, None, :])
        return state, jnp.einsum('bhvk,bhk->bhv', state, r_t)

    xs = tuple(t.swapaxes(0, 1) for t in (r, decay, k, v, kk, a))
    s0 = jnp.zeros((B, R_HEADS, R_N, R_N), f32)
    _, y = lax.scan(step, s0, xs)
    y = y.swapaxes(0, 1)
    y = group_norm(y, lnx_g.reshape(R_HEADS, R_N), lnx_b.reshape(R_HEADS, R_N), RWKV_GN_EPS)
    bonus = jnp.sum(r * k * r_k.astype(f32), axis=-1, keepdims=True) * v
    out = (y + bonus) * g.reshape(B, S, R_HEADS, R_N).astype(f32)
    return out.reshape(B, S, R_W).astype(slab.dtype)


def gla_mix(q, k, v, log_alpha, gate, norm_g):
    B, S = q.shape[:2]
    nc = S // G_CHUNK
    f32 = jnp.float32

    def chunks(t, d):
        return t.reshape(B, nc, G_CHUNK, G_HEADS, d).transpose(1, 0, 3, 2, 4).astype(f32)

    qc = chunks(q, G_DK) * (G_DK ** -0.5)
    kc, vc, lc = chunks(k, G_DK), chunks(v, G_DV), chunks(log_alpha, G_DK)
    causal = jnp.tril(jnp.ones((G_CHUNK, G_CHUNK), dtype=bool))[:, :, None]

    def step(state, inp):
        q_, k_, v_, l_ = inp
        b = jnp.cumsum(l_, axis=2)
        o_inter = jnp.einsum('bhid,bhde->bhie', q_ * jnp.exp(b), state)
        diff = b[:, :, :, None, :] - b[:, :, None, :, :]
        dec = jnp.exp(jnp.where(causal, diff, -jnp.inf))
        scores = jnp.einsum('bhid,bhjd,bhijd->bhij', q_, k_, dec)
        o_intra = jnp.einsum('bhij,bhje->bhie', scores, v_)
        b_last = b[:, :, -1:, :]
        state = (state * jnp.exp(b_last)[:, :, 0, :, None]
                 + jnp.einsum('bhjd,bhje->bhde', k_ * jnp.exp(b_last - b), v_))
        return state, o_inter + o_intra

    s0 = jnp.zeros((B, G_HEADS, G_DK, G_DV), f32)
    _, o = lax.scan(step, s0, (qc, kc, vc, lc))
    o = o.transpose(1, 0, 3, 2, 4).reshape(B, S, G_HEADS, G_DV)
    o = rms_norm(o, norm_g) * jax.nn.silu(gate.astype(f32))
    return o.reshape(B, S, G_VW).astype(q.dtype)


def hier_moe(h, rg_w, rg_b, re_w, re_b, w_gate, w_up, w_down):
    B, S, D = h.shape
    T = B * S
    A = T * TOP_K
    xt = h.reshape(T, D)
    g_logits = (xt @ rg_w + rg_b).astype(jnp.float32)
    grp = jnp.argmax(g_logits, axis=-1)
    g_prob = jnp.take_along_axis(jax.nn.softmax(g_logits, axis=-1), grp[:, None], axis=1)
    e_logits = (xt @ re_w + re_b).astype(jnp.float32).reshape(T, N_GROUPS, EXP_PER_GROUP)
    e_in = jnp.take_along_axis(e_logits, grp[:, None, None], axis=1)[:, 0]
    top_v, top_i = lax.top_k(e_in, TOP_K)
    w_tok = g_prob * jax.nn.softmax(top_v, axis=-1)
    eid = (grp[:, None] * EXP_PER_GROUP + top_i).reshape(-1).astype(jnp.int32)
    tok = jnp.repeat(jnp.arange(T, dtype=jnp.int32), TOP_K)
    wts = w_tok.reshape(-1)
    order = jnp.argsort(eid)
    eid_s, tok_s, w_s = eid[order], tok[order], wts[order]
    counts = jax.ops.segment_sum(jnp.ones_like(eid), eid, num_segments=N_EXPERTS)
    padded = (counts + MOE_BLOCK - 1) // MOE_BLOCK * MOE_BLOCK
    pad_end = jnp.cumsum(padded)
    pad_start = pad_end - padded
    start = jnp.cumsum(counts) - counts
    dest = pad_start[eid_s] + jnp.arange(A, dtype=jnp.int32) - start[eid_s]
    n_blocks = -(-A // MOE_BLOCK) + N_EXPERTS
    R = n_blocks * MOE_BLOCK
    row_tok = jnp.zeros((R,), jnp.int32).at[dest].set(tok_s)
    blk_exp = jnp.minimum(jnp.searchsorted(pad_end, jnp.arange(n_blocks) * MOE_BLOCK, side='right'),
                          N_EXPERTS - 1)
    xin = xt[row_tok].reshape(n_blocks, MOE_BLOCK, D)

    def run(args):
        xb, e = args
        hid = jax.nn.silu(xb @ w_gate[e]) * (xb @ w_up[e])
        return hid @ w_down[e]

    y = lax.map(run, (xin, blk_exp)).reshape(R, D)
    out = jnp.zeros((T, D), h.dtype).at[tok_s].add(y[dest] * w_s[:, None].astype(h.dtype))
    return out.reshape(B, S, D)


def setup_inputs(seed: int = 0) -> dict:
    key = jax.random.key(seed)
    ks = iter(jax.random.split(key, 64))
    L, D = DEPTH, D_MODEL
    nrm = lambda shape, s: jax.random.normal(next(ks), shape, jnp.float32) * s
    ones_n = lambda shape: 1.0 + nrm(shape, 0.02)
    ada_b = nrm((L, 6 * D), 0.05)
    ada_b = ada_b.at[:, 2 * D:3 * D].add(0.5).at[:, 5 * D:6 * D].add(0.5)
    return {
        'x': nrm((BATCH, SEQ, D), 1.0),
        'c': nrm((BATCH, D), 1.0),
        'ada_w': nrm((L, D, 6 * D), 0.2 * D ** -0.5),
        'ada_b': ada_b,
        'norm1_g': ones_n((L, D)),
        'norm2_g': ones_n((L, D)),
        'w_in': nrm((L, D, N_IN), D ** -0.5),
        'attn_qn_g': ones_n((L, A_DH)),
        'attn_kn_g': ones_n((L, A_DH)),
        'attn_lambda': nrm((L, 4, A_DH), 0.1),
        'attn_subln_g': ones_n((L, A_DV)),
        'rwkv_mu': jax.random.uniform(next(ks), (L, R_COLS), jnp.float32),
        'rwkv_w_up': nrm((L, R_LORA_W, R_W), 0.1 * R_LORA_W ** -0.5),
        'rwkv_w0': jax.random.uniform(next(ks), (L, R_W), jnp.float32, -6.5, -1.5),
        'rwkv_a_up': nrm((L, R_LORA_A, R_W), R_LORA_A ** -0.5),
        'rwkv_a0': nrm((L, R_W), 0.1),
        'rwkv_g_up': nrm((L, R_LORA_G, R_W), R_LORA_G ** -0.5),
        'rwkv_k_k': 0.85 + nrm((L, R_W), 0.05),
        'rwkv_k_a': 1.0 + nrm((L, R_W), 0.05),
        'rwkv_r_k': nrm((L, R_HEADS, R_N), 0.1),
        'rwkv_lnx_g': ones_n((L, R_W)),
        'rwkv_lnx_b': nrm((L, R_W), 0.02),
        'gla_alpha_up': nrm((L, G_LORA, G_KW), G_LORA ** -0.5),
        'gla_alpha_b': 1.0 + nrm((L, G_KW), 0.5),
        'gla_norm_g': ones_n((L, G_DV)),
        'proj_attn': nrm((L, A_VW, D), A_VW ** -0.5),
        'proj_rwkv': nrm((L, R_W, D), R_W ** -0.5),
        'proj_gla': nrm((L, G_VW, D), G_VW ** -0.5),
        'w_out': nrm((L, D, D), D ** -0.5),
        'router_grp_w': nrm((L, D, N_GROUPS), D ** -0.5),
        'router_grp_b': nrm((L, N_GROUPS), 0.01),
        'router_exp_w': nrm((L, D, N_EXPERTS), D ** -0.5),
        'router_exp_b': nrm((L, N_EXPERTS), 0.01),
        'exp_w_gate': nrm((L, N_EXPERTS, D, D_EXPERT), D ** -0.5),
        'exp_w_up': nrm((L, N_EXPERTS, D, D_EXPERT), D ** -0.5),
        'exp_w_down': nrm((L, N_EXPERTS, D_EXPERT, D), D_EXPERT ** -0.5),
    }


def reference(x, c, ada_w, ada_b, norm1_g, norm2_g, w_in, attn_qn_g, attn_kn_g, attn_lambda,
              attn_subln_g, rwkv_mu, rwkv_w_up, rwkv_w0, rwkv_a_up, rwkv_a0, rwkv_g_up, rwkv_k_k,
              rwkv_k_a, rwkv_r_k, rwkv_lnx_g, rwkv_lnx_b, gla_alpha_up, gla_alpha_b, gla_norm_g,
              proj_attn, proj_rwkv, proj_gla, w_out, router_grp_w, router_grp_b, router_exp_w,
              router_exp_b, exp_w_gate, exp_w_up, exp_w_down):
    B, S, D = x.shape
    c_act = jax.nn.silu(c)
    for l in range(DEPTH):
        lambda_init = 0.8 - 0.6 * math.exp(-0.3 * l)
        mod = (c_act @ ada_w[l] + ada_b[l])[:, None, :]
        sh1, sc1, gt1, sh2, sc2, gt2 = jnp.split(mod, 6, axis=-1)
        h = rms_norm(x, norm1_g[l]) * (1.0 + sc1) + sh1
        p = h @ w_in[l]
        p_attn, p_rwkv, p_gla, p_gate = _split(p, [A_COLS, R_COLS, G_COLS, GATE_COLS])
        aq, ak, av = _split(p_attn, [A_QW, A_QW, A_VW])
        o_a = diff_attention(aq.reshape(B, S, A_HEADS, 2, A_DH), ak.reshape(B, S, A_HEADS, 2, A_DH),
                             av.reshape(B, S, A_HEADS, A_DV), attn_qn_g[l], attn_kn_g[l],
                             attn_lambda[l], attn_subln_g[l], lambda_init)
        o_r = rwkv7_mix(p_rwkv, rwkv_mu[l], rwkv_w_up[l], rwkv_w0[l], rwkv_a_up[l], rwkv_a0[l],
                        rwkv_g_up[l], rwkv_k_k[l], rwkv_k_a[l], rwkv_r_k[l], rwkv_lnx_g[l],
                        rwkv_lnx_b[l])
        gq, gk, gv, gad, ggate = _split(p_gla, [G_KW, G_KW, G_VW, G_LORA, G_VW])
        log_alpha = jax.nn.log_sigmoid((gad @ gla_alpha_up[l] + gla_alpha_b[l]).astype(jnp.float32)) / G_TAU
        o_g = gla_mix(gq.reshape(B, S, G_HEADS, G_DK), gk.reshape(B, S, G_HEADS, G_DK),
                      gv.reshape(B, S, G_HEADS, G_DV), log_alpha.reshape(B, S, G_HEADS, G_DK),
                      ggate.reshape(B, S, G_HEADS, G_DV), gla_norm_g[l])
        g_a, g_r, g_g = jnp.split(p_gate, 3, axis=-1)
        merged = (jax.nn.sigmoid(g_a) * (o_a @ proj_attn[l])
                  + jax.nn.sigmoid(g_r) * (o_r @ proj_rwkv[l])
                  + jax.nn.sigmoid(g_g) * (o_g @ proj_gla[l]))
        x = x + gt1 * (merged @ w_out[l])
        h2 = rms_norm(x, norm2_g[l]) * (1.0 + sc2) + sh2
        x = x + gt2 * hier_moe(h2, router_grp_w[l], router_grp_b[l], router_exp_w[l], router_exp_b[l],
                               exp_w_gate[l], exp_w_up[l], exp_w_down[l])
    return x
```

```python
import contextlib
import math
import numpy as np
import concourse.bass as bass
import concourse.mybir as mybir
from concourse.bass_utils import run_bass_kernel_spmd

F32 = mybir.dt.float32
BF16 = mybir.dt.bfloat16
I32 = mybir.dt.int32
AF = mybir.ActivationFunctionType
ALU = mybir.AluOpType
AX = mybir.AxisListType

D = 1024
S = 2048
NT = S // 128
DEPTH = 2
N_IN = 7952
NPROJ = 4880
EPS = 1e-6
SAME_ENGINE_SYNC = True


def _region(ap):
    t = ap.tensor
    a = ap.ap
    off = int(ap.offset)
    es = mybir.dt.size(ap.dtype)
    tn = type(t).__name__
    if 'DRam' in tn:
        ext = sum(int(s) * (int(c) - 1) for s, c in a) + 1
        return (t.name, 0, 1, off * es, (off + ext) * es)
    if 'PSum' in tn:
        return (t.name, 0, 128, 0, 1 << 20)
    ps = int(a[0][0])
    npart = int(a[0][1])
    if ps == 0:
        ps = 1 << 40
    p0 = off // ps
    f0 = off % ps
    ext = sum(int(s) * (int(c) - 1) for s, c in a[1:]) + 1
    return (t.name, p0, p0 + npart, f0 * es, (f0 + ext) * es)


def _ovl(r, q):
    return r[1] < q[2] and q[1] < r[2] and r[3] < q[4] and q[3] < r[4]


def _contains(big, small):
    return big[1] <= small[1] and small[2] <= big[2] and big[3] <= small[3] and small[4] <= big[4]


class Sched:
    def __init__(self, nc, stack):
        self.nc = nc
        self.engs = {'pe': nc.tensor, 'act': nc.scalar, 'dve': nc.vector, 'pool': nc.gpsimd, 'sp': nc.sync}
        self.sem = {}
        self.cnt = {}
        for e in ['pe', 'act', 'dve', 'pool']:
            self.sem[e] = stack.enter_context(nc.semaphore('s_' + e))
            self.cnt[e] = 0
        self.ndsem = {'sp': 16, 'pool': 8}
        for q in ['sp', 'pool']:
            for j in range(self.ndsem[q]):
                k = ('d', q, j)
                self.sem[k] = stack.enter_context(nc.semaphore('d_%s_%d' % (q, j)))
                self.cnt[k] = 0
        self.drr = {'sp': 0, 'pool': 0}
        self.known = {e: {} for e in self.engs}
        self.recs = {}
        self.npsum = 0
        self.banks = []
        self.ninst = 0
        import os
        self.max_ops = int(os.environ.get('MAX_OPS', '1000000000'))
        self.force_dma = False
        self.log_ops = bool(os.environ.get('LOG_OPS'))

    def _deps(self, reads, writes):
        deps = {}
        for ap in reads:
            r = _region(ap)
            for rec in self.recs.get(r[0], ()):
                if rec[1] and _ovl(rec[0], r):
                    if deps.get(rec[2], 0) < rec[3]:
                        deps[rec[2]] = rec[3]
        for ap in writes:
            r = _region(ap)
            for rec in self.recs.get(r[0], ()):
                if _ovl(rec[0], r):
                    if deps.get(rec[2], 0) < rec[3]:
                        deps[rec[2]] = rec[3]
        return deps

    def _record(self, reads, writes, k, v, noprune=False):
        for ap in reads:
            r = _region(ap)
            lst = self.recs.setdefault(r[0], [])
            for rec in lst:
                if (not rec[1]) and rec[2] == k and rec[0] == r:
                    rec[3] = max(rec[3], v)
                    break
            else:
                lst.append([r, False, k, v])
        for ap in writes:
            r = _region(ap)
            lst = self.recs.setdefault(r[0], [])
            if not noprune:
                lst[:] = [rec for rec in lst if not _contains(r, rec[0])]
            lst.append([r, True, k, v])

    def _waits(self, e, deps):
        eng = self.engs[e]
        for k, v in deps.items():
            if self.known[e].get(k, 0) >= v:
                continue
            if k == e:
                if e == 'pe' or not SAME_ENGINE_SYNC or v > self.cnt[e]:
                    continue
            eng.wait_ge(self.sem[k], v)
            self.known[e][k] = v
            self.ninst += 1

    def op(self, e, fn, reads, writes, inc=True):
        if self.ninst >= self.max_ops:
            return None
        if self.log_ops:
            import inspect
            fr = inspect.stack()
            print('OP', self.ninst, e, [f.lineno for f in fr[1:4]], flush=True)
        pr = [r for r in reads if 'PSum' in type(r.tensor).__name__]
        if pr:
            reads = [r for r in reads if 'PSum' not in type(r.tensor).__name__]
            writes = list(writes) + pr
        self._waits(e, self._deps(reads, writes))
        ins = fn(self.engs[e])
        self.ninst += 1
        if inc:
            self.cnt[e] += 1
            ins.then_inc(self.sem[e], 1)
            val = self.cnt[e]
        else:
            val = self.cnt[e] + 1
        self._record(reads, writes, e, val)
        return ins

    def dma(self, out, in_, q='sp', **kw):
        if self.ninst >= self.max_ops and not self.force_dma:
            return
        if self.log_ops:
            import inspect
            fr = inspect.stack()
            print('DMA', self.ninst, q, [f.lineno for f in fr[1:3]], flush=True)
        self._waits(q, self._deps([in_], [out]))
        j = self.drr[q]
        self.drr[q] = (j + 1) % self.ndsem[q]
        k = ('d', q, j)
        if self.known[q].get(k, 0) < self.cnt[k]:
            self.engs[q].wait_ge(self.sem[k], self.cnt[k])
            self.known[q][k] = self.cnt[k]
            self.ninst += 1
        self.engs[q].dma_start(out=out, in_=in_, **kw).then_inc(self.sem[k], 16)
        self.ninst += 1
        self.cnt[k] += 16
        self._record([in_], [out], k, self.cnt[k])

    def idma(self, out, in_, idx, scatter, nrows):
        q = 'pool'
        deps = self._deps([in_, idx], [out])
        if scatter:
            deps = {k_: v_ for k_, v_ in deps.items() if not (isinstance(k_, tuple) and k_[1] == 'pool')}
        self._waits(q, deps)
        j = self.drr[q]
        self.drr[q] = (j + 1) % self.ndsem[q]
        k = ('d', q, j)
        if self.known[q].get(k, 0) < self.cnt[k]:
            self.engs[q].wait_ge(self.sem[k], self.cnt[k])
            self.known[q][k] = self.cnt[k]
        off = bass.IndirectOffsetOnAxis(ap=idx, axis=0)
        if not hasattr(self, 'bc_regs'):
            self.bc_regs = {}
        if nrows not in self.bc_regs:
            r = self.engs[q].alloc_register("bc%d" % nrows)
            self.engs[q].reg_mov(r, nrows - 1)
            self.bc_regs[nrows] = r
        bcr = self.bc_regs[nrows]
        if scatter:
            ins = self.engs[q].indirect_dma_start(out=out, out_offset=off, in_=in_, in_offset=None,
                                                  bounds_check=bcr, oob_is_err=False)
        else:
            ins = self.engs[q].indirect_dma_start(out=out, out_offset=None, in_=in_, in_offset=off,
                                                  bounds_check=bcr, oob_is_err=False)
        ins.then_inc(self.sem[k], 16)
        self.ninst += 1
        self.cnt[k] += 16
        self._record([in_, idx], [out], k, self.cnt[k], noprune=scatter)

    def finish(self):
        sp = self.engs['sp']
        for k, v in self.cnt.items():
            if v > 0:
                sp.wait_ge(self.sem[k], v)

    def mm(self, out, lhsT, rhs, start=True, stop=True):
        return self.op('pe', lambda e: e.matmul(out, lhsT, rhs, start=start, stop=stop),
                       [lhsT, rhs], [out], inc=stop)

    def tr(self, out, in_, ident):
        return self.op('pe', lambda e: e.transpose(out, in_, ident), [in_, ident], [out])

    def act(self, out, in_, func, bias=None, scale=None, accum_out=None, eng='act'):
        kw = {}
        reads = [in_]
        writes = [out]
        if bias is not None:
            kw['bias'] = bias
            if not isinstance(bias, (int, float)):
                reads.append(bias)
        if scale is not None:
            kw['scale'] = scale
            if not isinstance(scale, (int, float)):
                reads.append(scale)
        if accum_out is not None:
            kw['accum_out'] = accum_out
            writes.append(accum_out)
        return self.op('act', lambda e: e.activation(out, in_, func, **kw), reads, writes)

    def copy(self, e, out, in_):
        if e == 'act':
            return self.op('act', lambda g: g.copy(out, in_), [in_], [out])
        return self.op(e, lambda g: g.tensor_copy(out, in_), [in_], [out])

    def tt(self, e, out, in0, in1, op):
        return self.op(e, lambda g: g.tensor_tensor(out, in0, in1, op), [in0, in1], [out])

    def ts(self, e, out, in0, s1, s2, op0, op1=None, accum_out=None):
        reads = [in0] + [s for s in (s1, s2) if s is not None and not isinstance(s, (int, float))]
        writes = [out] + ([accum_out] if accum_out is not None else [])
        kw = {}
        if accum_out is not None:
            kw['accum_out'] = accum_out
        if op1 is None:
            return self.op(e, lambda g: g.tensor_scalar(out, in0, s1, s2, op0, **kw), reads, writes)
        return self.op(e, lambda g: g.tensor_scalar(out, in0, s1, s2, op0, op1, **kw), reads, writes)

    def stt(self, e, out, in0, scalar, in1, op0, op1):
        reads = [in0, in1] + ([scalar] if not isinstance(scalar, (int, float)) else [])
        return self.op(e, lambda g: g.scalar_tensor_tensor(out, in0, scalar, in1, op0, op1), reads, [out])

    def memset(self, e, ap, val):
        return self.op(e, lambda g: g.memset(ap, val), [], [ap])

    def reduce(self, e, out, in_, op, axis=None):
        axis = AX.X if axis is None else axis
        return self.op(e, lambda g: g.tensor_reduce(out, in_, axis, op), [in_], [out])

    def bank(self):
        b = self.banks[self.npsum % len(self.banks)]
        self.npsum += 1
        return b


def bc_last(ap, n):
    return bass.AP(ap.tensor, ap.offset, [list(p) for p in ap.ap] + [[0, n]])


def bc_part(ap, n=128):
    dims = [list(p) for p in ap.ap]
    if len(dims) == 2 and dims[0][1] == 1:
        dims = dims[1:]
    return bass.AP(ap.tensor, ap.offset, [[0, n]] + dims)


C_ID, C_ML, C_MSL, C_MSLT, C_MB, C_CI = 0, 128, 256, 384, 512, 640
C_CM = 704
C_TRI, C_ONE, C_ECAP = 960, 1088, 1216
NCONST = 1248
CAP = 512
NE = 32


def make_consts():
    c = np.zeros((128, NCONST), np.float32)
    i = np.arange(128)
    same = (i[:, None] // 64) == (i[None, :] // 64)
    c[:, C_ID:C_ID + 128] = np.eye(128)
    c[:, C_ML:C_ML + 128] = (same & (i[:, None] <= i[None, :]))
    c[:, C_MSL:C_MSL + 128] = (same & (i[:, None] < i[None, :]))
    c[:, C_MSLT:C_MSLT + 128] = (same & (i[:, None] > i[None, :]))
    c[:, C_MB:C_MB + 128] = np.where(i[:, None] > i[None, :], -30000.0, 0.0)
    c[:, C_CI] = (i < 64)
    c[:, C_CI + 1] = (i >= 64)
    c[:, C_CM:C_CM + 64] = 1.0
    c[:, C_CM + 128 + 64:C_CM + 256] = 1.0
    c[:, C_TRI:C_TRI + 128] = (i[:, None] < i[None, :])
    c[:, C_ONE:C_ONE + 128] = 1.0
    c[:, C_ECAP:C_ECAP + 32] = np.arange(32)[None, :] * CAP
    aug = np.zeros((4, 2, 4, S), np.float32)
    t = np.arange(S)
    for h in range(4):
        sl = 2.0 ** (-8.0 * (h + 1) / 4)
        aug[0, 0, h] = 1.0
        aug[1, 0, h] = -sl * (t % 128)
        aug[2, 0, h] = 1.0
        aug[3, 0, h] = -sl * 128 * (t // 128)
        aug[0, 1, h] = sl * (t % 128)
        aug[1, 1, h] = 1.0
        aug[2, 1, h] = sl * 128 * (t // 128)
        aug[3, 1, h] = 1.0
    return c, aug


WSPECS = [
    ('ada_w', [DEPTH, D, 6 * D]), ('ada_b', [DEPTH, 6 * D]), ('norm1_g', [DEPTH, D]), ('norm2_g', [DEPTH, D]),
    ('w_in', [DEPTH, D, N_IN]), ('attn_qkg', [DEPTH, 1024]), ('attn_lambda', [DEPTH, 256]),
    ('attn_subln_g', [DEPTH, 128]), ('rwkv_mu', [DEPTH, 1792]), ('rwkv_w_up', [DEPTH, 64, 512]),
    ('rwkv_w0', [DEPTH, 512]), ('rwkv_a_up', [DEPTH, 64, 512]), ('rwkv_a0', [DEPTH, 512]),
    ('rwkv_g_up', [DEPTH, 128, 512]), ('rwkv_k_k', [DEPTH, 512]), ('rwkv_k_a', [DEPTH, 512]),
    ('rwkv_r_k', [DEPTH, 512]), ('rwkv_lnx_g', [DEPTH, 512]), ('rwkv_lnx_b', [DEPTH, 512]),
    ('gla_alpha_up', [DEPTH, 16, 256]), ('gla_alpha_b', [DEPTH, 256]), ('gla_norm_g4', [DEPTH, 512]),
    ('proj_attn', [DEPTH, 512, D]), ('proj_rwkv', [DEPTH, 512, D]), ('proj_gla', [DEPTH, 512, D]),
    ('w_out', [DEPTH, D, D]), ('router_w', [DEPTH, D, 36]), ('router_b', [DEPTH, 36]),
    ('exp_w_gate', [DEPTH, 32, D, 512]), ('exp_w_up', [DEPTH, 32, D, 512]), ('exp_w_down', [DEPTH, 32, 512, D]),
]


class Arena:
    def __init__(self, ap, nbytes):
        self.ap = ap
        self.nbytes = nbytes
        self.off = 0

    def reset(self):
        self.off = 0

    def get(self, shape, dt=F32, parts=128):
        es = mybir.dt.size(dt)
        n = 1
        for v in shape:
            n *= v
        nb = (n * es + 31) // 32 * 32
        assert self.off + nb <= self.nbytes, ("arena overflow", self.off, nb)
        a = self.ap[:, self.off // 4:(self.off + nb) // 4]
        self.off += nb
        if dt != F32:
            a = a.bitcast(dt)
        a = a[0:parts, 0:n]
        if len(shape) == 2:
            a = a.rearrange("p (a b) -> p a b", a=shape[0])
        elif len(shape) == 3:
            a = a.rearrange("p (a b c) -> p a b c", a=shape[0], b=shape[1])
        return a


def build_program(nlayers=DEPTH, upto='all', dbg=None, phases=None, scr_io=False):
    dbg = dbg or {}
    nc = bass.Bass("TRN2", target_bir_lowering=False)
    dram = {}
    x_in = nc.dram_tensor("x", [S, D], F32, kind="ExternalInput").ap()
    c_in = nc.dram_tensor("c", [128, 8], F32, kind="ExternalInput").ap()
    cst_in = nc.dram_tensor("consts", [128, NCONST], F32, kind="ExternalInput").ap()
    aug_in = nc.dram_tensor("aug", [4, 2, 4, S], F32, kind="ExternalInput").ap()
    W = {}
    for name, shp in WSPECS:
        W[name] = nc.dram_tensor(name, shp, F32, kind="ExternalInput").ap()
    out = nc.dram_tensor("out", [S, D], F32, kind="ExternalOutput").ap()
    P_scr = nc.dram_tensor("P_scr", [S, NPROJ], F32, kind="ExternalInput" if scr_io else "Internal").ap()
    OT_scr = nc.dram_tensor("OT_scr", [3, 512, S], BF16, kind="ExternalOutput" if scr_io else "Internal").ap()
    H2_scr = nc.dram_tensor("H2_scr", [S, D], BF16, kind="Internal").ap()
    XG = nc.dram_tensor("XG", [NE * CAP, D], BF16, kind="Internal").ap()
    YG = nc.dram_tensor("YG", [NE * CAP, D], F32, kind="Internal").ap()
    DBG = {}
    for name, shp in dbg.items():
        DBG[name] = nc.dram_tensor("dbg_" + name, list(shp), F32, kind="ExternalOutput").ap()

    with contextlib.ExitStack() as stack:
        s = Sched(nc, stack)
        banks = [nc.alloc_psum_tensor("pb%d" % i, [128, 512], F32).ap() for i in range(8)]
        s.banks = banks
        hT = nc.alloc_sbuf_tensor("hT", [128, 8, S], BF16).ap()
        modbc = nc.alloc_sbuf_tensor("modbc", [128, 6 * D], F32).ap()
        cst = nc.alloc_sbuf_tensor("cst", [128, NCONST], F32).ap()
        idb = nc.alloc_sbuf_tensor("idb", [128, 128], BF16).ap()
        mbb = nc.alloc_sbuf_tensor("mbb", [128, 128], BF16).ap()
        ones = nc.alloc_sbuf_tensor("ones", [1, 128], F32).ap()
        rows = nc.alloc_sbuf_tensor("rows", [1, 1408], F32).ap()
        csb = nc.alloc_sbuf_tensor("csb", [128, 8], F32).ap()
        ARENA_BYTES = (int(nc.sbuf_bytes_remaining) - 2048) // 1024 * 1024
        print('arena bytes', ARENA_BYTES, flush=True)
        arena_t = nc.alloc_sbuf_tensor("arena", [128, ARENA_BYTES // 4], F32).ap()
        A = Arena(arena_t, ARENA_BYTES)
        idf = cst[:, C_ID:C_ID + 128]
        m_l = cst[:, C_ML:C_ML + 128]
        m_sl = cst[:, C_MSL:C_MSL + 128]
        m_slT = cst[:, C_MSLT:C_MSLT + 128]
        cind = cst[:, C_CI:C_CI + 2]

        rr = [0]

        def alt():
            rr[0] += 1
            return 'act' if rr[0] % 2 else 'dve'

        def evac(out_, in_):
            return s.copy(alt(), out_, in_)

        def dump(name, ap_sb, dst=None):
            if name in DBG:
                s.dma(DBG[name] if dst is None else dst, ap_sb)

        s.dma(cst, cst_in)
        s.dma(csb, c_in)
        s.memset('dve', ones, 1.0)
        s.copy('dve', idb, idf)
        s.copy('dve', mbb, cst[:, C_MB:C_MB + 128])
        s.act(csb, csb, AF.Silu)

        def rstd_from_ssq(dst, ssq, scale, eps, tmp):
            s.ts('dve', tmp, ssq, scale, eps, ALU.mult, ALU.add)
            s.act(tmp, tmp, AF.Sqrt)
            s.op('dve', lambda g: g.reciprocal(dst, tmp), [tmp], [dst])

        def phase_mod(l):
            A.reset()
            adaw = [A.get([8, 512]) for _ in range(2)]
            adab = [A.get([512]) for _ in range(2)]
            cbc = A.get([8, 128])
            for k in range(8):
                s.copy('dve', cbc[:, k:k + 1, :], bc_last(csb[:, k:k + 1], 128))
            for cg in range(12):
                buf = adaw[cg % 2]
                bb = adab[cg % 2]
                s.dma(buf, W['ada_w'][l, :, cg * 512:(cg + 1) * 512].rearrange("(k p) n -> p k n", p=128))
                s.dma(bb, bc_part(W['ada_b'][l:l + 1, cg * 512:(cg + 1) * 512]))
                pb = s.bank()
                for k in range(8):
                    s.mm(pb, cbc[:, k, :], buf[:, k, :], start=(k == 0), stop=(k == 7))
                s.tt('dve', modbc[:, cg * 512:(cg + 1) * 512], pb, bb, ALU.add)

        def phase_norm(l, which, src, router=None):
            A.reset()
            sh = modbc[:, (3 * which) * D:(3 * which + 1) * D]
            sc = modbc[:, (3 * which + 1) * D:(3 * which + 2) * D]
            G1 = A.get([D])
            gb = A.get([D])
            ssq = A.get([NT])
            rstd = A.get([NT])
            tmpn = A.get([NT])
            junk = A.get([D])
            xts = [A.get([D]) for _ in range(3)]
            tmps = [A.get([D]) for _ in range(2)]
            hbs = [A.get([D], BF16 if router is None else F32) for _ in range(2)]
            s.dma(gb, bc_part(W['norm1_g' if which == 0 else 'norm2_g'][l:l + 1, :]))
            s.stt('dve', G1, sc, 1.0, gb, ALU.add, ALU.mult)
            s.memset('dve', ssq, 0.0)
            if router is not None:
                rw, lg_all, hTf = router
                hbbs = [A.get([D], BF16) for _ in range(2)]
            for tt in range(NT):
                xt = xts[tt % 3]
                s.dma(xt, src[tt * 128:(tt + 1) * 128, :])
                s.act(junk, xt, AF.Square, accum_out=ssq[:, tt:tt + 1])
                rstd_from_ssq(rstd[:, tt:tt + 1], ssq[:, tt:tt + 1], 1.0 / D, EPS, tmpn[:, tt:tt + 1])
                tmp = tmps[tt % 2]
                hb = hbs[tt % 2]
                s.stt('dve', tmp, xt, rstd[:, tt:tt + 1], G1, ALU.mult, ALU.mult)
                s.tt('pool', hb, tmp, sh, ALU.add)
                if router is None:
                    pb = s.bank().bitcast(BF16)
                    for k in range(8):
                        s.tr(pb[:, k * 128:(k + 1) * 128], hb[:, k * 128:(k + 1) * 128], idb)
                    evac(hT[:, :, tt * 128:(tt + 1) * 128], pb.rearrange("p (k t) -> p k t", k=8))
                else:
                    hf = hTf[tt % 2]
                    for half in range(2):
                        pb = s.bank()
                        for k in range(4):
                            kk = half * 4 + k
                            s.tr(pb[:, k * 128:(k + 1) * 128], hb[:, kk * 128:(kk + 1) * 128], idf)
                        s.copy('act', hT[:, half * 4:half * 4 + 4, tt * 128:(tt + 1) * 128],
                               pb.rearrange("p (k t) -> p k t", k=4))
                        s.copy('dve', hf[:, half * 4:half * 4 + 4, :], pb.rearrange("p (k t) -> p k t", k=4))
                    pl = s.bank()
                    for k in range(8):
                        s.mm(pl[:, 0:64], hf[:, k, :], rw[:, k, :], start=(k == 0), stop=False)
                    s.mm(pl[:, 0:64], ones[0:1, 0:128], rows[0:1, 1280:1344], start=False, stop=True)
                    s.copy('dve', lg_all[:, tt, :], pl[:, 0:36])
                    hbb = hbbs[tt % 2]
                    s.copy('act', hbb, hb)
                    s.dma(H2_scr[tt * 128:(tt + 1) * 128, :], hbb)

        def phase_proj(l):
            A.reset()
            wb = [A.get([8, 512], BF16) for _ in range(2)]
            stg = [A.get([512]) for _ in range(4)]
            nchunk = (NPROJ + 511) // 512
            si = 0
            for j in range(nchunk):
                c0 = j * 512
                ncol = min(512, NPROJ - c0)
                w = wb[j % 2]
                s.dma(w[:, :, 0:ncol], W['w_in'][l, :, c0:c0 + ncol].rearrange("(k p) n -> p k n", p=128), q='pool')
                for tt in range(NT):
                    pb = s.bank()
                    for k in range(8):
                        s.mm(pb[:, 0:ncol], hT[:, k, tt * 128:(tt + 1) * 128], w[:, k, 0:ncol],
                             start=(k == 0), stop=(k == 7))
                    st = stg[si % 4]
                    si += 1
                    evac(st[:, 0:ncol], pb[:, 0:ncol])
                    s.dma(P_scr[tt * 128:(tt + 1) * 128, c0:c0 + ncol], st[:, 0:ncol])

        def phase_attn(l):
            lambda_init = 0.8 - 0.6 * math.exp(-0.3 * l)
            A.reset()
            qkT = A.get([16, S], BF16, parts=68)
            vaug = A.get([NT, 4 * 129], BF16)
            qkg = A.get([1024])
            sub_g = A.get([128])
            lamb = A.get([256])
            small = A.get([64])
            qk = [A.get([1024]) for _ in range(2)]
            vt = [A.get([512]) for _ in range(2)]
            sq = A.get([1024])
            qn = [A.get([1024], BF16) for _ in range(2)]
            PT = [A.get([512], BF16) for _ in range(3)]
            t0 = [A.get([128]) for _ in range(2)]
            ot = [A.get([128]) for _ in range(2)]
            oa = [A.get([512], BF16) for _ in range(2)]
            oT = [A.get([4, 128], BF16) for _ in range(2)]
            s.dma(qkg, bc_part(W['attn_qkg'][l:l + 1, :]))
            s.dma(sub_g, bc_part(W['attn_subln_g'][l:l + 1, :]))
            s.dma(lamb, bc_part(W['attn_lambda'][l:l + 1, :]))
            lp = small[:, 0:2]
            s.tt('dve', sq[:, 0:64], lamb[:, 0:64], lamb[:, 64:128], ALU.mult)
            s.tt('dve', sq[:, 64:128], lamb[:, 128:192], lamb[:, 192:256], ALU.mult)
            s.reduce('dve', lp, sq[:, 0:128].rearrange("p (a b) -> p a b", a=2), ALU.add)
            s.act(lp, lp, AF.Exp)
            nlam = small[:, 2:3]
            s.tt('dve', nlam, lp[:, 1:2], lp[:, 0:1], ALU.subtract)
            s.ts('dve', nlam, nlam, -lambda_init, None, ALU.add)
            for g in range(16):
                isk = g // 8
                h = (g % 8) // 2
                s.dma(qkT[64:68, g, :], aug_in[:, isk, h, :], q='pool')
            s.memset('dve', vaug, 1.0)
            ssq = small[:, 8:24]
            rs = small[:, 24:40]
            tm = small[:, 40:56]
            for tt in range(NT):
                q_ = qk[tt % 2]
                v_ = vt[tt % 2]
                s.dma(q_, P_scr[tt * 128:(tt + 1) * 128, 0:1024])
                s.dma(v_, P_scr[tt * 128:(tt + 1) * 128, 1024:1536])
                s.tt('pool', sq, q_, q_, ALU.mult)
                s.reduce('dve', ssq, sq.rearrange("p (a b) -> p a b", a=16), ALU.add)
                rstd_from_ssq(rs[:, 0:8], ssq[:, 0:8], 1.0, 64 * EPS, tm[:, 0:8])
                rstd_from_ssq(rs[:, 8:16], ssq[:, 8:16], 1.0 / 64, EPS, tm[:, 8:16])
                s.tt('dve', sq.rearrange("p (a b) -> p a b", a=16), q_.rearrange("p (a b) -> p a b", a=16),
                     bc_last(rs, 64), ALU.mult)
                qb_ = qn[tt % 2]
                s.tt('pool', qb_, sq, qkg, ALU.mult)
                for half in range(2):
                    pb = s.bank().bitcast(BF16)
                    for j in range(8):
                        g = half * 8 + j
                        s.tr(pb[0:64, j * 128:(j + 1) * 128], qb_[:, g * 64:(g + 1) * 64], idb)
                    evac(qkT[0:64, half * 8:half * 8 + 8, tt * 128:(tt + 1) * 128],
                         pb[0:64, :].rearrange("p (g t) -> p g t", g=8))
                s.copy('act', vaug[:, tt, :].rearrange("p (h d) -> p h d", h=4)[:, :, 0:128],
                       v_.rearrange("p (h d) -> p h d", h=4))
            import os
            stage = int(os.environ.get('ATT_STAGE', '9'))
            if stage < 2:
                return
            SB = banks[0:4]
            ACC = banks[4:8]
            sbi = 0
            pti = 0
            for qb in range(NT):
                for h in range(4):
                    accs = [ACC[(2 * (qb * 4 + h)) % 4], ACC[(2 * (qb * 4 + h) + 1) % 4]]
                    for c in range(2):
                        gq = h * 2 + c
                        gk = 8 + h * 2 + c
                        acc = accs[c]
                        for jg in range(qb // 4 + 1):
                            jbs = list(range(jg * 4, min(jg * 4 + 4, qb + 1)))
                            sbk = SB[sbi % 4]
                            sbi += 1
                            for jj, jb in enumerate(jbs):
                                diag = (jb == qb)
                                s.mm(sbk[:, jj * 128:(jj + 1) * 128], qkT[0:68, gk, jb * 128:(jb + 1) * 128],
                                     qkT[0:68, gq, qb * 128:(qb + 1) * 128], start=True, stop=not diag)
                                if diag:
                                    s.mm(sbk[:, jj * 128:(jj + 1) * 128], idb, mbb, start=False, stop=True)
                            pt = PT[pti % 3]
                            pti += 1
                            n = len(jbs) * 128
                            s.act(pt[:, 0:n], sbk[:, 0:n], AF.Exp)
                            for jj, jb in enumerate(jbs):
                                s.mm(acc[:, 0:129], pt[:, jj * 128:(jj + 1) * 128],
                                     vaug[:, jb, h * 129:(h + 1) * 129], start=(jb == 0), stop=(jb == qb))
                    if stage < 3:
                        continue
                    i2 = (qb * 4 + h) % 2
                    rd = small[:, 56 + 4 * i2:56 + 4 * i2 + 4]
                    s.op('dve', lambda g, a=rd[:, 0:1], b=accs[0][:, 128:129]: g.reciprocal(a, b),
                         [accs[0][:, 128:129]], [rd[:, 0:1]])
                    s.op('dve', lambda g, a=rd[:, 1:2], b=accs[1][:, 128:129]: g.reciprocal(a, b),
                         [accs[1][:, 128:129]], [rd[:, 1:2]])
                    s.tt('dve', rd[:, 1:2], rd[:, 1:2], nlam, ALU.mult)
                    s.ts('dve', t0[i2], accs[0][:, 0:128], rd[:, 0:1], None, ALU.mult)
                    s.stt('dve', ot[i2], accs[1][:, 0:128], rd[:, 1:2], t0[i2], ALU.mult, ALU.add)
                    s.memset('dve', rd[:, 2:3], 0.0)
                    s.act(t0[i2], ot[i2], AF.Square, accum_out=rd[:, 2:3])
                    rstd_from_ssq(rd[:, 2:3], rd[:, 2:3], 1.0 / 128, EPS, rd[:, 3:4])
                    s.ts('dve', rd[:, 2:3], rd[:, 2:3], 1.0 - lambda_init, None, ALU.mult)
                    oab = oa[qb % 2]
                    s.stt('dve', oab[:, h * 128:(h + 1) * 128], ot[i2], rd[:, 2:3], sub_g, ALU.mult, ALU.mult)
                if stage < 4:
                    continue
                oab = oa[qb % 2]
                pb = banks[(qb % 2)].bitcast(BF16)
                for k in range(4):
                    s.tr(pb[:, k * 128:(k + 1) * 128], oab[:, k * 128:(k + 1) * 128], idb)
                o_t = oT[qb % 2]
                evac(o_t, pb[:, 0:512].rearrange("p (k t) -> p k t", k=4))
                s.dma(OT_scr[0, :, qb * 128:(qb + 1) * 128].rearrange("(k p) t -> p k t", p=128), o_t)

        def chunk_core(H, Vd, Rb, Kb, Ab, Bb, Vt, epos, Hst, Y, wk, lowrank, KbM, BbM):
            G = wk['G']
            nq = 4 if lowrank else 2
            m_l2 = bass.AP(m_l.tensor, m_l.offset, [list(m_l.ap[0]), [0, 2], [1, 128]])
            m_sl2 = bass.AP(m_sl.tensor, m_sl.offset, [list(m_sl.ap[0]), [0, 2], [1, 128]])
            for g0 in range(0, H, G):
                hs = list(range(g0, min(g0 + G, H)))
                SL = {h: wk['slots'][j] for j, h in enumerate(hs)}
                ksl = lambda h: slice(h * 64, (h + 1) * 64)
                vsl = lambda h: slice(h * Vd, (h + 1) * Vd)
                pbs = {}
                for h in hs:
                    pb = s.bank()
                    pbs[h] = pb
                    for qi, src in enumerate([Rb, Kb, Ab, Bb][:nq]):
                        s.tr(pb[0:64, qi * 128:(qi + 1) * 128], src[:, ksl(h)], idf)
                for h in hs:
                    evac(SL[h]['FT'][:, 0:nq, :], pbs[h][0:64, 0:nq * 128].rearrange("p (q t) -> p q t", q=nq))
                for h in hs:
                    pb = s.bank()
                    pbs[h] = pb
                    s.tr(pb[0:64, 0:128], epos[:, ksl(h)], idf)
                for h in hs:
                    evac(SL[h]['ET'], pbs[h][0:64, 0:128])
                for h in hs:
                    FT = SL[h]['FT']
                    RT, KT = FT[:, 0, :], FT[:, 1, :]
                    pm = s.bank()
                    pbs[h] = pm
                    s.mm(pm[:, 0:128], KT, RT)
                    if lowrank:
                        AT, BT = FT[:, 2, :], FT[:, 3, :]
                        s.mm(pm[:, 128:256], BT, RT)
                        s.mm(pm[:, 256:384], KT, AT)
                        s.mm(pm[:, 384:512], BT, AT)
                for h in hs:
                    M1 = SL[h]['M1']
                    pm = pbs[h]
                    if lowrank:
                        s.tt('dve', M1[:, 0:2, :], pm[:, 0:256].rearrange("p (a b) -> p a b", a=2), m_l2, ALU.mult)
                        s.tt('dve', M1[:, 2:4, :], pm[:, 256:512].rearrange("p (a b) -> p a b", a=2), m_sl2, ALU.mult)
                    else:
                        s.tt('dve', M1[:, 0, :], pm[:, 0:128], m_l, ALU.mult)
                if lowrank:
                    cur = {}
                    for h in hs:
                        FT = SL[h]['FT']
                        pa = s.bank()
                        pbs[h] = pa
                        s.mm(pa[:, 0:128], FT[:, 2, :], FT[:, 3, :])
                    for h in hs:
                        sl = SL[h]
                        s.tt('dve', sl['P'][0], pbs[h][:, 0:128], m_slT, ALU.mult)
                        s.tt('pool', sl['W'][0], sl['M1'][:, 3, :], idf, ALU.add)
                        cur[h] = [sl['P'][0], sl['M1'][:, 3, :], sl['W'][0]]
                    for i in range(1, 6):
                        for h in hs:
                            Pc, Qc, Wc = cur[h]
                            pp = s.bank()
                            pbs[h] = pp
                            s.mm(pp[:, 0:128], Qc, Pc)
                            if i < 5:
                                s.mm(pp[:, 128:256], Pc, Qc)
                        for h in hs:
                            sl = SL[h]
                            Pn = sl['P'][i % 2]
                            s.copy('act', Pn, pbs[h][:, 0:128])
                            if i < 5:
                                Qn = sl['Q'][i % 2]
                                s.copy('dve', Qn, pbs[h][:, 128:256])
                                cur[h][1] = Qn
                            cur[h][0] = Pn
                        for h in hs:
                            pw = s.bank()
                            pbs[h] = pw
                            s.mm(pw[:, 0:128], cur[h][0], cur[h][2])
                        for h in hs:
                            Wn = SL[h]['W'][i % 2]
                            s.tt('dve', Wn, pbs[h][:, 0:128], cur[h][2], ALU.add)
                            cur[h][2] = Wn
                    for h in hs:
                        pz = s.bank()
                        pbs[h] = pz
                        s.mm(pz[:, 0:Vd], SL[h]['M1'][:, 2, :], Vt[:, vsl(h)])
                    for h in hs:
                        ZA = SL[h]['ZA']
                        s.copy('act', ZA[:, 0:Vd], pbs[h][:, 0:Vd])
                        s.copy('dve', ZA[:, Vd:Vd + 64], Ab[:, ksl(h)])
                    for h in hs:
                        pu = s.bank()
                        pbs[h] = pu
                        s.mm(pu[:, 0:Vd + 64], cur[h][2], SL[h]['ZA'][:, 0:Vd + 64])
                    for h in hs:
                        s.copy('act', SL[h]['UA'][:, 0:Vd + 64], pbs[h][:, 0:Vd + 64])
                    for h in hs:
                        pr = s.bank()
                        pbs[h] = pr
                        s.mm(pr[0:64, 0:128], SL[h]['UA'][:, Vd:Vd + 64], SL[h]['M1'][:, 1, :])
                    for h in hs:
                        s.tt('dve', SL[h]['RtT'], pbs[h][0:64, 0:128], SL[h]['FT'][:, 0, :], ALU.add)
                RtTs = {h: (SL[h]['RtT'] if lowrank else SL[h]['FT'][:, 0, :]) for h in hs}
                for c in range(2):
                    for h in hs:
                        s.copy('dve', SL[h]['Hc'][:, c, :], Hst[:, h, :])
                    if lowrank:
                        for h in hs:
                            pn = s.bank()
                            pbs[h] = pn
                            s.mm(pn[0:64, 0:64], SL[h]['UA'][:, Vd:Vd + 64], BbM[c][:, ksl(h)])
                            s.mm(pn[0:64, 64:64 + Vd], BbM[c][:, ksl(h)], SL[h]['UA'][:, 0:Vd], start=True, stop=False)
                            s.mm(pn[0:64, 64:64 + Vd], KbM[c][:, ksl(h)], Vt[:, vsl(h)], start=False, stop=True)
                        for h in hs:
                            s.tt('dve', SL[h]['BTI'], pbs[h][0:64, 0:64], idf[0:64, 0:64], ALU.add)
                        for h in hs:
                            s.mm(pbs[h][0:64, 256:256 + Vd], SL[h]['BTI'], SL[h]['Hc'][:, c, :], start=True, stop=True)
                        for h in hs:
                            gsc = SL[h]['ET'][:, c * 64 + 63:c * 64 + 64]
                            s.ts('dve', SL[h]['tmpH'], pbs[h][0:64, 256:256 + Vd], gsc, None, ALU.mult)
                            s.stt('dve', Hst[:, h, :], pbs[h][0:64, 64:64 + Vd], gsc, SL[h]['tmpH'], ALU.mult, ALU.add)
                    else:
                        for h in hs:
                            pn = s.bank()
                            pbs[h] = pn
                            s.mm(pn[0:64, 0:Vd], KbM[c][:, ksl(h)], Vt[:, vsl(h)], start=True, stop=True)
                        for h in hs:
                            gsc = SL[h]['ET'][:, c * 64 + 63:c * 64 + 64]
                            s.ts('dve', SL[h]['tmpH'], SL[h]['Hc'][:, c, :], gsc, None, ALU.mult)
                            s.stt('dve', Hst[:, h, :], pbs[h][0:64, 0:Vd], gsc, SL[h]['tmpH'], ALU.mult, ALU.add)
                for h in hs:
                    for c in range(2):
                        s.tt('pool', SL[h]['RtM'][:, c, :], RtTs[h], cst[0:64, C_CM + c * 128:C_CM + (c + 1) * 128], ALU.mult)
                pys = {}
                for h in hs:
                    py = s.bank()
                    pys[h] = py
                    if lowrank:
                        s.mm(py[:, 0:Vd], SL[h]['M1'][:, 1, :], SL[h]['UA'][:, 0:Vd], start=True, stop=False)
                        s.mm(py[:, 0:Vd], SL[h]['M1'][:, 0, :], Vt[:, vsl(h)], start=False, stop=True)
                    else:
                        s.mm(py[:, 0:Vd], SL[h]['M1'][:, 0, :], Vt[:, vsl(h)], start=True, stop=True)
                for h in hs:
                    s.copy('act', Y[:, vsl(h)], pys[h][:, 0:Vd])
                for h in hs:
                    pyh = s.bank()
                    pys[h] = pyh
                    for c in range(2):
                        s.mm(pyh[:, 0:Vd], SL[h]['RtM'][:, c, :], SL[h]['Hc'][:, c, :], start=(c == 0), stop=(c == 1))
                for h in hs:
                    s.tt('dve', Y[:, vsl(h)], pys[h][:, 0:Vd], Y[:, vsl(h)], ALU.add)

        def alloc_wk(Vd, lowrank, G=4):
            wk = {'G': G, 'slots': []}
            for j in range(G):
                sl = {}
                sl['FT'] = A.get([4 if lowrank else 2, 128], F32, parts=64)
                sl['ET'] = A.get([128], F32, parts=64)
                sl['M1'] = A.get([4 if lowrank else 1, 128])
                sl['Hc'] = A.get([2, Vd], F32, parts=64)
                sl['tmpH'] = A.get([Vd], F32, parts=64)
                sl['RtM'] = A.get([2, 128], F32, parts=64)
                if lowrank:
                    sl['P'] = [A.get([128]) for _ in range(2)]
                    sl['Q'] = [A.get([128]) for _ in range(2)]
                    sl['W'] = [A.get([128]) for _ in range(2)]
                    sl['ZA'] = A.get([Vd + 64])
                    sl['UA'] = A.get([Vd + 64])
                    sl['RtT'] = A.get([128], F32, parts=64)
                    sl['BTI'] = A.get([64], F32, parts=64)
                wk['slots'].append(sl)
            return wk

        def gammas(H, lw, gam):
            pg = s.bank()
            for h in range(H):
                s.mm(pg[0:64, h * 32:(h + 1) * 32], lw[:, h * 64:(h + 1) * 64], cst[:, C_CI:C_CI + 32])
            s.act(gam, pg[0:64, 0:32 * H].rearrange("p (h c) -> p h c", h=H)[:, :, 0:2], AF.Exp)

        def store_T(idx, src_bf, tt, oTb):
            pb = s.bank().bitcast(BF16)
            for k in range(4):
                s.tr(pb[:, k * 128:(k + 1) * 128], src_bf[:, k * 128:(k + 1) * 128], idb)
            evac(oTb, pb[:, 0:512].rearrange("p (k t) -> p k t", k=4))
            s.dma(OT_scr[idx, :, tt * 128:(tt + 1) * 128].rearrange("(k p) t -> p k t", p=128), oTb)

        def phase_rwkv(l):
            A.reset()
            wk = alloc_wk(64, True)
            mu = A.get([1792])
            wup = A.get([512], F32, parts=64)
            aup = A.get([512], F32, parts=64)
            gup = A.get([512])
            kkb = A.get([512])
            kab = A.get([512])
            rkb = A.get([512])
            lng = A.get([512])
            lnb = A.get([512])
            Hst = A.get([8, 64], F32, parts=64)
            cur = [A.get([1792]) for _ in range(2)]
            prv = A.get([1792])
            lora = A.get([256])
            sgT = A.get([128])
            lw = A.get([512])
            av = A.get([512])
            gv = A.get([512])
            epos = A.get([512])
            eneg = A.get([512])
            eprv = A.get([512])
            kkn = A.get([512])
            kmod = A.get([512])
            Rb = A.get([512])
            Ab = A.get([512])
            Bb = A.get([512])
            Kb = A.get([512])
            KbM = [A.get([512]) for _ in range(2)]
            BbM = [A.get([512]) for _ in range(2)]
            Y = A.get([512])
            t1 = A.get([512])
            t2 = A.get([512])
            st8 = A.get([64])
            orb = [A.get([512], BF16) for _ in range(2)]
            oTb = [A.get([4, 128], BF16) for _ in range(2)]
            s.dma(mu, bc_part(W['rwkv_mu'][l:l + 1, :]))
            s.dma(wup, W['rwkv_w_up'][l])
            s.dma(aup, W['rwkv_a_up'][l])
            s.dma(gup, W['rwkv_g_up'][l])
            s.dma(rows[0:1, 0:512], W['rwkv_w0'][l:l + 1, :])
            s.dma(rows[0:1, 512:1024], W['rwkv_a0'][l:l + 1, :])
            for dst, nm in ((kkb, 'rwkv_k_k'), (kab, 'rwkv_k_a'), (rkb, 'rwkv_r_k'), (lng, 'rwkv_lnx_g'),
                            (lnb, 'rwkv_lnx_b')):
                s.dma(dst, bc_part(W[nm][l:l + 1, :]))
            s.memset('dve', Hst, 0.0)
            v3 = lambda ap: ap.rearrange("p (h k) -> p h k", h=8)
            import os
            for tt in range(int(os.environ.get('RW_TILES', NT))):
                cu = cur[tt % 2]
                r0 = tt * 128
                s.dma(cu, P_scr[r0:r0 + 128, 1536:3328])
                if tt == 0:
                    s.memset('dve', prv[0:1, :], 0.0)
                    s.dma(prv[1:128, :], P_scr[0:127, 1536:3328])
                else:
                    s.dma(prv, P_scr[r0 - 1:r0 + 127, 1536:3328])
                s.tt('pool', prv, prv, cu, ALU.subtract)
                s.tt('dve', prv, prv, mu, ALU.mult)
                s.tt('pool', cu, cu, prv, ALU.add)
                r_, k_, v_ = cu[:, 0:512], cu[:, 512:1024], cu[:, 1024:1536]
                pl = s.bank()
                s.tr(pl[0:64, 0:128], cu[:, 1536:1600], idf)
                s.tr(pl[0:64, 128:256], cu[:, 1600:1664], idf)
                s.tr(pl[:, 256:384], cu[:, 1664:1792], idf)
                s.act(lora[0:64, 0:128], pl[0:64, 0:128], AF.Tanh)
                s.copy('dve', lora[0:64, 128:256], pl[0:64, 128:256])
                s.act(sgT, pl[:, 256:384], AF.Sigmoid)
                px = s.bank()
                s.mm(px, lora[0:64, 0:128], wup, start=True, stop=False)
                s.mm(px, ones[0:1, 0:128], rows[0:1, 0:512], start=False, stop=True)
                s.act(lw, px, AF.Sigmoid)
                s.ts('dve', lw, lw, -math.exp(-0.5), None, ALU.mult)
                pa = s.bank()
                s.mm(pa, lora[0:64, 128:256], aup, start=True, stop=False)
                s.mm(pa, ones[0:1, 0:128], rows[0:1, 512:1024], start=False, stop=True)
                s.act(av, pa, AF.Sigmoid)
                pg = s.bank()
                s.mm(pg, sgT, gup)
                s.copy('dve', gv, pg)
                pc = s.bank()
                s.mm(pc, m_l, lw)
                s.act(epos, pc, AF.Exp)
                s.act(eneg, pc, AF.Exp, scale=-1.0)
                s.tt('dve', t1, pc, lw, ALU.subtract)
                s.act(eprv, t1, AF.Exp)
                s.tt('pool', kkn, k_, kkb, ALU.mult)
                s.tt('dve', t1, kkn, kkn, ALU.mult)
                s.reduce('dve', st8[:, 0:8], v3(t1), ALU.add)
                s.act(st8[:, 8:16], st8[:, 0:8], AF.Sqrt)
                s.ts('dve', st8[:, 8:16], st8[:, 8:16], 1e-12, None, ALU.max)
                s.op('dve', lambda g: g.reciprocal(st8[:, 16:24], st8[:, 8:16]), [st8[:, 8:16]], [st8[:, 16:24]])
                s.tt('dve', v3(kkn), v3(kkn), bc_last(st8[:, 16:24], 64), ALU.mult)
                s.stt('dve', t2, av, -1.0, kab, ALU.add, ALU.mult)
                s.stt('dve', kmod, t2, 1.0, k_, ALU.add, ALU.mult)
                s.tt('pool', Rb, r_, epos, ALU.mult)
                s.stt('dve', Ab, kkn, -1.0, eprv, ALU.mult, ALU.mult)
                s.tt('pool', t2, kkn, av, ALU.mult)
                s.tt('dve', Bb, t2, eneg, ALU.mult)
                s.tt('pool', Kb, kmod, eneg, ALU.mult)
                import os
                rst = int(os.environ.get('RW_STAGE', '9'))
                if rst < 2:
                    continue
                if rst < 3:
                    continue
                for c in range(2):
                    s.ts('dve', KbM[c], Kb, cst[:, C_CI + c:C_CI + c + 1], None, ALU.mult)
                    s.ts('dve', BbM[c], Bb, cst[:, C_CI + c:C_CI + c + 1], None, ALU.mult)
                chunk_core(8, 64, Rb, Kb, Ab, Bb, v_, epos, Hst, Y, wk, True, KbM, BbM)
                if rst < 4:
                    continue
                s.reduce('dve', st8[:, 24:32], v3(Y), ALU.add)
                s.tt('pool', t1, Y, Y, ALU.mult)
                s.reduce('dve', st8[:, 32:40], v3(t1), ALU.add)
                s.ts('dve', st8[:, 24:32], st8[:, 24:32], 1.0 / 64, None, ALU.mult)
                s.tt('dve', st8[:, 40:48], st8[:, 24:32], st8[:, 24:32], ALU.mult)
                s.stt('dve', st8[:, 32:40], st8[:, 32:40], 1.0 / 64, st8[:, 40:48], ALU.mult, ALU.subtract)
                rstd_from_ssq(st8[:, 48:56], st8[:, 32:40], 1.0, 64e-5, st8[:, 56:64])
                s.tt('dve', v3(t1), v3(Y), bc_last(st8[:, 24:32], 64), ALU.subtract)
                s.tt('dve', v3(t1), v3(t1), bc_last(st8[:, 48:56], 64), ALU.mult)
                s.tt('pool', t1, t1, lng, ALU.mult)
                s.tt('pool', t1, t1, lnb, ALU.add)
                s.tt('dve', t2, r_, kmod, ALU.mult)
                s.tt('dve', t2, t2, rkb, ALU.mult)
                s.reduce('dve', st8[:, 0:8], v3(t2), ALU.add)
                s.tt('dve', v3(t2), v3(v_), bc_last(st8[:, 0:8], 64), ALU.mult)
                s.tt('pool', t1, t1, t2, ALU.add)
                ob = orb[tt % 2]
                s.tt('dve', ob, t1, gv, ALU.mult)
                store_T(1, ob, tt, oTb[tt % 2])

        def phase_gla(l):
            A.reset()
            wk = alloc_wk(128, False)
            aup = A.get([256], F32, parts=16)
            ng = A.get([512])
            Hst = A.get([4, 128], F32, parts=64)
            cur = [A.get([1552]) for _ in range(2)]
            adT = A.get([128], F32, parts=16)
            ll = A.get([256])
            epos = A.get([256])
            eneg = A.get([256])
            Rb = A.get([256])
            Kb = A.get([256])
            KbM = [A.get([256]) for _ in range(2)]
            Y = A.get([512])
            t1 = A.get([512])
            sg = A.get([512])
            st4 = A.get([16])
            ogb = [A.get([512], BF16) for _ in range(2)]
            oTb = [A.get([4, 128], BF16) for _ in range(2)]
            s.dma(aup, W['gla_alpha_up'][l])
            s.dma(rows[0:1, 1024:1280], W['gla_alpha_b'][l:l + 1, :])
            s.dma(ng, bc_part(W['gla_norm_g4'][l:l + 1, :]))
            s.memset('dve', Hst, 0.0)
            v4 = lambda ap: ap.rearrange("p (h k) -> p h k", h=4)
            for tt in range(NT):
                cu = cur[tt % 2]
                r0 = tt * 128
                s.dma(cu, P_scr[r0:r0 + 128, 3328:4880])
                q_, k_, v_, gate = cu[:, 0:256], cu[:, 256:512], cu[:, 512:1024], cu[:, 1040:1552]
                pl = s.bank()
                s.tr(pl[0:16, 0:128], cu[:, 1024:1040], idf)
                s.copy('dve', adT, pl[0:16, 0:128])
                pz = s.bank()
                s.mm(pz[:, 0:256], adT, aup, start=True, stop=False)
                s.mm(pz[:, 0:256], ones[0:1, 0:128], rows[0:1, 1024:1280], start=False, stop=True)
                s.act(ll, pz[:, 0:256], AF.Sigmoid)
                s.act(ll, ll, AF.Ln)
                s.ts('dve', ll, ll, 1.0 / 16, None, ALU.mult)
                pc = s.bank()
                s.mm(pc[:, 0:256], m_l, ll)
                s.act(epos, pc[:, 0:256], AF.Exp)
                s.act(eneg, pc[:, 0:256], AF.Exp, scale=-1.0)
                s.stt('dve', Rb, q_, 0.125, epos, ALU.mult, ALU.mult)
                s.tt('pool', Kb, k_, eneg, ALU.mult)
                for c in range(2):
                    s.ts('dve', KbM[c], Kb, cst[:, C_CI + c:C_CI + c + 1], None, ALU.mult)
                chunk_core(4, 128, Rb, Kb, None, None, v_, epos, Hst, Y, wk, False, KbM, None)
                s.tt('pool', t1, Y, Y, ALU.mult)
                s.reduce('dve', st4[:, 0:4], v4(t1), ALU.add)
                rstd_from_ssq(st4[:, 4:8], st4[:, 0:4], 1.0 / 128, EPS, st4[:, 8:12])
                s.tt('dve', v4(t1), v4(Y), bc_last(st4[:, 4:8], 128), ALU.mult)
                s.tt('pool', t1, t1, ng, ALU.mult)
                s.act(sg, gate, AF.Silu)
                ob = ogb[tt % 2]
                s.tt('dve', ob, t1, sg, ALU.mult)
                store_T(2, ob, tt, oTb[tt % 2])

        def phase_merge(l, xsrc):
            A.reset()
            gt1 = modbc[:, 2 * D:3 * D]
            gw = A.get([8, 3072], BF16)
            pw = A.get([12, 1024], BF16)
            ow = A.get([8, 1024], BF16)
            oTs = [A.get([12, 512], BF16) for _ in range(2)]
            mT = A.get([8, 512], BF16)
            macc = A.get([512])
            sgm = A.get([512])
            xt = [A.get([1024]) for _ in range(2)]
            tmp = A.get([512])
            for j in range(6):
                s.dma(gw[:, :, j * 512:(j + 1) * 512],
                      W['w_in'][l, :, NPROJ + j * 512:NPROJ + (j + 1) * 512].rearrange("(k p) n -> p k n", p=128),
                      q='pool')
            for bi, nm in enumerate(('proj_attn', 'proj_rwkv', 'proj_gla')):
                s.dma(pw[:, bi * 4:(bi + 1) * 4, :], W[nm][l].rearrange("(k p) n -> p k n", p=128), q='pool')
            s.dma(ow, W['w_out'][l].rearrange("(k p) n -> p k n", p=128), q='pool')
            for tg in range(4):
                ts_ = slice(tg * 512, (tg + 1) * 512)
                oT = oTs[tg % 2]
                for bi in range(3):
                    s.dma(oT[:, bi * 4:(bi + 1) * 4, :], OT_scr[bi, :, ts_].rearrange("(k p) t -> p k t", p=128))
                for dc in range(8):
                    for bi in range(3):
                        pg = s.bank()
                        for k in range(8):
                            s.mm(pg, gw[:, k, bi * 1024 + dc * 128:bi * 1024 + (dc + 1) * 128], hT[:, k, ts_],
                                 start=(k == 0), stop=(k == 7))
                        pp = s.bank()
                        for k in range(4):
                            s.mm(pp, pw[:, bi * 4 + k, dc * 128:(dc + 1) * 128], oT[:, bi * 4 + k, :],
                                 start=(k == 0), stop=(k == 3))
                        s.act(sgm, pg, AF.Sigmoid)
                        if bi == 0:
                            s.tt('dve', macc, pp, sgm, ALU.mult)
                        else:
                            s.tt('dve', sgm, pp, sgm, ALU.mult)
                            if bi == 1:
                                s.tt('pool', macc, macc, sgm, ALU.add)
                            else:
                                s.tt('pool', mT[:, dc, :], macc, sgm, ALU.add)
                for t4 in range(4):
                    tt = tg * 4 + t4
                    x_ = xt[tt % 2]
                    s.dma(x_, xsrc[tt * 128:(tt + 1) * 128, :])
                    for half in range(2):
                        hs = slice(half * 512, (half + 1) * 512)
                        po = s.bank()
                        for k in range(8):
                            s.mm(po, mT[:, k, t4 * 128:(t4 + 1) * 128], ow[:, k, hs], start=(k == 0), stop=(k == 7))
                        s.tt('dve', tmp, po, gt1[:, hs], ALU.mult)
                        s.tt('pool', x_[:, hs], x_[:, hs], tmp, ALU.add)
                    s.dma(out[tt * 128:(tt + 1) * 128, :], x_)

        def phase_moe(l):
            A.reset()
            gt2 = modbc[:, 5 * D:6 * D]
            slots_i = A.get([NT, 2], I32)
            wts2 = A.get([NT, 2])
            mark = A.off
            rw = A.get([8, 64])
            lg_all = A.get([NT, 36])
            hTf = [A.get([8, 128]) for _ in range(2)]
            s.memset('dve', rw, 0.0)
            s.memset('dve', rows[0:1, 1280:1344], 0.0)
            s.dma(rw[:, :, 0:36], W['router_w'][l].rearrange("(k p) n -> p k n", p=128))
            s.dma(rows[0:1, 1280:1316], W['router_b'][l:l + 1, :])
            base = A.off
            A2 = Arena(A.ap[:, base // 4:], A.nbytes - base)
            _norm_with(A2, l, rw, lg_all, hTf)
            G3 = lambda: A2.get([NT, 4])
            E3 = lambda: A2.get([NT, 32])
            T1 = lambda: A2.get([NT])
            oh, pen, ge = G3(), G3(), G3()
            em, m1, m2, am, pos, ovf = E3(), E3(), E3(), E3(), E3(), E3()
            carry_all = A2.get([NT + 1, 32])
            gmax, gs, gp, v1, v2, dd, ex, w1, w2 = T1(), T1(), T1(), T1(), T1(), T1(), T1(), T1(), T1()
            slots_f = A2.get([NT, 2])
            hrow = [A2.get([D], BF16) for _ in range(2)]
            gl = lg_all[:, :, 0:4]
            el = lg_all[:, :, 4:36]
            s.reduce('dve', gmax, gl, ALU.max)
            s.tt('dve', oh, gl, bc_last(gmax, 4), ALU.is_equal)
            s.tt('dve', ge, gl, bc_last(gmax, 4), ALU.subtract)
            s.act(ge, ge, AF.Exp)
            s.reduce('dve', gs, ge, ALU.add)
            s.op('dve', lambda g: g.reciprocal(gp, gs), [gs], [gp])
            s.ts('dve', pen, oh, 1e9, -1e9, ALU.mult, ALU.add)
            s.copy('dve', em, el)
            em64 = em.rearrange("p t (g e) -> p (t g) e", g=4)
            s.tt('dve', em64, em64, bc_last(pen.rearrange("p t g -> p (t g)"), 8), ALU.add)
            s.reduce('dve', v1, em, ALU.max)
            s.tt('dve', m1, em, bc_last(v1, 32), ALU.is_equal)
            s.stt('dve', em, m1, -1e9, em, ALU.mult, ALU.add)
            s.reduce('dve', v2, em, ALU.max)
            s.tt('dve', m2, em, bc_last(v2, 32), ALU.is_equal)
            s.tt('dve', dd, v2, v1, ALU.subtract)
            s.act(ex, dd, AF.Exp)
            s.ts('dve', w1, ex, 1.0, None, ALU.add)
            s.op('dve', lambda g: g.reciprocal(w1, w1), [w1], [w1])
            s.tt('dve', w2, ex, w1, ALU.mult)
            s.tt('dve', wts2[:, :, 0], w1, gp, ALU.mult)
            s.tt('dve', wts2[:, :, 1], w2, gp, ALU.mult)
            s.tt('dve', am, m1, m2, ALU.add)
            amf = am.rearrange("p t e -> p (t e)")
            pp = s.bank()
            s.mm(pp, cst[:, C_TRI:C_TRI + 128], amf)
            pt = s.bank()
            s.mm(pt, cst[:, C_ONE:C_ONE + 128], amf)
            s.memset('dve', carry_all[:, 0, :], 0.0)
            for tt in range(NT):
                s.tt('dve', carry_all[:, tt + 1, :], carry_all[:, tt, :], pt[:, tt * 32:(tt + 1) * 32], ALU.add)
            carry = carry_all[:, NT, :]
            s.tt('dve', pos.rearrange("p t e -> p (t e)"), pp, carry_all[:, 0:NT, :].rearrange("p t e -> p (t e)"), ALU.add)
            s.ts('dve', ovf, pos, float(CAP), 1e6, ALU.is_ge, ALU.mult)
            ecap = cst[:, C_ECAP:C_ECAP + 32]
            s.tt('dve', pos, pos, bass.AP(ecap.tensor, ecap.offset, [list(ecap.ap[0]), [0, NT], [1, 32]]), ALU.add)
            s.tt('dve', pos, pos, ovf, ALU.add)
            s.tt('dve', m1, m1, pos, ALU.mult)
            s.tt('dve', m2, m2, pos, ALU.mult)
            s.reduce('dve', slots_f[:, :, 0], m1, ALU.add)
            s.reduce('dve', slots_f[:, :, 1], m2, ALU.add)
            s.copy('dve', slots_i, slots_f)
            for tt in range(NT):
                hr = hrow[tt % 2]
                s.dma(hr, H2_scr[tt * 128:(tt + 1) * 128, :])
                for j in range(2):
                    s.idma(XG, hr, slots_i[:, tt, j:j + 1], True, NE * CAP)
            if 'cnt' in DBG:
                s.dma(DBG['cnt'][l], carry)
            A.off = mark
            NB = 2
            wg = [A.get([8, 512], BF16) for _ in range(NB)]
            wu = [A.get([8, 512], BF16) for _ in range(NB)]
            wd = [A.get([4, 1024], BF16) for _ in range(NB)]
            RT_ = CAP // 128
            xg = [A.get([RT_, D], BF16) for _ in range(2)]
            xT = [A.get([8, CAP], BF16) for _ in range(2)]
            hid = [A.get([4, CAP], BF16) for _ in range(2)]
            sg = [A.get([CAP], BF16) for _ in range(2)]
            yst = [A.get([D]) for _ in range(2)]
            y12 = [[A.get([D]) for _ in range(2)] for _ in range(2)]
            xts = [A.get([D]) for _ in range(2)]
            yi = 0
            for e in range(NE):
                b = e % NB
                s.dma(wg[b], W['exp_w_gate'][l, e].rearrange("(k p) n -> p k n", p=128), q='pool')
                s.dma(wu[b], W['exp_w_up'][l, e].rearrange("(k p) n -> p k n", p=128), q='pool')
                s.dma(wd[b], W['exp_w_down'][l, e].rearrange("(k p) n -> p k n", p=128), q='pool')
                xg_ = xg[e % 2]
                s.dma(xg_, XG[e * CAP:(e + 1) * CAP, :].rearrange("(r p) d -> p r d", p=128))
                xT_ = xT[e % 2]
                for r in range(RT_):
                    pb = s.bank().bitcast(BF16)
                    for k in range(8):
                        s.tr(pb[:, k * 128:(k + 1) * 128], xg_[:, r, k * 128:(k + 1) * 128], idb)
                    evac(xT_[:, :, r * 128:(r + 1) * 128], pb.rearrange("p (k t) -> p k t", k=8))
                hid_ = hid[e % 2]
                for fc in range(4):
                    fs = slice(fc * 128, (fc + 1) * 128)
                    pg = s.bank()
                    for k in range(8):
                        s.mm(pg[:, 0:CAP], wg[b][:, k, fs], xT_[:, k, :], start=(k == 0), stop=(k == 7))
                    pu = s.bank()
                    for k in range(8):
                        s.mm(pu[:, 0:CAP], wu[b][:, k, fs], xT_[:, k, :], start=(k == 0), stop=(k == 7))
                    sgb = sg[fc % 2]
                    s.act(sgb, pg[:, 0:CAP], AF.Silu)
                    s.tt('dve', hid_[:, fc, :], pu[:, 0:CAP], sgb, ALU.mult)
                for r in range(RT_):
                    ys = yst[yi % 2]
                    yi += 1
                    for half in range(2):
                        hs = slice(half * 512, (half + 1) * 512)
                        py = s.bank()
                        for k in range(4):
                            s.mm(py, hid_[:, k, r * 128:(r + 1) * 128], wd[b][:, k, hs], start=(k == 0), stop=(k == 3))
                        evac(ys[:, hs], py)
                    s.dma(YG[e * CAP + r * 128:e * CAP + (r + 1) * 128, :], ys)
            for tt in range(NT):
                ya, yb = y12[tt % 2]
                x_ = xts[tt % 2]
                s.dma(x_, out[tt * 128:(tt + 1) * 128, :])
                s.memset('pool', ya, 0.0)
                s.memset('pool', yb, 0.0)
                s.idma(ya, YG, slots_i[:, tt, 0:1], False, NE * CAP)
                s.idma(yb, YG, slots_i[:, tt, 1:2], False, NE * CAP)
                s.ts('dve', ya, ya, wts2[:, tt, 0:1], None, ALU.mult)
                s.stt('dve', ya, yb, wts2[:, tt, 1:2], ya, ALU.mult, ALU.add)
                s.tt('pool', ya, ya, gt2, ALU.mult)
                s.tt('dve', x_, x_, ya, ALU.add)
                s.dma(out[tt * 128:(tt + 1) * 128, :], x_)

        def _norm_with(A2, l, rw, lg_all, hTf):
            nonlocal A
            saved = A
            A = A2
            try:
                phase_norm(l, 1, out, router=(rw, lg_all, hTf))
            finally:
                A = saved

        order = ['mod', 'norm', 'proj', 'attn', 'rwkv', 'gla', 'merge', 'moe']
        stop_l, stop_p = (nlayers - 1, 'moe') if upto == 'all' else upto
        done = False
        for l in range(nlayers):
            for ph in order:
                if phases is not None and (l, ph) not in phases:
                    continue
                if ph == 'mod':
                    phase_mod(l)
                    dump('mod', modbc[0:1, :])
                elif ph == 'norm':
                    phase_norm(l, 0, x_in if l == 0 else out)
                elif ph == 'proj':
                    phase_proj(l)
                elif ph == 'attn':
                    phase_attn(l)
                elif ph == 'rwkv':
                    phase_rwkv(l)
                elif ph == 'gla':
                    phase_gla(l)
                elif ph == 'merge':
                    phase_merge(l, x_in if l == 0 else out)
                elif ph == 'moe':
                    phase_moe(l)
                if (l, ph) == (stop_l, stop_p):
                    done = True
                    break
            if done:
                break
        if 'P' in DBG:
            s.dma(DBG['P'], P_scr)
        if 'OT' in DBG:
            s.dma(DBG['OT'], OT_scr, q='pool')
        if 'hT' in DBG:
            hf = arena_t[:, 0:4096]
            for k in range(8):
                for g in range(0, S, 4096 // 1):
                    pass
        s.finish()
        print("instructions:", s.ninst, "sbuf left:", nc.sbuf_bytes_remaining, flush=True)
    return nc


def prep_weights(inp):
    f = lambda a: np.ascontiguousarray(np.asarray(a, dtype=np.float32))
    w = {}
    for k in ('ada_w', 'ada_b', 'norm1_g', 'norm2_g', 'w_in', 'attn_subln_g', 'rwkv_mu', 'rwkv_w_up', 'rwkv_w0',
              'rwkv_a_up', 'rwkv_a0', 'rwkv_g_up', 'rwkv_k_k', 'rwkv_k_a', 'rwkv_lnx_g', 'rwkv_lnx_b',
              'gla_alpha_up', 'gla_alpha_b', 'proj_attn', 'proj_rwkv', 'proj_gla', 'w_out', 'exp_w_gate',
              'exp_w_up', 'exp_w_down'):
        w[k] = f(inp[k])
    w['attn_qkg'] = f(np.concatenate([np.tile(inp['attn_qn_g'], (1, 8)), np.tile(inp['attn_kn_g'], (1, 8))], axis=1))
    w['attn_lambda'] = f(np.reshape(inp['attn_lambda'], (DEPTH, 256)))
    w['rwkv_r_k'] = f(np.reshape(inp['rwkv_r_k'], (DEPTH, 512)))
    w['gla_norm_g4'] = f(np.tile(inp['gla_norm_g'], (1, 4)))
    w['router_w'] = f(np.concatenate([inp['router_grp_w'], inp['router_exp_w']], axis=2))
    w['router_b'] = f(np.concatenate([inp['router_grp_b'], inp['router_exp_b']], axis=1))
    return w


def kernel(**inputs):
    x = np.asarray(inputs['x'], dtype=np.float32)
    c = np.asarray(inputs['c'], dtype=np.float32)
    w = prep_weights(inputs)
    cst, aug = make_consts()
    nc = build_program()
    in_maps = []
    for b in range(8):
        m = dict(w)
        m['x'] = np.ascontiguousarray(x[b])
        m['c'] = np.ascontiguousarray(c[b].reshape(8, 128).T)
        m['consts'] = cst
        m['aug'] = aug
        in_maps.append(m)
    res = run_bass_kernel_spmd(nc, in_maps, core_ids=list(range(8)))
    return np.stack([np.asarray(r['out'], dtype=np.float32) for r in res.results], axis=0)
```

```python
import contextlib
import math
import numpy as np
import concourse.bass as bass
import concourse.mybir as mybir
from concourse.bass_utils import run_bass_kernel_spmd

F32 = mybir.dt.float32
BF16 = mybir.dt.bfloat16
I32 = mybir.dt.int32
F32R = mybir.dt.float32r
AF = mybir.ActivationFunctionType
ALU = mybir.AluOpType
AX = mybir.AxisListType

D = 1024
S = 2048
NT = S // 128
DEPTH = 2
N_IN = 7952
NPROJ = 4880
EPS = 1e-6
SAME_ENGINE_SYNC = True
USE_F32R = True


def _region(ap):
    t = ap.tensor
    a = ap.ap
    off = int(ap.offset)
    es = mybir.dt.size(ap.dtype)
    tn = type(t).__name__
    if 'DRam' in tn:
        ext = sum(int(s) * (int(c) - 1) for s, c in a) + 1
        return (t.name, 0, 1, off * es, (off + ext) * es)
    if 'PSum' in tn:
        return (t.name, 0, 128, 0, 1 << 20)
    ps = int(a[0][0])
    npart = int(a[0][1])
    if ps == 0:
        ps = 1 << 40
    p0 = off // ps
    f0 = off % ps
    ext = sum(int(s) * (int(c) - 1) for s, c in a[1:]) + 1
    return (t.name, p0, p0 + npart, f0 * es, (f0 + ext) * es)


def _ovl(r, q):
    return r[1] < q[2] and q[1] < r[2] and r[3] < q[4] and q[3] < r[4]


def _contains(big, small):
    return big[1] <= small[1] and small[2] <= big[2] and big[3] <= small[3] and small[4] <= big[4]


class Sched:
    def __init__(self, nc, stack):
        self.nc = nc
        self.engs = {'pe': nc.tensor, 'act': nc.scalar, 'dve': nc.vector, 'pool': nc.gpsimd, 'sp': nc.sync}
        self.sem = {}
        self.cnt = {}
        for e in ['pe', 'act', 'dve', 'pool']:
            self.sem[e] = stack.enter_context(nc.semaphore('s_' + e))
            self.cnt[e] = 0
        self.ndsem = {'sp': 16, 'pool': 8}
        for q in ['sp', 'pool']:
            for j in range(self.ndsem[q]):
                k = ('d', q, j)
                self.sem[k] = stack.enter_context(nc.semaphore('d_%s_%d' % (q, j)))
                self.cnt[k] = 0
        self.drr = {'sp': 0, 'pool': 0}
        self.known = {e: {} for e in self.engs}
        self.recs = {}
        self.npsum = 0
        self.banks = []
        self.ninst = 0
        import os
        self.max_ops = int(os.environ.get('MAX_OPS', '1000000000'))
        self.force_dma = False
        self.log_ops = bool(os.environ.get('LOG_OPS'))

    def _deps(self, reads, writes):
        deps = {}
        for ap in reads:
            r = _region(ap)
            for rec in self.recs.get(r[0], ()):
                if rec[1] and _ovl(rec[0], r):
                    if deps.get(rec[2], 0) < rec[3]:
                        deps[rec[2]] = rec[3]
        for ap in writes:
            r = _region(ap)
            for rec in self.recs.get(r[0], ()):
                if _ovl(rec[0], r):
                    if deps.get(rec[2], 0) < rec[3]:
                        deps[rec[2]] = rec[3]
        return deps

    def _record(self, reads, writes, k, v, noprune=False):
        for ap in reads:
            r = _region(ap)
            lst = self.recs.setdefault(r[0], [])
            for rec in lst:
                if (not rec[1]) and rec[2] == k and rec[0] == r:
                    rec[3] = max(rec[3], v)
                    break
            else:
                lst.append([r, False, k, v])
        for ap in writes:
            r = _region(ap)
            lst = self.recs.setdefault(r[0], [])
            if not noprune:
                lst[:] = [rec for rec in lst if not _contains(r, rec[0])]
            lst.append([r, True, k, v])

    def _waits(self, e, deps):
        eng = self.engs[e]
        for k, v in deps.items():
            if self.known[e].get(k, 0) >= v:
                continue
            if k == e:
                if e == 'pe' or not SAME_ENGINE_SYNC or v > self.cnt[e]:
                    continue
            eng.wait_ge(self.sem[k], v)
            self.known[e][k] = v
            self.ninst += 1

    def op(self, e, fn, reads, writes, inc=True):
        if self.ninst >= self.max_ops:
            return None
        if self.log_ops:
            import inspect
            fr = inspect.stack()
            print('OP', self.ninst, e, [f.lineno for f in fr[1:4]], flush=True)
        pr = [r for r in reads if 'PSum' in type(r.tensor).__name__]
        if pr:
            reads = [r for r in reads if 'PSum' not in type(r.tensor).__name__]
            writes = list(writes) + pr
        self._waits(e, self._deps(reads, writes))
        ins = fn(self.engs[e])
        self.ninst += 1
        if inc:
            self.cnt[e] += 1
            ins.then_inc(self.sem[e], 1)
            val = self.cnt[e]
        else:
            val = self.cnt[e] + 1
        self._record(reads, writes, e, val)
        return ins

    def dma(self, out, in_, q='sp', **kw):
        if self.ninst >= self.max_ops and not self.force_dma:
            return
        if self.log_ops:
            import inspect
            fr = inspect.stack()
            print('DMA', self.ninst, q, [f.lineno for f in fr[1:3]], flush=True)
        self._waits(q, self._deps([in_], [out]))
        j = self.drr[q]
        self.drr[q] = (j + 1) % self.ndsem[q]
        k = ('d', q, j)
        if self.known[q].get(k, 0) < self.cnt[k]:
            self.engs[q].wait_ge(self.sem[k], self.cnt[k])
            self.known[q][k] = self.cnt[k]
            self.ninst += 1
        self.engs[q].dma_start(out=out, in_=in_, **kw).then_inc(self.sem[k], 16)
        self.ninst += 1
        self.cnt[k] += 16
        self._record([in_], [out], k, self.cnt[k])

    def idma(self, out, in_, idx, scatter, nrows):
        q = 'pool'
        deps = self._deps([in_, idx], [out])
        if scatter:
            deps = {k_: v_ for k_, v_ in deps.items() if not (isinstance(k_, tuple) and k_[1] == 'pool')}
        self._waits(q, deps)
        j = self.drr[q]
        self.drr[q] = (j + 1) % self.ndsem[q]
        k = ('d', q, j)
        if self.known[q].get(k, 0) < self.cnt[k]:
            self.engs[q].wait_ge(self.sem[k], self.cnt[k])
            self.known[q][k] = self.cnt[k]
        off = bass.IndirectOffsetOnAxis(ap=idx, axis=0)
        if not hasattr(self, 'bc_regs'):
            self.bc_regs = {}
        if nrows not in self.bc_regs:
            r = self.engs[q].alloc_register("bc%d" % nrows)
            self.engs[q].reg_mov(r, nrows - 1)
            self.bc_regs[nrows] = r
        bcr = self.bc_regs[nrows]
        if scatter:
            ins = self.engs[q].indirect_dma_start(out=out, out_offset=off, in_=in_, in_offset=None,
                                                  bounds_check=bcr, oob_is_err=False)
        else:
            ins = self.engs[q].indirect_dma_start(out=out, out_offset=None, in_=in_, in_offset=off,
                                                  bounds_check=bcr, oob_is_err=False)
        ins.then_inc(self.sem[k], 16)
        self.ninst += 1
        self.cnt[k] += 16
        self._record([in_, idx], [out], k, self.cnt[k], noprune=scatter)

    def barrier(self):
        for e in ('pe', 'act', 'dve', 'pool', 'sp'):
            for k, v in self.cnt.items():
                if v > 0 and self.known[e].get(k, 0) < v:
                    self.engs[e].wait_ge(self.sem[k], v)
                    self.known[e][k] = v
                    self.ninst += 1

    def finish(self):
        sp = self.engs['sp']
        for k, v in self.cnt.items():
            if v > 0:
                sp.wait_ge(self.sem[k], v)

    def mm(self, out, lhsT, rhs, start=True, stop=True):
        return self.op('pe', lambda e: e.matmul(out, lhsT, rhs, start=start, stop=stop),
                       [lhsT, rhs], [out], inc=stop)

    def mmr(self, out, lhsT, rhs, start=True, stop=True):
        if not USE_F32R:
            return self.mm(out, lhsT, rhs, start, stop)
        return self.mm(out, lhsT, rhs, start, stop)

    def tr(self, out, in_, ident):
        return self.op('pe', lambda e: e.transpose(out, in_, ident), [in_, ident], [out])

    def act(self, out, in_, func, bias=None, scale=None, accum_out=None, eng='act'):
        kw = {}
        reads = [in_]
        writes = [out]
        if bias is not None:
            kw['bias'] = bias
            if not isinstance(bias, (int, float)):
                reads.append(bias)
        if scale is not None:
            kw['scale'] = scale
            if not isinstance(scale, (int, float)):
                reads.append(scale)
        if accum_out is not None:
            kw['accum_out'] = accum_out
            writes.append(accum_out)
        return self.op('act', lambda e: e.activation(out, in_, func, **kw), reads, writes)

    def copy(self, e, out, in_):
        if e == 'act':
            if out.dtype == F32R:
                return self.op('act', lambda g: g.activation(out, in_, AF.Copy), [in_], [out])
            return self.op('act', lambda g: g.copy(out, in_), [in_], [out])
        return self.op(e, lambda g: g.tensor_copy(out, in_), [in_], [out])

    def tt(self, e, out, in0, in1, op):
        return self.op(e, lambda g: g.tensor_tensor(out, in0, in1, op), [in0, in1], [out])

    def ts(self, e, out, in0, s1, s2, op0, op1=None, accum_out=None):
        reads = [in0] + [s for s in (s1, s2) if s is not None and not isinstance(s, (int, float))]
        writes = [out] + ([accum_out] if accum_out is not None else [])
        kw = {}
        if accum_out is not None:
            kw['accum_out'] = accum_out
        if op1 is None:
            return self.op(e, lambda g: g.tensor_scalar(out, in0, s1, s2, op0, **kw), reads, writes)
        return self.op(e, lambda g: g.tensor_scalar(out, in0, s1, s2, op0, op1, **kw), reads, writes)

    def stt(self, e, out, in0, scalar, in1, op0, op1):
        reads = [in0, in1] + ([scalar] if not isinstance(scalar, (int, float)) else [])
        return self.op(e, lambda g: g.scalar_tensor_tensor(out, in0, scalar, in1, op0, op1), reads, [out])

    def memset(self, e, ap, val):
        return self.op(e, lambda g: g.memset(ap, val), [], [ap])

    def reduce(self, e, out, in_, op, axis=None):
        axis = AX.X if axis is None else axis
        return self.op(e, lambda g: g.tensor_reduce(out, in_, axis, op), [in_], [out])

    def bank(self):
        b = self.banks[self.npsum % len(self.banks)]
        self.npsum += 1
        return b


def bc_last(ap, n):
    return bass.AP(ap.tensor, ap.offset, [list(p) for p in ap.ap] + [[0, n]])


def bc_part(ap, n=128):
    dims = [list(p) for p in ap.ap]
    if len(dims) == 2 and dims[0][1] == 1:
        dims = dims[1:]
    return bass.AP(ap.tensor, ap.offset, [[0, n]] + dims)


C_ID, C_ML, C_MSL, C_MSLT, C_MB, C_CI = 0, 128, 256, 384, 512, 640
C_CM = 704
C_TRI, C_ONE, C_ECAP = 960, 1088, 1216
NCONST = 1248
CAP = 512
NE = 32


def make_consts():
    c = np.zeros((128, NCONST), np.float32)
    i = np.arange(128)
    same = (i[:, None] // 64) == (i[None, :] // 64)
    c[:, C_ID:C_ID + 128] = np.eye(128)
    c[:, C_ML:C_ML + 128] = (same & (i[:, None] <= i[None, :]))
    c[:, C_MSL:C_MSL + 128] = (same & (i[:, None] < i[None, :]))
    c[:, C_MSLT:C_MSLT + 128] = (same & (i[:, None] > i[None, :]))
    c[:, C_MB:C_MB + 128] = np.where(i[:, None] > i[None, :], -30000.0, 0.0)
    c[:, C_CI] = (i < 64)
    c[:, C_CI + 1] = (i >= 64)
    c[:, C_CM:C_CM + 64] = 1.0
    c[:, C_CM + 128 + 64:C_CM + 256] = 1.0
    c[:, C_TRI:C_TRI + 128] = (i[:, None] < i[None, :])
    c[:, C_ONE:C_ONE + 128] = 1.0
    c[:, C_ECAP:C_ECAP + 32] = np.arange(32)[None, :] * CAP
    aug = np.zeros((4, 2, 4, S), np.float32)
    t = np.arange(S)
    for h in range(4):
        sl = 2.0 ** (-8.0 * (h + 1) / 4)
        aug[0, 0, h] = 1.0
        aug[1, 0, h] = -sl * (t % 128)
        aug[2, 0, h] = 1.0
        aug[3, 0, h] = -sl * 128 * (t // 128)
        aug[0, 1, h] = sl * (t % 128)
        aug[1, 1, h] = 1.0
        aug[2, 1, h] = sl * 128 * (t // 128)
        aug[3, 1, h] = 1.0
    return c, aug


WSPECS = [
    ('ada_w', [DEPTH, D, 6 * D]), ('ada_b', [DEPTH, 6 * D]), ('norm1_g', [DEPTH, D]), ('norm2_g', [DEPTH, D]),
    ('w_in', [DEPTH, D, N_IN]), ('attn_qkg', [DEPTH, 1024]), ('attn_lambda', [DEPTH, 256]),
    ('attn_subln_g', [DEPTH, 128]), ('rwkv_mu', [DEPTH, 1792]), ('rwkv_w_up', [DEPTH, 64, 512]),
    ('rwkv_w0', [DEPTH, 512]), ('rwkv_a_up', [DEPTH, 64, 512]), ('rwkv_a0', [DEPTH, 512]),
    ('rwkv_g_up', [DEPTH, 128, 512]), ('rwkv_k_k', [DEPTH, 512]), ('rwkv_k_a', [DEPTH, 512]),
    ('rwkv_r_k', [DEPTH, 512]), ('rwkv_lnx_g', [DEPTH, 512]), ('rwkv_lnx_b', [DEPTH, 512]),
    ('gla_alpha_up', [DEPTH, 16, 256]), ('gla_alpha_b', [DEPTH, 256]), ('gla_norm_g4', [DEPTH, 512]),
    ('proj_attn', [DEPTH, 512, D]), ('proj_rwkv', [DEPTH, 512, D]), ('proj_gla', [DEPTH, 512, D]),
    ('w_out', [DEPTH, D, D]), ('router_w', [DEPTH, D, 36]), ('router_b', [DEPTH, 36]),
    ('exp_w_gate', [DEPTH, 32, D, 512]), ('exp_w_up', [DEPTH, 32, D, 512]), ('exp_w_down', [DEPTH, 32, 512, D]),
]


class Arena:
    def __init__(self, ap, nbytes, native=F32):
        self.ap = ap
        self.nbytes = nbytes
        self.off = 0
        self.native = native

    def reset(self):
        self.off = 0

    def get(self, shape, dt=F32, parts=128):
        es = mybir.dt.size(dt)
        n = 1
        for v in shape:
            n *= v
        nb = (n * es + 31) // 32 * 32
        assert self.off + nb <= self.nbytes, ("arena overflow", self.off, nb)
        a = self.ap[:, self.off // 4:(self.off + nb) // 4]
        self.off += nb
        if dt != self.native:
            a = a.bitcast(dt)
        a = a[0:parts, 0:n]
        if len(shape) == 2:
            a = a.rearrange("p (a b) -> p a b", a=shape[0])
        elif len(shape) == 3:
            a = a.rearrange("p (a b c) -> p a b c", a=shape[0], b=shape[1])
        return a


def build_program(nlayers=DEPTH, upto='all', dbg=None, phases=None, scr_io=False):
    dbg = dbg or {}
    nc = bass.Bass("TRN2", target_bir_lowering=False)
    dram = {}
    x_in = nc.dram_tensor("x", [S, D], F32, kind="ExternalInput").ap()
    c_in = nc.dram_tensor("c", [128, 8], F32, kind="ExternalInput").ap()
    cst_in = nc.dram_tensor("consts", [128, NCONST], F32, kind="ExternalInput").ap()
    aug_in = nc.dram_tensor("aug", [4, 2, 4, S], F32, kind="ExternalInput").ap()
    W = {}
    for name, shp in WSPECS:
        W[name] = nc.dram_tensor(name, shp, F32, kind="ExternalInput").ap()
    out = nc.dram_tensor("out", [S, D], F32, kind="ExternalOutput").ap()
    P_scr = nc.dram_tensor("P_scr", [S, NPROJ], F32, kind="ExternalInput" if scr_io else "Internal").ap()
    OT_scr = nc.dram_tensor("OT_scr", [3, 512, S], BF16, kind="ExternalOutput" if scr_io else "Internal").ap()
    H2_scr = nc.dram_tensor("H2_scr", [S, D], BF16, kind="Internal").ap()
    XG = nc.dram_tensor("XG", [NE * CAP, D], BF16, kind="Internal").ap()
    YG = nc.dram_tensor("YG", [NE * CAP, D], F32, kind="Internal").ap()
    DBG = {}
    for name, shp in dbg.items():
        DBG[name] = nc.dram_tensor("dbg_" + name, list(shp), F32, kind="ExternalOutput").ap()

    with contextlib.ExitStack() as stack:
        s = Sched(nc, stack)
        banks = [nc.alloc_psum_tensor("pb%d" % i, [128, 512], F32).ap() for i in range(8)]
        s.banks = banks
        hT = nc.alloc_sbuf_tensor("hT", [128, 8, S], BF16).ap()
        modbc = nc.alloc_sbuf_tensor("modbc", [128, 6 * D], F32).ap()
        cst = nc.alloc_sbuf_tensor("cst", [128, NCONST], F32).ap()
        idb = nc.alloc_sbuf_tensor("idb", [128, 128], BF16).ap()
        mbb = nc.alloc_sbuf_tensor("mbb", [128, 128], BF16).ap()
        ones = nc.alloc_sbuf_tensor("ones", [1, 128], F32).ap()
        rows = nc.alloc_sbuf_tensor("rows", [1, 1408], F32).ap()
        csb = nc.alloc_sbuf_tensor("csb", [128, 8], F32).ap()
        ARENA_BYTES = (int(nc.sbuf_bytes_remaining) - 2048) // 1024 * 1024
        print('arena bytes', ARENA_BYTES, flush=True)
        arena_t = nc.alloc_sbuf_tensor("arena", [128, ARENA_BYTES // 4], F32).ap()
        A = Arena(arena_t, ARENA_BYTES)
        FR_BYTES = 52 * 1024
        arena_addr = int(nc.lookup_mloc("arena").addr)
        fr_t = nc.alloc_sbuf_tensor_at("fr_pool", [128, FR_BYTES // 4], F32R, offset=arena_addr + ARENA_BYTES - FR_BYTES).ap()
        FR = Arena(fr_t, FR_BYTES, native=F32R)
        idf = cst[:, C_ID:C_ID + 128]
        m_l = cst[:, C_ML:C_ML + 128]
        m_sl = cst[:, C_MSL:C_MSL + 128]
        m_slT = cst[:, C_MSLT:C_MSLT + 128]
        cind = cst[:, C_CI:C_CI + 2]

        rr = [0]

        def alt():
            rr[0] += 1
            return 'act' if rr[0] % 2 else 'dve'

        def evac(out_, in_):
            return s.copy(alt(), out_, in_)

        def dump(name, ap_sb, dst=None):
            if name in DBG:
                s.dma(DBG[name] if dst is None else dst, ap_sb)

        s.dma(cst, cst_in)
        s.dma(csb, c_in)
        s.memset('dve', ones, 1.0)
        s.copy('dve', idb, idf)
        s.copy('dve', mbb, cst[:, C_MB:C_MB + 128])
        s.act(csb, csb, AF.Silu)

        def rstd_from_ssq(dst, ssq, scale, eps, tmp):
            s.ts('dve', tmp, ssq, scale, eps, ALU.mult, ALU.add)
            s.act(tmp, tmp, AF.Sqrt)
            s.op('dve', lambda g: g.reciprocal(dst, tmp), [tmp], [dst])

        def phase_mod(l):
            A.reset()
            adaw = [A.get([8, 512]) for _ in range(2)]
            adab = [A.get([512]) for _ in range(2)]
            cbc = A.get([8, 128])
            for k in range(8):
                s.copy('dve', cbc[:, k:k + 1, :], bc_last(csb[:, k:k + 1], 128))
            for cg in range(12):
                buf = adaw[cg % 2]
                bb = adab[cg % 2]
                s.dma(buf, W['ada_w'][l, :, cg * 512:(cg + 1) * 512].rearrange("(k p) n -> p k n", p=128))
                s.dma(bb, bc_part(W['ada_b'][l:l + 1, cg * 512:(cg + 1) * 512]))
                pb = s.bank()
                for k in range(8):
                    s.mm(pb, cbc[:, k, :], buf[:, k, :], start=(k == 0), stop=(k == 7))
                s.tt('dve', modbc[:, cg * 512:(cg + 1) * 512], pb, bb, ALU.add)

        def phase_norm(l, which, src, router=None):
            A.reset()
            sh = modbc[:, (3 * which) * D:(3 * which + 1) * D]
            sc = modbc[:, (3 * which + 1) * D:(3 * which + 2) * D]
            G1 = A.get([D])
            gb = A.get([D])
            ssq = A.get([NT])
            rstd = A.get([NT])
            tmpn = A.get([NT])
            junk = A.get([D])
            xts = [A.get([D]) for _ in range(3)]
            tmps = [A.get([D]) for _ in range(2)]
            hbs = [A.get([D], BF16 if router is None else F32) for _ in range(2)]
            s.dma(gb, bc_part(W['norm1_g' if which == 0 else 'norm2_g'][l:l + 1, :]))
            s.stt('dve', G1, sc, 1.0, gb, ALU.add, ALU.mult)
            s.memset('dve', ssq, 0.0)
            if router is not None:
                rw, lg_all, hTf = router
                hbbs = [A.get([D], BF16) for _ in range(2)]
            for tt in range(NT):
                xt = xts[tt % 3]
                s.dma(xt, src[tt * 128:(tt + 1) * 128, :])
                s.act(junk, xt, AF.Square, accum_out=ssq[:, tt:tt + 1])
                rstd_from_ssq(rstd[:, tt:tt + 1], ssq[:, tt:tt + 1], 1.0 / D, EPS, tmpn[:, tt:tt + 1])
                tmp = tmps[tt % 2]
                hb = hbs[tt % 2]
                s.stt('dve', tmp, xt, rstd[:, tt:tt + 1], G1, ALU.mult, ALU.mult)
                s.tt('pool', hb, tmp, sh, ALU.add)
                if router is None:
                    pb = s.bank().bitcast(BF16)
                    for k in range(8):
                        s.tr(pb[:, k * 128:(k + 1) * 128], hb[:, k * 128:(k + 1) * 128], idb)
                    evac(hT[:, :, tt * 128:(tt + 1) * 128], pb.rearrange("p (k t) -> p k t", k=8))
                else:
                    hf = hTf[tt % 2]
                    for half in range(2):
                        pb = s.bank()
                        for k in range(4):
                            kk = half * 4 + k
                            s.tr(pb[:, k * 128:(k + 1) * 128], hb[:, kk * 128:(kk + 1) * 128], idf)
                        s.copy('act', hT[:, half * 4:half * 4 + 4, tt * 128:(tt + 1) * 128],
                               pb.rearrange("p (k t) -> p k t", k=4))
                        s.copy('dve', hf[:, half * 4:half * 4 + 4, :], pb.rearrange("p (k t) -> p k t", k=4))
                    pl = s.bank()
                    for k in range(8):
                        s.mm(pl[:, 0:64], hf[:, k, :], rw[:, k, :], start=(k == 0), stop=False)
                    s.mm(pl[:, 0:64], ones[0:1, 0:128], rows[0:1, 1280:1344], start=False, stop=True)
                    s.copy('dve', lg_all[:, tt, :], pl[:, 0:36])
                    hbb = hbbs[tt % 2]
                    s.copy('act', hbb, hb)
                    s.dma(H2_scr[tt * 128:(tt + 1) * 128, :], hbb)

        def phase_proj(l):
            A.reset()
            wb = [A.get([8, 512], BF16) for _ in range(2)]
            stg = [A.get([512]) for _ in range(4)]
            nchunk = (NPROJ + 511) // 512
            si = 0
            for j in range(nchunk):
                c0 = j * 512
                ncol = min(512, NPROJ - c0)
                w = wb[j % 2]
                s.dma(w[:, :, 0:ncol], W['w_in'][l, :, c0:c0 + ncol].rearrange("(k p) n -> p k n", p=128), q='pool')
                for tt in range(NT):
                    pb = s.bank()
                    for k in range(8):
                        s.mm(pb[:, 0:ncol], hT[:, k, tt * 128:(tt + 1) * 128], w[:, k, 0:ncol],
                             start=(k == 0), stop=(k == 7))
                    st = stg[si % 4]
                    si += 1
                    evac(st[:, 0:ncol], pb[:, 0:ncol])
                    s.dma(P_scr[tt * 128:(tt + 1) * 128, c0:c0 + ncol], st[:, 0:ncol])

        def phase_attn(l):
            lambda_init = 0.8 - 0.6 * math.exp(-0.3 * l)
            A.reset()
            qkT = A.get([16, S], BF16, parts=68)
            vaug = A.get([NT, 4 * 129], BF16)
            qkg = A.get([1024])
            sub_g = A.get([128])
            lamb = A.get([256])
            small = A.get([64])
            qk = [A.get([1024]) for _ in range(2)]
            vt = [A.get([512]) for _ in range(2)]
            sq = A.get([1024])
            qn = [A.get([1024], BF16) for _ in range(2)]
            PT = [A.get([512], BF16) for _ in range(3)]
            t0 = [A.get([128]) for _ in range(2)]
            ot = [A.get([128]) for _ in range(2)]
            oa = [A.get([512], BF16) for _ in range(2)]
            oT = [A.get([4, 128], BF16) for _ in range(2)]
            s.dma(qkg, bc_part(W['attn_qkg'][l:l + 1, :]))
            s.dma(sub_g, bc_part(W['attn_subln_g'][l:l + 1, :]))
            s.dma(lamb, bc_part(W['attn_lambda'][l:l + 1, :]))
            lp = small[:, 0:2]
            s.tt('dve', sq[:, 0:64], lamb[:, 0:64], lamb[:, 64:128], ALU.mult)
            s.tt('dve', sq[:, 64:128], lamb[:, 128:192], lamb[:, 192:256], ALU.mult)
            s.reduce('dve', lp, sq[:, 0:128].rearrange("p (a b) -> p a b", a=2), ALU.add)
            s.act(lp, lp, AF.Exp)
            nlam = small[:, 2:3]
            s.tt('dve', nlam, lp[:, 1:2], lp[:, 0:1], ALU.subtract)
            s.ts('dve', nlam, nlam, -lambda_init, None, ALU.add)
            for g in range(16):
                isk = g // 8
                h = (g % 8) // 2
                s.dma(qkT[64:68, g, :], aug_in[:, isk, h, :], q='pool')
            s.memset('dve', vaug, 1.0)
            ssq = small[:, 8:24]
            rs = small[:, 24:40]
            tm = small[:, 40:56]
            for tt in range(NT):
                q_ = qk[tt % 2]
                v_ = vt[tt % 2]
                s.dma(q_, P_scr[tt * 128:(tt + 1) * 128, 0:1024])
                s.dma(v_, P_scr[tt * 128:(tt + 1) * 128, 1024:1536])
                s.tt('pool', sq, q_, q_, ALU.mult)
                s.reduce('dve', ssq, sq.rearrange("p (a b) -> p a b", a=16), ALU.add)
                rstd_from_ssq(rs[:, 0:8], ssq[:, 0:8], 1.0, 64 * EPS, tm[:, 0:8])
                rstd_from_ssq(rs[:, 8:16], ssq[:, 8:16], 1.0 / 64, EPS, tm[:, 8:16])
                s.tt('dve', sq.rearrange("p (a b) -> p a b", a=16), q_.rearrange("p (a b) -> p a b", a=16),
                     bc_last(rs, 64), ALU.mult)
                qb_ = qn[tt % 2]
                s.tt('pool', qb_, sq, qkg, ALU.mult)
                for half in range(2):
                    pb = s.bank().bitcast(BF16)
                    for j in range(8):
                        g = half * 8 + j
                        s.tr(pb[0:64, j * 128:(j + 1) * 128], qb_[:, g * 64:(g + 1) * 64], idb)
                    evac(qkT[0:64, half * 8:half * 8 + 8, tt * 128:(tt + 1) * 128],
                         pb[0:64, :].rearrange("p (g t) -> p g t", g=8))
                s.copy('act', vaug[:, tt, :].rearrange("p (h d) -> p h d", h=4)[:, :, 0:128],
                       v_.rearrange("p (h d) -> p h d", h=4))
            import os
            stage = int(os.environ.get('ATT_STAGE', '9'))
            if stage < 2:
                return
            SB = banks[0:4]
            ACC = banks[4:8]
            sbi = 0
            pti = 0
            for qb in range(NT):
                for h in range(4):
                    accs = [ACC[(2 * (qb * 4 + h)) % 4], ACC[(2 * (qb * 4 + h) + 1) % 4]]
                    for c in range(2):
                        gq = h * 2 + c
                        gk = 8 + h * 2 + c
                        acc = accs[c]
                        for jg in range(qb // 4 + 1):
                            jbs = list(range(jg * 4, min(jg * 4 + 4, qb + 1)))
                            sbk = SB[sbi % 4]
                            sbi += 1
                            for jj, jb in enumerate(jbs):
                                diag = (jb == qb)
                                s.mm(sbk[:, jj * 128:(jj + 1) * 128], qkT[0:68, gk, jb * 128:(jb + 1) * 128],
                                     qkT[0:68, gq, qb * 128:(qb + 1) * 128], start=True, stop=not diag)
                                if diag:
                                    s.mm(sbk[:, jj * 128:(jj + 1) * 128], idb, mbb, start=False, stop=True)
                            pt = PT[pti % 3]
                            pti += 1
                            n = len(jbs) * 128
                            s.act(pt[:, 0:n], sbk[:, 0:n], AF.Exp)
                            for jj, jb in enumerate(jbs):
                                s.mm(acc[:, 0:129], pt[:, jj * 128:(jj + 1) * 128],
                                     vaug[:, jb, h * 129:(h + 1) * 129], start=(jb == 0), stop=(jb == qb))
                    if stage < 3:
                        continue
                    i2 = (qb * 4 + h) % 2
                    rd = small[:, 56 + 4 * i2:56 + 4 * i2 + 4]
                    s.op('dve', lambda g, a=rd[:, 0:1], b=accs[0][:, 128:129]: g.reciprocal(a, b),
                         [accs[0][:, 128:129]], [rd[:, 0:1]])
                    s.op('dve', lambda g, a=rd[:, 1:2], b=accs[1][:, 128:129]: g.reciprocal(a, b),
                         [accs[1][:, 128:129]], [rd[:, 1:2]])
                    s.tt('dve', rd[:, 1:2], rd[:, 1:2], nlam, ALU.mult)
                    s.ts('dve', t0[i2], accs[0][:, 0:128], rd[:, 0:1], None, ALU.mult)
                    s.stt('dve', ot[i2], accs[1][:, 0:128], rd[:, 1:2], t0[i2], ALU.mult, ALU.add)
                    s.memset('dve', rd[:, 2:3], 0.0)
                    s.act(t0[i2], ot[i2], AF.Square, accum_out=rd[:, 2:3])
                    rstd_from_ssq(rd[:, 2:3], rd[:, 2:3], 1.0 / 128, EPS, rd[:, 3:4])
                    s.ts('dve', rd[:, 2:3], rd[:, 2:3], 1.0 - lambda_init, None, ALU.mult)
                    oab = oa[qb % 2]
                    s.stt('dve', oab[:, h * 128:(h + 1) * 128], ot[i2], rd[:, 2:3], sub_g, ALU.mult, ALU.mult)
                if stage < 4:
                    continue
                oab = oa[qb % 2]
                pb = banks[(qb % 2)].bitcast(BF16)
                for k in range(4):
                    s.tr(pb[:, k * 128:(k + 1) * 128], oab[:, k * 128:(k + 1) * 128], idb)
                o_t = oT[qb % 2]
                evac(o_t, pb[:, 0:512].rearrange("p (k t) -> p k t", k=4))
                s.dma(OT_scr[0, :, qb * 128:(qb + 1) * 128].rearrange("(k p) t -> p k t", p=128), o_t)

        def chunk_core(H, Vd, Rb, Kb, Ab, Bb, Vt, epos, Hst, Y, wk, lowrank, KbM, BbM):
            G = wk['G']
            nq = 4 if lowrank else 2
            m_l2 = bass.AP(m_l.tensor, m_l.offset, [list(m_l.ap[0]), [0, 2], [1, 128]])
            m_sl2 = bass.AP(m_sl.tensor, m_sl.offset, [list(m_sl.ap[0]), [0, 2], [1, 128]])
            for g0 in range(0, H, G):
                hs = list(range(g0, min(g0 + G, H)))
                SL = {h: wk['slots'][j] for j, h in enumerate(hs)}
                ksl = lambda h: slice(h * 64, (h + 1) * 64)
                vsl = lambda h: slice(h * Vd, (h + 1) * Vd)
                pbs = {}
                for h in hs:
                    pb = s.bank()
                    pbs[h] = pb
                    for qi, src in enumerate([Rb, Kb, Ab, Bb][:nq]):
                        s.tr(pb[0:64, qi * 128:(qi + 1) * 128], src[:, ksl(h)], idf)
                for h in hs:
                    evac(SL[h]['FT'][:, 0:nq, :], pbs[h][0:64, 0:nq * 128].rearrange("p (q t) -> p q t", q=nq))
                for h in hs:
                    pb = s.bank()
                    pbs[h] = pb
                    s.tr(pb[0:64, 0:128], epos[:, ksl(h)], idf)
                for h in hs:
                    evac(SL[h]['ET'], pbs[h][0:64, 0:128])
                for h in hs:
                    FT = SL[h]['FT']
                    RT, KT = FT[:, 0, :], FT[:, 1, :]
                    pm = s.bank()
                    pbs[h] = pm
                    s.mmr(pm[:, 0:128], KT, RT)
                    if lowrank:
                        AT, BT = FT[:, 2, :], FT[:, 3, :]
                        s.mmr(pm[:, 128:256], BT, RT)
                        s.mmr(pm[:, 256:384], KT, AT)
                        s.mmr(pm[:, 384:512], BT, AT)
                for h in hs:
                    M1 = SL[h]['M1']
                    pm = pbs[h]
                    if lowrank:
                        s.tt('dve', M1[:, 0:2, :], pm[:, 0:256].rearrange("p (a b) -> p a b", a=2), m_l2, ALU.mult)
                        s.tt('dve', M1[:, 2:4, :], pm[:, 256:512].rearrange("p (a b) -> p a b", a=2), m_sl2, ALU.mult)
                    else:
                        s.tt('dve', M1[:, 0, :], pm[:, 0:128], m_l, ALU.mult)
                if lowrank:
                    cur = {}
                    for h in hs:
                        FT = SL[h]['FT']
                        pa = s.bank()
                        pbs[h] = pa
                        s.mmr(pa[:, 0:128], FT[:, 2, :], FT[:, 3, :])
                    for h in hs:
                        sl = SL[h]
                        s.tt('dve', sl['P'][0], pbs[h][:, 0:128], m_slT, ALU.mult)
                        s.tt('dve', sl['W'][0], sl['M1'][:, 3, :], idf, ALU.add)
                        cur[h] = [sl['P'][0], sl['M1'][:, 3, :], sl['W'][0]]
                    for i in range(1, 6):
                        for h in hs:
                            Pc, Qc, Wc = cur[h]
                            pp = s.bank()
                            pbs[h] = pp
                            s.mmr(pp[:, 0:128], Qc, Pc)
                            if i < 5:
                                s.mmr(pp[:, 128:256], Pc, Qc)
                        for h in hs:
                            sl = SL[h]
                            Pn = sl['P'][i % 2]
                            s.copy('act', Pn, pbs[h][:, 0:128])
                            if i < 5:
                                Qn = sl['Q'][i % 2]
                                s.copy('dve', Qn, pbs[h][:, 128:256])
                                cur[h][1] = Qn
                            cur[h][0] = Pn
                        for h in hs:
                            pw = s.bank()
                            pbs[h] = pw
                            s.mmr(pw[:, 0:128], cur[h][0], cur[h][2])
                        for h in hs:
                            Wn = SL[h]['W'][i % 2]
                            s.tt('dve', Wn, pbs[h][:, 0:128], cur[h][2], ALU.add)
                            cur[h][2] = Wn
                    for h in hs:
                        pz = s.bank()
                        pbs[h] = pz
                        s.mmr(pz[:, 0:Vd], SL[h]['M1'][:, 2, :], Vt[:, vsl(h)])
                    for h in hs:
                        ZA = SL[h]['ZA']
                        s.copy('act', ZA[:, 0:Vd], pbs[h][:, 0:Vd])
                        s.copy('dve', ZA[:, Vd:Vd + 64], Ab[:, ksl(h)])
                    for h in hs:
                        pu = s.bank()
                        pbs[h] = pu
                        s.mmr(pu[:, 0:Vd + 64], cur[h][2], SL[h]['ZA'][:, 0:Vd + 64])
                    for h in hs:
                        s.copy('act', SL[h]['UA'][:, 0:Vd + 64], pbs[h][:, 0:Vd + 64])
                    for h in hs:
                        pr = s.bank()
                        pbs[h] = pr
                        s.mmr(pr[0:64, 0:128], SL[h]['UA'][:, Vd:Vd + 64], SL[h]['M1'][:, 1, :])
                    for h in hs:
                        s.tt('dve', SL[h]['RtT'], pbs[h][0:64, 0:128], SL[h]['FT'][:, 0, :], ALU.add)
                RtTs = {h: (SL[h]['RtT'] if lowrank else SL[h]['FT'][:, 0, :]) for h in hs}
                for c in range(2):
                    for h in hs:
                        s.copy('dve', SL[h]['Hc'][:, c, :], Hst[:, h, :])
                    if lowrank:
                        for h in hs:
                            pn = s.bank()
                            pbs[h] = pn
                            s.mmr(pn[0:64, 0:64], SL[h]['UA'][:, Vd:Vd + 64], BbM[c][:, ksl(h)])
                            s.mmr(pn[0:64, 64:64 + Vd], BbM[c][:, ksl(h)], SL[h]['UA'][:, 0:Vd], start=True, stop=False)
                            s.mmr(pn[0:64, 64:64 + Vd], KbM[c][:, ksl(h)], Vt[:, vsl(h)], start=False, stop=True)
                        for h in hs:
                            s.tt('dve', SL[h]['BTI'], pbs[h][0:64, 0:64], idf[0:64, 0:64], ALU.add)
                        for h in hs:
                            s.mmr(pbs[h][0:64, 256:256 + Vd], SL[h]['BTI'], SL[h]['Hc'][:, c, :], start=True, stop=True)
                        for h in hs:
                            gsc = SL[h]['ET'][:, c * 64 + 63:c * 64 + 64]
                            s.ts('dve', SL[h]['tmpH'], pbs[h][0:64, 256:256 + Vd], gsc, None, ALU.mult)
                            s.stt('dve', Hst[:, h, :], pbs[h][0:64, 64:64 + Vd], gsc, SL[h]['tmpH'], ALU.mult, ALU.add)
                    else:
                        for h in hs:
                            pn = s.bank()
                            pbs[h] = pn
                            s.mmr(pn[0:64, 0:Vd], KbM[c][:, ksl(h)], Vt[:, vsl(h)], start=True, stop=True)
                        for h in hs:
                            gsc = SL[h]['ET'][:, c * 64 + 63:c * 64 + 64]
                            s.ts('dve', SL[h]['tmpH'], SL[h]['Hc'][:, c, :], gsc, None, ALU.mult)
                            s.stt('dve', Hst[:, h, :], pbs[h][0:64, 0:Vd], gsc, SL[h]['tmpH'], ALU.mult, ALU.add)
                for h in hs:
                    for c in range(2):
                        s.tt('dve', SL[h]['RtM'][:, c, :], RtTs[h], cst[0:64, C_CM + c * 128:C_CM + (c + 1) * 128], ALU.mult)
                pys = {}
                for h in hs:
                    py = s.bank()
                    pys[h] = py
                    if lowrank:
                        s.mmr(py[:, 0:Vd], SL[h]['M1'][:, 1, :], SL[h]['UA'][:, 0:Vd], start=True, stop=False)
                        s.mmr(py[:, 0:Vd], SL[h]['M1'][:, 0, :], Vt[:, vsl(h)], start=False, stop=True)
                    else:
                        s.mmr(py[:, 0:Vd], SL[h]['M1'][:, 0, :], Vt[:, vsl(h)], start=True, stop=True)
                for h in hs:
                    s.copy('act', Y[:, vsl(h)], pys[h][:, 0:Vd])
                for h in hs:
                    pyh = s.bank()
                    pys[h] = pyh
                    for c in range(2):
                        s.mmr(pyh[:, 0:Vd], SL[h]['RtM'][:, c, :], SL[h]['Hc'][:, c, :], start=(c == 0), stop=(c == 1))
                for h in hs:
                    s.tt('dve', Y[:, vsl(h)], pys[h][:, 0:Vd], Y[:, vsl(h)], ALU.add)

        def alloc_wk(Vd, lowrank, G=4):
            wk = {'G': G, 'slots': []}
            for j in range(G):
                sl = {}
                MT = F32R if USE_F32R else F32
                sl['FT'] = FR.get([4 if lowrank else 2, 128], MT, parts=64)
                sl['ET'] = A.get([128], F32, parts=64)
                sl['M1'] = FR.get([4 if lowrank else 1, 128], MT)
                sl['Hc'] = FR.get([2, Vd], MT, parts=64)
                sl['tmpH'] = A.get([Vd], F32, parts=64)
                sl['RtM'] = FR.get([2, 128], MT, parts=64)
                if lowrank:
                    sl['P'] = [FR.get([128], MT) for _ in range(2)]
                    sl['Q'] = [FR.get([128], MT) for _ in range(2)]
                    sl['W'] = [FR.get([128], MT) for _ in range(2)]
                    sl['ZA'] = FR.get([Vd + 64], MT)
                    sl['UA'] = FR.get([Vd + 64], MT)
                    sl['RtT'] = FR.get([128], MT, parts=64)
                    sl['BTI'] = FR.get([64], MT, parts=64)
                wk['slots'].append(sl)
            return wk

        def gammas(H, lw, gam):
            pg = s.bank()
            for h in range(H):
                s.mm(pg[0:64, h * 32:(h + 1) * 32], lw[:, h * 64:(h + 1) * 64], cst[:, C_CI:C_CI + 32])
            s.act(gam, pg[0:64, 0:32 * H].rearrange("p (h c) -> p h c", h=H)[:, :, 0:2], AF.Exp)

        def store_T(idx, src_bf, tt, oTb):
            pb = s.bank().bitcast(BF16)
            for k in range(4):
                s.tr(pb[:, k * 128:(k + 1) * 128], src_bf[:, k * 128:(k + 1) * 128], idb)
            evac(oTb, pb[:, 0:512].rearrange("p (k t) -> p k t", k=4))
            s.dma(OT_scr[idx, :, tt * 128:(tt + 1) * 128].rearrange("(k p) t -> p k t", p=128), oTb)

        def phase_rwkv(l):
            A.reset()
            FR.reset()
            A.nbytes = ARENA_BYTES - FR_BYTES
            wk = alloc_wk(64, True)
            mu = A.get([1792])
            wup = A.get([512], F32, parts=64)
            aup = A.get([512], F32, parts=64)
            gup = A.get([512])
            kkb = A.get([512])
            kab = A.get([512])
            rkb = A.get([512])
            lng = A.get([512])
            lnb = A.get([512])
            Hst = A.get([8, 64], F32, parts=64)
            cur = [A.get([1792]) for _ in range(2)]
            prv = A.get([1792])
            lora = A.get([256])
            sgT = A.get([128])
            lw = A.get([512])
            av = A.get([512])
            gv = A.get([512])
            epos = A.get([512])
            eneg = A.get([512])
            eprv = A.get([512])
            kkn = A.get([512])
            kmod = A.get([512])
            Rb = A.get([512])
            Ab = A.get([512])
            Bb = A.get([512])
            Kb = A.get([512])
            MT = F32R if USE_F32R else F32
            KbM = [FR.get([512], MT) for _ in range(2)]
            BbM = [FR.get([512], MT) for _ in range(2)]
            Vr = FR.get([512], MT)
            Y = A.get([512])
            t1 = A.get([512])
            t2 = A.get([512])
            st8 = A.get([64])
            orb = [A.get([512], BF16) for _ in range(2)]
            oTb = [A.get([4, 128], BF16) for _ in range(2)]
            s.dma(mu, bc_part(W['rwkv_mu'][l:l + 1, :]))
            s.dma(wup, W['rwkv_w_up'][l])
            s.dma(aup, W['rwkv_a_up'][l])
            s.dma(gup, W['rwkv_g_up'][l])
            s.dma(rows[0:1, 0:512], W['rwkv_w0'][l:l + 1, :])
            s.dma(rows[0:1, 512:1024], W['rwkv_a0'][l:l + 1, :])
            for dst, nm in ((kkb, 'rwkv_k_k'), (kab, 'rwkv_k_a'), (rkb, 'rwkv_r_k'), (lng, 'rwkv_lnx_g'),
                            (lnb, 'rwkv_lnx_b')):
                s.dma(dst, bc_part(W[nm][l:l + 1, :]))
            s.memset('dve', Hst, 0.0)
            v3 = lambda ap: ap.rearrange("p (h k) -> p h k", h=8)
            import os
            for tt in range(int(os.environ.get('RW_TILES', NT))):
                cu = cur[tt % 2]
                r0 = tt * 128
                s.dma(cu, P_scr[r0:r0 + 128, 1536:3328])
                if tt == 0:
                    s.memset('dve', prv[0:1, :], 0.0)
                    s.dma(prv[1:128, :], P_scr[0:127, 1536:3328])
                else:
                    s.dma(prv, P_scr[r0 - 1:r0 + 127, 1536:3328])
                s.tt('pool', prv, prv, cu, ALU.subtract)
                s.tt('dve', prv, prv, mu, ALU.mult)
                s.tt('pool', cu, cu, prv, ALU.add)
                r_, k_, v_ = cu[:, 0:512], cu[:, 512:1024], cu[:, 1024:1536]
                pl = s.bank()
                s.tr(pl[0:64, 0:128], cu[:, 1536:1600], idf)
                s.tr(pl[0:64, 128:256], cu[:, 1600:1664], idf)
                s.tr(pl[:, 256:384], cu[:, 1664:1792], idf)
                s.act(lora[0:64, 0:128], pl[0:64, 0:128], AF.Tanh)
                s.copy('dve', lora[0:64, 128:256], pl[0:64, 128:256])
                s.act(sgT, pl[:, 256:384], AF.Sigmoid)
                px = s.bank()
                s.mm(px, lora[0:64, 0:128], wup, start=True, stop=False)
                s.mm(px, ones[0:1, 0:128], rows[0:1, 0:512], start=False, stop=True)
                s.act(lw, px, AF.Sigmoid)
                s.ts('dve', lw, lw, -math.exp(-0.5), None, ALU.mult)
                pa = s.bank()
                s.mm(pa, lora[0:64, 128:256], aup, start=True, stop=False)
                s.mm(pa, ones[0:1, 0:128], rows[0:1, 512:1024], start=False, stop=True)
                s.act(av, pa, AF.Sigmoid)
                pg = s.bank()
                s.mm(pg, sgT, gup)
                s.copy('dve', gv, pg)
                pc = s.bank()
                s.mm(pc, m_l, lw)
                s.act(epos, pc, AF.Exp)
                s.act(eneg, pc, AF.Exp, scale=-1.0)
                s.tt('dve', t1, pc, lw, ALU.subtract)
                s.act(eprv, t1, AF.Exp)
                s.tt('pool', kkn, k_, kkb, ALU.mult)
                s.tt('dve', t1, kkn, kkn, ALU.mult)
                s.reduce('dve', st8[:, 0:8], v3(t1), ALU.add)
                s.act(st8[:, 8:16], st8[:, 0:8], AF.Sqrt)
                s.ts('dve', st8[:, 8:16], st8[:, 8:16], 1e-12, None, ALU.max)
                s.op('dve', lambda g: g.reciprocal(st8[:, 16:24], st8[:, 8:16]), [st8[:, 8:16]], [st8[:, 16:24]])
                s.tt('dve', v3(kkn), v3(kkn), bc_last(st8[:, 16:24], 64), ALU.mult)
                s.stt('dve', t2, av, -1.0, kab, ALU.add, ALU.mult)
                s.stt('dve', kmod, t2, 1.0, k_, ALU.add, ALU.mult)
                s.tt('pool', Rb, r_, epos, ALU.mult)
                s.stt('dve', Ab, kkn, -1.0, eprv, ALU.mult, ALU.mult)
                s.tt('pool', t2, kkn, av, ALU.mult)
                s.tt('dve', Bb, t2, eneg, ALU.mult)
                s.tt('pool', Kb, kmod, eneg, ALU.mult)
                import os
                rst = int(os.environ.get('RW_STAGE', '9'))
                if rst < 2:
                    continue
                if rst < 3:
                    continue
                for c in range(2):
                    s.ts('dve', KbM[c], Kb, cst[:, C_CI + c:C_CI + c + 1], None, ALU.mult)
                    s.ts('dve', BbM[c], Bb, cst[:, C_CI + c:C_CI + c + 1], None, ALU.mult)
                s.copy('act', Vr, v_)
                chunk_core(8, 64, Rb, Kb, Ab, Bb, Vr, epos, Hst, Y, wk, True, KbM, BbM)
                if rst < 4:
                    continue
                s.reduce('dve', st8[:, 24:32], v3(Y), ALU.add)
                s.tt('pool', t1, Y, Y, ALU.mult)
                s.reduce('dve', st8[:, 32:40], v3(t1), ALU.add)
                s.ts('dve', st8[:, 24:32], st8[:, 24:32], 1.0 / 64, None, ALU.mult)
                s.tt('dve', st8[:, 40:48], st8[:, 24:32], st8[:, 24:32], ALU.mult)
                s.stt('dve', st8[:, 32:40], st8[:, 32:40], 1.0 / 64, st8[:, 40:48], ALU.mult, ALU.subtract)
                rstd_from_ssq(st8[:, 48:56], st8[:, 32:40], 1.0, 64e-5, st8[:, 56:64])
                s.tt('dve', v3(t1), v3(Y), bc_last(st8[:, 24:32], 64), ALU.subtract)
                s.tt('dve', v3(t1), v3(t1), bc_last(st8[:, 48:56], 64), ALU.mult)
                s.tt('pool', t1, t1, lng, ALU.mult)
                s.tt('pool', t1, t1, lnb, ALU.add)
                s.tt('dve', t2, r_, kmod, ALU.mult)
                s.tt('dve', t2, t2, rkb, ALU.mult)
                s.reduce('dve', st8[:, 0:8], v3(t2), ALU.add)
                s.tt('dve', v3(t2), v3(v_), bc_last(st8[:, 0:8], 64), ALU.mult)
                s.tt('pool', t1, t1, t2, ALU.add)
                ob = orb[tt % 2]
                s.tt('dve', ob, t1, gv, ALU.mult)
                store_T(1, ob, tt, oTb[tt % 2])

        def phase_gla(l):
            A.reset()
            FR.reset()
            A.nbytes = ARENA_BYTES - FR_BYTES
            wk = alloc_wk(128, False)
            aup = A.get([256], F32, parts=16)
            ng = A.get([512])
            Hst = A.get([4, 128], F32, parts=64)
            cur = [A.get([1552]) for _ in range(2)]
            adT = A.get([128], F32, parts=16)
            ll = A.get([256])
            epos = A.get([256])
            eneg = A.get([256])
            Rb = A.get([256])
            Kb = A.get([256])
            MT = F32R if USE_F32R else F32
            KbM = [FR.get([256], MT) for _ in range(2)]
            Vr = FR.get([512], MT)
            Y = A.get([512])
            t1 = A.get([512])
            sg = A.get([512])
            st4 = A.get([16])
            ogb = [A.get([512], BF16) for _ in range(2)]
            oTb = [A.get([4, 128], BF16) for _ in range(2)]
            s.dma(aup, W['gla_alpha_up'][l])
            s.dma(rows[0:1, 1024:1280], W['gla_alpha_b'][l:l + 1, :])
            s.dma(ng, bc_part(W['gla_norm_g4'][l:l + 1, :]))
            s.memset('dve', Hst, 0.0)
            v4 = lambda ap: ap.rearrange("p (h k) -> p h k", h=4)
            for tt in range(NT):
                cu = cur[tt % 2]
                r0 = tt * 128
                s.dma(cu, P_scr[r0:r0 + 128, 3328:4880])
                q_, k_, v_, gate = cu[:, 0:256], cu[:, 256:512], cu[:, 512:1024], cu[:, 1040:1552]
                pl = s.bank()
                s.tr(pl[0:16, 0:128], cu[:, 1024:1040], idf)
                s.copy('dve', adT, pl[0:16, 0:128])
                pz = s.bank()
                s.mm(pz[:, 0:256], adT, aup, start=True, stop=False)
                s.mm(pz[:, 0:256], ones[0:1, 0:128], rows[0:1, 1024:1280], start=False, stop=True)
                s.act(ll, pz[:, 0:256], AF.Sigmoid)
                s.act(ll, ll, AF.Ln)
                s.ts('dve', ll, ll, 1.0 / 16, None, ALU.mult)
                pc = s.bank()
                s.mm(pc[:, 0:256], m_l, ll)
                s.act(epos, pc[:, 0:256], AF.Exp)
                s.act(eneg, pc[:, 0:256], AF.Exp, scale=-1.0)
                s.stt('dve', Rb, q_, 0.125, epos, ALU.mult, ALU.mult)
                s.tt('pool', Kb, k_, eneg, ALU.mult)
                for c in range(2):
                    s.ts('dve', KbM[c], Kb, cst[:, C_CI + c:C_CI + c + 1], None, ALU.mult)
                s.copy('act', Vr, v_)
                chunk_core(4, 128, Rb, Kb, None, None, Vr, epos, Hst, Y, wk, False, KbM, None)
                s.tt('pool', t1, Y, Y, ALU.mult)
                s.reduce('dve', st4[:, 0:4], v4(t1), ALU.add)
                rstd_from_ssq(st4[:, 4:8], st4[:, 0:4], 1.0 / 128, EPS, st4[:, 8:12])
                s.tt('dve', v4(t1), v4(Y), bc_last(st4[:, 4:8], 128), ALU.mult)
                s.tt('pool', t1, t1, ng, ALU.mult)
                s.act(sg, gate, AF.Silu)
                ob = ogb[tt % 2]
                s.tt('dve', ob, t1, sg, ALU.mult)
                store_T(2, ob, tt, oTb[tt % 2])

        def phase_merge(l, xsrc):
            A.reset()
            gt1 = modbc[:, 2 * D:3 * D]
            gw = A.get([8, 3072], BF16)
            pw = A.get([12, 1024], BF16)
            ow = A.get([8, 1024], BF16)
            oTs = [A.get([12, 512], BF16) for _ in range(2)]
            mT = A.get([8, 512], BF16)
            macc = A.get([512])
            sgm = A.get([512])
            xt = [A.get([1024]) for _ in range(2)]
            tmp = A.get([512])
            for j in range(6):
                s.dma(gw[:, :, j * 512:(j + 1) * 512],
                      W['w_in'][l, :, NPROJ + j * 512:NPROJ + (j + 1) * 512].rearrange("(k p) n -> p k n", p=128),
                      q='pool')
            for bi, nm in enumerate(('proj_attn', 'proj_rwkv', 'proj_gla')):
                s.dma(pw[:, bi * 4:(bi + 1) * 4, :], W[nm][l].rearrange("(k p) n -> p k n", p=128), q='pool')
            s.dma(ow, W['w_out'][l].rearrange("(k p) n -> p k n", p=128), q='pool')
            for tg in range(4):
                ts_ = slice(tg * 512, (tg + 1) * 512)
                oT = oTs[tg % 2]
                for bi in range(3):
                    s.dma(oT[:, bi * 4:(bi + 1) * 4, :], OT_scr[bi, :, ts_].rearrange("(k p) t -> p k t", p=128))
                for dc in range(8):
                    for bi in range(3):
                        pg = s.bank()
                        for k in range(8):
                            s.mm(pg, gw[:, k, bi * 1024 + dc * 128:bi * 1024 + (dc + 1) * 128], hT[:, k, ts_],
                                 start=(k == 0), stop=(k == 7))
                        pp = s.bank()
                        for k in range(4):
                            s.mm(pp, pw[:, bi * 4 + k, dc * 128:(dc + 1) * 128], oT[:, bi * 4 + k, :],
                                 start=(k == 0), stop=(k == 3))
                        s.act(sgm, pg, AF.Sigmoid)
                        if bi == 0:
                            s.tt('dve', macc, pp, sgm, ALU.mult)
                        else:
                            s.tt('dve', sgm, pp, sgm, ALU.mult)
                            if bi == 1:
                                s.tt('pool', macc, macc, sgm, ALU.add)
                            else:
                                s.tt('pool', mT[:, dc, :], macc, sgm, ALU.add)
                for t4 in range(4):
                    tt = tg * 4 + t4
                    x_ = xt[tt % 2]
                    s.dma(x_, xsrc[tt * 128:(tt + 1) * 128, :])
                    for half in range(2):
                        hs = slice(half * 512, (half + 1) * 512)
                        po = s.bank()
                        for k in range(8):
                            s.mm(po, mT[:, k, t4 * 128:(t4 + 1) * 128], ow[:, k, hs], start=(k == 0), stop=(k == 7))
                        s.tt('dve', tmp, po, gt1[:, hs], ALU.mult)
                        s.tt('pool', x_[:, hs], x_[:, hs], tmp, ALU.add)
                    s.dma(out[tt * 128:(tt + 1) * 128, :], x_)

        def phase_moe(l):
            A.reset()
            gt2 = modbc[:, 5 * D:6 * D]
            slots_i = A.get([NT, 2], I32)
            wts2 = A.get([NT, 2])
            mark = A.off
            rw = A.get([8, 64])
            lg_all = A.get([NT, 36])
            hTf = [A.get([8, 128]) for _ in range(2)]
            s.memset('dve', rw, 0.0)
            s.memset('dve', rows[0:1, 1280:1344], 0.0)
            s.dma(rw[:, :, 0:36], W['router_w'][l].rearrange("(k p) n -> p k n", p=128))
            s.dma(rows[0:1, 1280:1316], W['router_b'][l:l + 1, :])
            base = A.off
            A2 = Arena(A.ap[:, base // 4:], A.nbytes - base)
            _norm_with(A2, l, rw, lg_all, hTf)
            G3 = lambda: A2.get([NT, 4])
            E3 = lambda: A2.get([NT, 32])
            T1 = lambda: A2.get([NT])
            oh, pen, ge = G3(), G3(), G3()
            em, m1, m2, am, pos, ovf = E3(), E3(), E3(), E3(), E3(), E3()
            carry_all = A2.get([NT + 1, 32])
            gmax, gs, gp, v1, v2, dd, ex, w1, w2 = T1(), T1(), T1(), T1(), T1(), T1(), T1(), T1(), T1()
            slots_f = A2.get([NT, 2])
            hrow = [A2.get([D], BF16) for _ in range(2)]
            gl = lg_all[:, :, 0:4]
            el = lg_all[:, :, 4:36]
            s.reduce('dve', gmax, gl, ALU.max)
            s.tt('dve', oh, gl, bc_last(gmax, 4), ALU.is_equal)
            s.tt('dve', ge, gl, bc_last(gmax, 4), ALU.subtract)
            s.act(ge, ge, AF.Exp)
            s.reduce('dve', gs, ge, ALU.add)
            s.op('dve', lambda g: g.reciprocal(gp, gs), [gs], [gp])
            s.ts('dve', pen, oh, 1e9, -1e9, ALU.mult, ALU.add)
            s.copy('dve', em, el)
            em64 = em.rearrange("p t (g e) -> p (t g) e", g=4)
            s.tt('dve', em64, em64, bc_last(pen.rearrange("p t g -> p (t g)"), 8), ALU.add)
            s.reduce('dve', v1, em, ALU.max)
            s.tt('dve', m1, em, bc_last(v1, 32), ALU.is_equal)
            s.stt('dve', em, m1, -1e9, em, ALU.mult, ALU.add)
            s.reduce('dve', v2, em, ALU.max)
            s.tt('dve', m2, em, bc_last(v2, 32), ALU.is_equal)
            s.tt('dve', dd, v2, v1, ALU.subtract)
            s.act(ex, dd, AF.Exp)
            s.ts('dve', w1, ex, 1.0, None, ALU.add)
            s.op('dve', lambda g: g.reciprocal(w1, w1), [w1], [w1])
            s.tt('dve', w2, ex, w1, ALU.mult)
            s.tt('dve', wts2[:, :, 0], w1, gp, ALU.mult)
            s.tt('dve', wts2[:, :, 1], w2, gp, ALU.mult)
            s.tt('dve', am, m1, m2, ALU.add)
            amf = am.rearrange("p t e -> p (t e)")
            pp = s.bank()
            s.mm(pp, cst[:, C_TRI:C_TRI + 128], amf)
            pt = s.bank()
            s.mm(pt, cst[:, C_ONE:C_ONE + 128], amf)
            s.memset('dve', carry_all[:, 0, :], 0.0)
            for tt in range(NT):
                s.tt('dve', carry_all[:, tt + 1, :], carry_all[:, tt, :], pt[:, tt * 32:(tt + 1) * 32], ALU.add)
            carry = carry_all[:, NT, :]
            s.tt('dve', pos.rearrange("p t e -> p (t e)"), pp, carry_all[:, 0:NT, :].rearrange("p t e -> p (t e)"), ALU.add)
            s.ts('dve', ovf, pos, float(CAP), 1e6, ALU.is_ge, ALU.mult)
            ecap = cst[:, C_ECAP:C_ECAP + 32]
            s.tt('dve', pos, pos, bass.AP(ecap.tensor, ecap.offset, [list(ecap.ap[0]), [0, NT], [1, 32]]), ALU.add)
            s.tt('dve', pos, pos, ovf, ALU.add)
            s.tt('dve', m1, m1, pos, ALU.mult)
            s.tt('dve', m2, m2, pos, ALU.mult)
            s.reduce('dve', slots_f[:, :, 0], m1, ALU.add)
            s.reduce('dve', slots_f[:, :, 1], m2, ALU.add)
            s.copy('dve', slots_i, slots_f)
            for tt in range(NT):
                hr = hrow[tt % 2]
                s.dma(hr, H2_scr[tt * 128:(tt + 1) * 128, :])
                for j in range(2):
                    s.idma(XG, hr, slots_i[:, tt, j:j + 1], True, NE * CAP)
            if 'cnt' in DBG:
                s.dma(DBG['cnt'][l], carry)
            A.off = mark
            NB = 2
            wg = [A.get([8, 512], BF16) for _ in range(NB)]
            wu = [A.get([8, 512], BF16) for _ in range(NB)]
            wd = [A.get([4, 1024], BF16) for _ in range(NB)]
            RT_ = CAP // 128
            xg = [A.get([RT_, D], BF16) for _ in range(2)]
            xT = [A.get([8, CAP], BF16) for _ in range(2)]
            hid = [A.get([4, CAP], BF16) for _ in range(2)]
            sg = [A.get([CAP], BF16) for _ in range(2)]
            yst = [A.get([D]) for _ in range(2)]
            y12 = [[A.get([D]) for _ in range(2)] for _ in range(2)]
            xts = [A.get([D]) for _ in range(2)]
            yi = 0
            for e in range(NE):
                b = e % NB
                s.dma(wg[b], W['exp_w_gate'][l, e].rearrange("(k p) n -> p k n", p=128), q='pool')
                s.dma(wu[b], W['exp_w_up'][l, e].rearrange("(k p) n -> p k n", p=128), q='pool')
                s.dma(wd[b], W['exp_w_down'][l, e].rearrange("(k p) n -> p k n", p=128), q='pool')
                xg_ = xg[e % 2]
                s.dma(xg_, XG[e * CAP:(e + 1) * CAP, :].rearrange("(r p) d -> p r d", p=128))
                xT_ = xT[e % 2]
                for r in range(RT_):
                    pb = s.bank().bitcast(BF16)
                    for k in range(8):
                        s.tr(pb[:, k * 128:(k + 1) * 128], xg_[:, r, k * 128:(k + 1) * 128], idb)
                    evac(xT_[:, :, r * 128:(r + 1) * 128], pb.rearrange("p (k t) -> p k t", k=8))
                hid_ = hid[e % 2]
                for fc in range(4):
                    fs = slice(fc * 128, (fc + 1) * 128)
                    pg = s.bank()
                    for k in range(8):
                        s.mm(pg[:, 0:CAP], wg[b][:, k, fs], xT_[:, k, :], start=(k == 0), stop=(k == 7))
                    pu = s.bank()
                    for k in range(8):
                        s.mm(pu[:, 0:CAP], wu[b][:, k, fs], xT_[:, k, :], start=(k == 0), stop=(k == 7))
                    sgb = sg[fc % 2]
                    s.act(sgb, pg[:, 0:CAP], AF.Silu)
                    s.tt('dve', hid_[:, fc, :], pu[:, 0:CAP], sgb, ALU.mult)
                for r in range(RT_):
                    ys = yst[yi % 2]
                    yi += 1
                    for half in range(2):
                        hs = slice(half * 512, (half + 1) * 512)
                        py = s.bank()
                        for k in range(4):
                            s.mm(py, hid_[:, k, r * 128:(r + 1) * 128], wd[b][:, k, hs], start=(k == 0), stop=(k == 3))
                        evac(ys[:, hs], py)
                    s.dma(YG[e * CAP + r * 128:e * CAP + (r + 1) * 128, :], ys)
            for tt in range(NT):
                ya, yb = y12[tt % 2]
                x_ = xts[tt % 2]
                s.dma(x_, out[tt * 128:(tt + 1) * 128, :])
                s.memset('pool', ya, 0.0)
                s.memset('pool', yb, 0.0)
                s.idma(ya, YG, slots_i[:, tt, 0:1], False, NE * CAP)
                s.idma(yb, YG, slots_i[:, tt, 1:2], False, NE * CAP)
                s.ts('dve', ya, ya, wts2[:, tt, 0:1], None, ALU.mult)
                s.stt('dve', ya, yb, wts2[:, tt, 1:2], ya, ALU.mult, ALU.add)
                s.tt('pool', ya, ya, gt2, ALU.mult)
                s.tt('dve', x_, x_, ya, ALU.add)
                s.dma(out[tt * 128:(tt + 1) * 128, :], x_)

        def _norm_with(A2, l, rw, lg_all, hTf):
            nonlocal A
            saved = A
            A = A2
            try:
                phase_norm(l, 1, out, router=(rw, lg_all, hTf))
            finally:
                A = saved

        order = ['mod', 'norm', 'proj', 'attn', 'rwkv', 'gla', 'merge', 'moe']
        stop_l, stop_p = (nlayers - 1, 'moe') if upto == 'all' else upto
        done = False
        for l in range(nlayers):
            for ph in order:
                if phases is not None and (l, ph) not in phases:
                    continue
                if ph == 'mod':
                    phase_mod(l)
                    dump('mod', modbc[0:1, :])
                elif ph == 'norm':
                    phase_norm(l, 0, x_in if l == 0 else out)
                elif ph == 'proj':
                    phase_proj(l)
                elif ph == 'attn':
                    phase_attn(l)
                elif ph == 'rwkv':
                    s.barrier()
                    phase_rwkv(l)
                    s.barrier()
                    A.nbytes = ARENA_BYTES
                elif ph == 'gla':
                    s.barrier()
                    phase_gla(l)
                    s.barrier()
                    A.nbytes = ARENA_BYTES
                elif ph == 'merge':
                    phase_merge(l, x_in if l == 0 else out)
                elif ph == 'moe':
                    phase_moe(l)
                if (l, ph) == (stop_l, stop_p):
                    done = True
                    break
            if done:
                break
        if 'P' in DBG:
            s.dma(DBG['P'], P_scr)
        if 'OT' in DBG:
            s.dma(DBG['OT'], OT_scr, q='pool')
        if 'hT' in DBG:
            hf = arena_t[:, 0:4096]
            for k in range(8):
                for g in range(0, S, 4096 // 1):
                    pass
        s.finish()
        print("instructions:", s.ninst, "sbuf left:", nc.sbuf_bytes_remaining, flush=True)
    return nc


def prep_weights(inp):
    f = lambda a: np.ascontiguousarray(np.asarray(a, dtype=np.float32))
    w = {}
    for k in ('ada_w', 'ada_b', 'norm1_g', 'norm2_g', 'w_in', 'attn_subln_g', 'rwkv_mu', 'rwkv_w_up', 'rwkv_w0',
              'rwkv_a_up', 'rwkv_a0', 'rwkv_g_up', 'rwkv_k_k', 'rwkv_k_a', 'rwkv_lnx_g', 'rwkv_lnx_b',
              'gla_alpha_up', 'gla_alpha_b', 'proj_attn', 'proj_rwkv', 'proj_gla', 'w_out', 'exp_w_gate',
              'exp_w_up', 'exp_w_down'):
        w[k] = f(inp[k])
    w['attn_qkg'] = f(np.concatenate([np.tile(inp['attn_qn_g'], (1, 8)), np.tile(inp['attn_kn_g'], (1, 8))], axis=1))
    w['attn_lambda'] = f(np.reshape(inp['attn_lambda'], (DEPTH, 256)))
    w['rwkv_r_k'] = f(np.reshape(inp['rwkv_r_k'], (DEPTH, 512)))
    w['gla_norm_g4'] = f(np.tile(inp['gla_norm_g'], (1, 4)))
    w['router_w'] = f(np.concatenate([inp['router_grp_w'], inp['router_exp_w']], axis=2))
    w['router_b'] = f(np.concatenate([inp['router_grp_b'], inp['router_exp_b']], axis=1))
    return w


def kernel(**inputs):
    x = np.asarray(inputs['x'], dtype=np.float32)
    c = np.asarray(inputs['c'], dtype=np.float32)
    w = prep_weights(inputs)
    cst, aug = make_consts()
    nc = build_program()
    in_maps = []
    for b in range(8):
        m = dict(w)
        m['x'] = np.ascontiguousarray(x[b])
        m['c'] = np.ascontiguousarray(c[b].reshape(8, 128).T)
        m['consts'] = cst
        m['aug'] = aug
        in_maps.append(m)
    res = run_bass_kernel_spmd(nc, in_maps, core_ids=list(range(8)))
    return np.stack([np.asarray(r['out'], dtype=np.float32) for r in res.results], axis=0)
```

```python
import contextlib
import math
import numpy as np
import concourse.bass as bass
import concourse.mybir as mybir
from concourse.bass_utils import run_bass_kernel_spmd

F32 = mybir.dt.float32
BF16 = mybir.dt.bfloat16
I32 = mybir.dt.int32
AF = mybir.ActivationFunctionType
ALU = mybir.AluOpType
AX = mybir.AxisListType

D = 1024
S = 2048
NT = S // 128
DEPTH = 2
N_IN = 7952
NPROJ = 4880
EPS = 1e-6
SAME_ENGINE_SYNC = True


def _region(ap):
    t = ap.tensor
    a = ap.ap
    off = int(ap.offset)
    es = mybir.dt.size(ap.dtype)
    tn = type(t).__name__
    if 'DRam' in tn:
        ext = sum(int(s) * (int(c) - 1) for s, c in a) + 1
        return (t.name, 0, 1, off * es, (off + ext) * es)
    if 'PSum' in tn:
        return (t.name, 0, 128, 0, 1 << 20)
    ps = int(a[0][0])
    npart = int(a[0][1])
    if ps == 0:
        ps = 1 << 40
    p0 = off // ps
    f0 = off % ps
    ext = sum(int(s) * (int(c) - 1) for s, c in a[1:]) + 1
    return (t.name, p0, p0 + npart, f0 * es, (f0 + ext) * es)


def _ovl(r, q):
    return r[1] < q[2] and q[1] < r[2] and r[3] < q[4] and q[3] < r[4]


def _contains(big, small):
    return big[1] <= small[1] and small[2] <= big[2] and big[3] <= small[3] and small[4] <= big[4]


class Sched:
    def __init__(self, nc, stack):
        self.nc = nc
        self.engs = {'pe': nc.tensor, 'act': nc.scalar, 'dve': nc.vector, 'pool': nc.gpsimd, 'sp': nc.sync}
        self.sem = {}
        self.cnt = {}
        for e in ['pe', 'act', 'dve', 'pool']:
            self.sem[e] = stack.enter_context(nc.semaphore('s_' + e))
            self.cnt[e] = 0
        self.ndsem = {'sp': 16, 'pool': 8}
        for q in ['sp', 'pool']:
            for j in range(self.ndsem[q]):
                k = ('d', q, j)
                self.sem[k] = stack.enter_context(nc.semaphore('d_%s_%d' % (q, j)))
                self.cnt[k] = 0
        self.drr = {'sp': 0, 'pool': 0}
        self.known = {e: {} for e in self.engs}
        self.recs = {}
        self.npsum = 0
        self.banks = []
        self.ninst = 0
        import os
        self.max_ops = int(os.environ.get('MAX_OPS', '1000000000'))
        self.force_dma = False
        self.log_ops = bool(os.environ.get('LOG_OPS'))

    def _deps(self, reads, writes):
        deps = {}
        for ap in reads:
            r = _region(ap)
            for rec in self.recs.get(r[0], ()):
                if rec[1] and _ovl(rec[0], r):
                    if deps.get(rec[2], 0) < rec[3]:
                        deps[rec[2]] = rec[3]
        for ap in writes:
            r = _region(ap)
            for rec in self.recs.get(r[0], ()):
                if _ovl(rec[0], r):
                    if deps.get(rec[2], 0) < rec[3]:
                        deps[rec[2]] = rec[3]
        return deps

    def _record(self, reads, writes, k, v, noprune=False):
        for ap in reads:
            r = _region(ap)
            lst = self.recs.setdefault(r[0], [])
            for rec in lst:
                if (not rec[1]) and rec[2] == k and rec[0] == r:
                    rec[3] = max(rec[3], v)
                    break
            else:
                lst.append([r, False, k, v])
        for ap in writes:
            r = _region(ap)
            lst = self.recs.setdefault(r[0], [])
            if not noprune:
                lst[:] = [rec for rec in lst if not _contains(r, rec[0])]
            lst.append([r, True, k, v])

    def _waits(self, e, deps):
        eng = self.engs[e]
        for k, v in deps.items():
            if self.known[e].get(k, 0) >= v:
                continue
            if k == e:
                if e == 'pe' or not SAME_ENGINE_SYNC or v > self.cnt[e]:
                    continue
            eng.wait_ge(self.sem[k], v)
            self.known[e][k] = v
            self.ninst += 1

    def op(self, e, fn, reads, writes, inc=True):
        if self.ninst >= self.max_ops:
            return None
        if self.log_ops:
            import inspect
            fr = inspect.stack()
            print('OP', self.ninst, e, [f.lineno for f in fr[1:4]], flush=True)
        pr = [r for r in reads if 'PSum' in type(r.tensor).__name__]
        if pr:
            reads = [r for r in reads if 'PSum' not in type(r.tensor).__name__]
            writes = list(writes) + pr
        self._waits(e, self._deps(reads, writes))
        ins = fn(self.engs[e])
        self.ninst += 1
        if inc:
            self.cnt[e] += 1
            ins.then_inc(self.sem[e], 1)
            val = self.cnt[e]
        else:
            val = self.cnt[e] + 1
        self._record(reads, writes, e, val)
        return ins

    def dma(self, out, in_, q='sp', **kw):
        if self.ninst >= self.max_ops and not self.force_dma:
            return
        if self.log_ops:
            import inspect
            fr = inspect.stack()
            print('DMA', self.ninst, q, [f.lineno for f in fr[1:3]], flush=True)
        self._waits(q, self._deps([in_], [out]))
        j = self.drr[q]
        self.drr[q] = (j + 1) % self.ndsem[q]
        k = ('d', q, j)
        if self.known[q].get(k, 0) < self.cnt[k]:
            self.engs[q].wait_ge(self.sem[k], self.cnt[k])
            self.known[q][k] = self.cnt[k]
            self.ninst += 1
        self.engs[q].dma_start(out=out, in_=in_, **kw).then_inc(self.sem[k], 16)
        self.ninst += 1
        self.cnt[k] += 16
        self._record([in_], [out], k, self.cnt[k])

    def idma(self, out, in_, idx, scatter, nrows):
        q = 'pool'
        deps = self._deps([in_, idx], [out])
        if scatter:
            deps = {k_: v_ for k_, v_ in deps.items() if not (isinstance(k_, tuple) and k_[1] == 'pool')}
        self._waits(q, deps)
        j = self.drr[q]
        self.drr[q] = (j + 1) % self.ndsem[q]
        k = ('d', q, j)
        if self.known[q].get(k, 0) < self.cnt[k]:
            self.engs[q].wait_ge(self.sem[k], self.cnt[k])
            self.known[q][k] = self.cnt[k]
        off = bass.IndirectOffsetOnAxis(ap=idx, axis=0)
        if not hasattr(self, 'bc_regs'):
            self.bc_regs = {}
        if nrows not in self.bc_regs:
            r = self.engs[q].alloc_register("bc%d" % nrows)
            self.engs[q].reg_mov(r, nrows - 1)
            self.bc_regs[nrows] = r
        bcr = self.bc_regs[nrows]
        if scatter:
            ins = self.engs[q].indirect_dma_start(out=out, out_offset=off, in_=in_, in_offset=None,
                                                  bounds_check=bcr, oob_is_err=False)
        else:
            ins = self.engs[q].indirect_dma_start(out=out, out_offset=None, in_=in_, in_offset=off,
                                                  bounds_check=bcr, oob_is_err=False)
        ins.then_inc(self.sem[k], 16)
        self.ninst += 1
        self.cnt[k] += 16
        self._record([in_, idx], [out], k, self.cnt[k], noprune=scatter)

    def finish(self):
        sp = self.engs['sp']
        for k, v in self.cnt.items():
            if v > 0:
                sp.wait_ge(self.sem[k], v)

    def mm(self, out, lhsT, rhs, start=True, stop=True):
        return self.op('pe', lambda e: e.matmul(out, lhsT, rhs, start=start, stop=stop),
                       [lhsT, rhs], [out], inc=stop)

    def tr(self, out, in_, ident):
        return self.op('pe', lambda e: e.transpose(out, in_, ident), [in_, ident], [out])

    def act(self, out, in_, func, bias=None, scale=None, accum_out=None, eng='act'):
        kw = {}
        reads = [in_]
        writes = [out]
        if bias is not None:
            kw['bias'] = bias
            if not isinstance(bias, (int, float)):
                reads.append(bias)
        if scale is not None:
            kw['scale'] = scale
            if not isinstance(scale, (int, float)):
                reads.append(scale)
        if accum_out is not None:
            kw['accum_out'] = accum_out
            writes.append(accum_out)
        return self.op('act', lambda e: e.activation(out, in_, func, **kw), reads, writes)

    def copy(self, e, out, in_):
        if e == 'act':
            return self.op('act', lambda g: g.copy(out, in_), [in_], [out])
        return self.op(e, lambda g: g.tensor_copy(out, in_), [in_], [out])

    def tt(self, e, out, in0, in1, op):
        return self.op(e, lambda g: g.tensor_tensor(out, in0, in1, op), [in0, in1], [out])

    def ts(self, e, out, in0, s1, s2, op0, op1=None, accum_out=None):
        reads = [in0] + [s for s in (s1, s2) if s is not None and not isinstance(s, (int, float))]
        writes = [out] + ([accum_out] if accum_out is not None else [])
        kw = {}
        if accum_out is not None:
            kw['accum_out'] = accum_out
        if op1 is None:
            return self.op(e, lambda g: g.tensor_scalar(out, in0, s1, s2, op0, **kw), reads, writes)
        return self.op(e, lambda g: g.tensor_scalar(out, in0, s1, s2, op0, op1, **kw), reads, writes)

    def stt(self, e, out, in0, scalar, in1, op0, op1):
        reads = [in0, in1] + ([scalar] if not isinstance(scalar, (int, float)) else [])
        return self.op(e, lambda g: g.scalar_tensor_tensor(out, in0, scalar, in1, op0, op1), reads, [out])

    def memset(self, e, ap, val):
        return self.op(e, lambda g: g.memset(ap, val), [], [ap])

    def reduce(self, e, out, in_, op, axis=None):
        axis = AX.X if axis is None else axis
        return self.op(e, lambda g: g.tensor_reduce(out, in_, axis, op), [in_], [out])

    def bank(self):
        b = self.banks[self.npsum % len(self.banks)]
        self.npsum += 1
        return b


def bc_last(ap, n):
    return bass.AP(ap.tensor, ap.offset, [list(p) for p in ap.ap] + [[0, n]])


def bc_part(ap, n=128):
    dims = [list(p) for p in ap.ap]
    if len(dims) == 2 and dims[0][1] == 1:
        dims = dims[1:]
    return bass.AP(ap.tensor, ap.offset, [[0, n]] + dims)


C_ID, C_ML, C_MSL, C_MSLT, C_MB, C_CI = 0, 128, 256, 384, 512, 640
C_CM = 704
C_TRI, C_ONE, C_ECAP = 960, 1088, 1216
NCONST = 1248
CAP = 512
NE = 32


def make_consts():
    c = np.zeros((128, NCONST), np.float32)
    i = np.arange(128)
    same = (i[:, None] // 64) == (i[None, :] // 64)
    c[:, C_ID:C_ID + 128] = np.eye(128)
    c[:, C_ML:C_ML + 128] = (same & (i[:, None] <= i[None, :]))
    c[:, C_MSL:C_MSL + 128] = (same & (i[:, None] < i[None, :]))
    c[:, C_MSLT:C_MSLT + 128] = (same & (i[:, None] > i[None, :]))
    c[:, C_MB:C_MB + 128] = np.where(i[:, None] > i[None, :], -30000.0, 0.0)
    c[:, C_CI] = (i < 64)
    c[:, C_CI + 1] = (i >= 64)
    c[:, C_CM:C_CM + 64] = 1.0
    c[:, C_CM + 128 + 64:C_CM + 256] = 1.0
    c[:, C_TRI:C_TRI + 128] = (i[:, None] < i[None, :])
    c[:, C_ONE:C_ONE + 128] = 1.0
    c[:, C_ECAP:C_ECAP + 32] = np.arange(32)[None, :] * CAP
    aug = np.zeros((4, 2, 4, S), np.float32)
    t = np.arange(S)
    for h in range(4):
        sl = 2.0 ** (-8.0 * (h + 1) / 4)
        aug[0, 0, h] = 1.0
        aug[1, 0, h] = -sl * (t % 128)
        aug[2, 0, h] = 1.0
        aug[3, 0, h] = -sl * 128 * (t // 128)
        aug[0, 1, h] = sl * (t % 128)
        aug[1, 1, h] = 1.0
        aug[2, 1, h] = sl * 128 * (t // 128)
        aug[3, 1, h] = 1.0
    return c, aug


WSPECS = [
    ('ada_w', [DEPTH, D, 6 * D]), ('ada_b', [DEPTH, 6 * D]), ('norm1_g', [DEPTH, D]), ('norm2_g', [DEPTH, D]),
    ('w_in', [DEPTH, D, N_IN]), ('attn_qkg', [DEPTH, 1024]), ('attn_lambda', [DEPTH, 256]),
    ('attn_subln_g', [DEPTH, 128]), ('rwkv_mu', [DEPTH, 1792]), ('rwkv_w_up', [DEPTH, 64, 512]),
    ('rwkv_w0', [DEPTH, 512]), ('rwkv_a_up', [DEPTH, 64, 512]), ('rwkv_a0', [DEPTH, 512]),
    ('rwkv_g_up', [DEPTH, 128, 512]), ('rwkv_k_k', [DEPTH, 512]), ('rwkv_k_a', [DEPTH, 512]),
    ('rwkv_r_k', [DEPTH, 512]), ('rwkv_lnx_g', [DEPTH, 512]), ('rwkv_lnx_b', [DEPTH, 512]),
    ('gla_alpha_up', [DEPTH, 16, 256]), ('gla_alpha_b', [DEPTH, 256]), ('gla_norm_g4', [DEPTH, 512]),
    ('proj_attn', [DEPTH, 512, D]), ('proj_rwkv', [DEPTH, 512, D]), ('proj_gla', [DEPTH, 512, D]),
    ('w_out', [DEPTH, D, D]), ('router_w', [DEPTH, D, 36]), ('router_b', [DEPTH, 36]),
    ('exp_w_gate', [DEPTH, 32, D, 512]), ('exp_w_up', [DEPTH, 32, D, 512]), ('exp_w_down', [DEPTH, 32, 512, D]),
]


class Arena:
    def __init__(self, ap, nbytes):
        self.ap = ap
        self.nbytes = nbytes
        self.off = 0

    def reset(self):
        self.off = 0

    def get(self, shape, dt=F32, parts=128):
        es = mybir.dt.size(dt)
        n = 1
        for v in shape:
            n *= v
        nb = (n * es + 31) // 32 * 32
        assert self.off + nb <= self.nbytes, ("arena overflow", self.off, nb)
        a = self.ap[:, self.off // 4:(self.off + nb) // 4]
        self.off += nb
        if dt != F32:
            a = a.bitcast(dt)
        a = a[0:parts, 0:n]
        if len(shape) == 2:
            a = a.rearrange("p (a b) -> p a b", a=shape[0])
        elif len(shape) == 3:
            a = a.rearrange("p (a b c) -> p a b c", a=shape[0], b=shape[1])
        return a


def build_program(nlayers=DEPTH, upto='all', dbg=None, phases=None, scr_io=False):
    dbg = dbg or {}
    nc = bass.Bass("TRN2", target_bir_lowering=False)
    dram = {}
    x_in = nc.dram_tensor("x", [S, D], F32, kind="ExternalInput").ap()
    c_in = nc.dram_tensor("c", [128, 8], F32, kind="ExternalInput").ap()
    cst_in = nc.dram_tensor("consts", [128, NCONST], F32, kind="ExternalInput").ap()
    aug_in = nc.dram_tensor("aug", [4, 2, 4, S], F32, kind="ExternalInput").ap()
    W = {}
    for name, shp in WSPECS:
        W[name] = nc.dram_tensor(name, shp, F32, kind="ExternalInput").ap()
    out = nc.dram_tensor("out", [S, D], F32, kind="ExternalOutput").ap()
    P_scr = nc.dram_tensor("P_scr", [S, NPROJ], F32, kind="ExternalInput" if scr_io else "Internal").ap()
    OT_scr = nc.dram_tensor("OT_scr", [3, 512, S], BF16, kind="ExternalOutput" if scr_io else "Internal").ap()
    H2_scr = nc.dram_tensor("H2_scr", [S, D], BF16, kind="Internal").ap()
    XG = nc.dram_tensor("XG", [NE * CAP, D], BF16, kind="Internal").ap()
    YG = nc.dram_tensor("YG", [NE * CAP, D], F32, kind="Internal").ap()
    DBG = {}
    for name, shp in dbg.items():
        DBG[name] = nc.dram_tensor("dbg_" + name, list(shp), F32, kind="ExternalOutput").ap()

    with contextlib.ExitStack() as stack:
        s = Sched(nc, stack)
        banks = [nc.alloc_psum_tensor("pb%d" % i, [128, 512], F32).ap() for i in range(8)]
        s.banks = banks
        hT = nc.alloc_sbuf_tensor("hT", [128, 8, S], BF16).ap()
        modbc = nc.alloc_sbuf_tensor("modbc", [128, 6 * D], F32).ap()
        cst = nc.alloc_sbuf_tensor("cst", [128, NCONST], F32).ap()
        idb = nc.alloc_sbuf_tensor("idb", [128, 128], BF16).ap()
        mbb = nc.alloc_sbuf_tensor("mbb", [128, 128], BF16).ap()
        ones = nc.alloc_sbuf_tensor("ones", [1, 128], F32).ap()
        rows = nc.alloc_sbuf_tensor("rows", [1, 1408], F32).ap()
        csb = nc.alloc_sbuf_tensor("csb", [128, 8], F32).ap()
        ARENA_BYTES = (int(nc.sbuf_bytes_remaining) - 2048) // 1024 * 1024
        print('arena bytes', ARENA_BYTES, flush=True)
        arena_t = nc.alloc_sbuf_tensor("arena", [128, ARENA_BYTES // 4], F32).ap()
        A = Arena(arena_t, ARENA_BYTES)
        idf = cst[:, C_ID:C_ID + 128]
        m_l = cst[:, C_ML:C_ML + 128]
        m_sl = cst[:, C_MSL:C_MSL + 128]
        m_slT = cst[:, C_MSLT:C_MSLT + 128]
        cind = cst[:, C_CI:C_CI + 2]

        rr = [0]

        def alt():
            rr[0] += 1
            return 'act' if rr[0] % 2 else 'dve'

        def evac(out_, in_):
            return s.copy(alt(), out_, in_)

        def dump(name, ap_sb, dst=None):
            if name in DBG:
                s.dma(DBG[name] if dst is None else dst, ap_sb)

        s.dma(cst, cst_in)
        s.dma(csb, c_in)
        s.memset('dve', ones, 1.0)
        s.copy('dve', idb, idf)
        s.copy('dve', mbb, cst[:, C_MB:C_MB + 128])
        s.act(csb, csb, AF.Silu)

        def rstd_from_ssq(dst, ssq, scale, eps, tmp):
            s.ts('dve', tmp, ssq, scale, eps, ALU.mult, ALU.add)
            s.act(tmp, tmp, AF.Sqrt)
            s.op('dve', lambda g: g.reciprocal(dst, tmp), [tmp], [dst])

        def phase_mod(l):
            A.reset()
            adaw = [A.get([8, 512]) for _ in range(2)]
            adab = [A.get([512]) for _ in range(2)]
            cbc = A.get([8, 128])
            for k in range(8):
                s.copy('dve', cbc[:, k:k + 1, :], bc_last(csb[:, k:k + 1], 128))
            for cg in range(12):
                buf = adaw[cg % 2]
                bb = adab[cg % 2]
                s.dma(buf, W['ada_w'][l, :, cg * 512:(cg + 1) * 512].rearrange("(k p) n -> p k n", p=128))
                s.dma(bb, bc_part(W['ada_b'][l:l + 1, cg * 512:(cg + 1) * 512]))
                pb = s.bank()
                for k in range(8):
                    s.mm(pb, cbc[:, k, :], buf[:, k, :], start=(k == 0), stop=(k == 7))
                s.tt('dve', modbc[:, cg * 512:(cg + 1) * 512], pb, bb, ALU.add)

        def phase_norm(l, which, src, router=None):
            A.reset()
            sh = modbc[:, (3 * which) * D:(3 * which + 1) * D]
            sc = modbc[:, (3 * which + 1) * D:(3 * which + 2) * D]
            G1 = A.get([D])
            gb = A.get([D])
            ssq = A.get([NT])
            rstd = A.get([NT])
            tmpn = A.get([NT])
            junk = A.get([D])
            xts = [A.get([D]) for _ in range(3)]
            tmps = [A.get([D]) for _ in range(2)]
            hbs = [A.get([D], BF16 if router is None else F32) for _ in range(2)]
            s.dma(gb, bc_part(W['norm1_g' if which == 0 else 'norm2_g'][l:l + 1, :]))
            s.stt('dve', G1, sc, 1.0, gb, ALU.add, ALU.mult)
            s.memset('dve', ssq, 0.0)
            if router is not None:
                rw, lg_all, hTf = router
                hbbs = [A.get([D], BF16) for _ in range(2)]
            for tt in range(NT):
                xt = xts[tt % 3]
                s.dma(xt, src[tt * 128:(tt + 1) * 128, :])
                s.act(junk, xt, AF.Square, accum_out=ssq[:, tt:tt + 1])
                rstd_from_ssq(rstd[:, tt:tt + 1], ssq[:, tt:tt + 1], 1.0 / D, EPS, tmpn[:, tt:tt + 1])
                tmp = tmps[tt % 2]
                hb = hbs[tt % 2]
                s.stt('dve', tmp, xt, rstd[:, tt:tt + 1], G1, ALU.mult, ALU.mult)
                s.tt('dve', hb, tmp, sh, ALU.add)
                if router is None:
                    pb = s.bank().bitcast(BF16)
                    for k in range(8):
                        s.tr(pb[:, k * 128:(k + 1) * 128], hb[:, k * 128:(k + 1) * 128], idb)
                    evac(hT[:, :, tt * 128:(tt + 1) * 128], pb.rearrange("p (k t) -> p k t", k=8))
                else:
                    hf = hTf[tt % 2]
                    for half in range(2):
                        pb = s.bank()
                        for k in range(4):
                            kk = half * 4 + k
                            s.tr(pb[:, k * 128:(k + 1) * 128], hb[:, kk * 128:(kk + 1) * 128], idf)
                        s.copy('act', hT[:, half * 4:half * 4 + 4, tt * 128:(tt + 1) * 128],
                               pb.rearrange("p (k t) -> p k t", k=4))
                        s.copy('dve', hf[:, half * 4:half * 4 + 4, :], pb.rearrange("p (k t) -> p k t", k=4))
                    pl = s.bank()
                    for k in range(8):
                        s.mm(pl[:, 0:64], hf[:, k, :], rw[:, k, :], start=(k == 0), stop=False)
                    s.mm(pl[:, 0:64], ones[0:1, 0:128], rows[0:1, 1280:1344], start=False, stop=True)
                    s.copy('dve', lg_all[:, tt, :], pl[:, 0:36])
                    hbb = hbbs[tt % 2]
                    s.copy('act', hbb, hb)
                    s.dma(H2_scr[tt * 128:(tt + 1) * 128, :], hbb)

        def phase_proj(l):
            A.reset()
            wb = [A.get([8, 512], BF16) for _ in range(2)]
            stg = [A.get([512]) for _ in range(4)]
            nchunk = (NPROJ + 511) // 512
            si = 0
            for j in range(nchunk):
                c0 = j * 512
                ncol = min(512, NPROJ - c0)
                w = wb[j % 2]
                s.dma(w[:, :, 0:ncol], W['w_in'][l, :, c0:c0 + ncol].rearrange("(k p) n -> p k n", p=128), q='pool')
                for tt in range(NT):
                    pb = s.bank()
                    for k in range(8):
                        s.mm(pb[:, 0:ncol], hT[:, k, tt * 128:(tt + 1) * 128], w[:, k, 0:ncol],
                             start=(k == 0), stop=(k == 7))
                    st = stg[si % 4]
                    si += 1
                    evac(st[:, 0:ncol], pb[:, 0:ncol])
                    s.dma(P_scr[tt * 128:(tt + 1) * 128, c0:c0 + ncol], st[:, 0:ncol])

        def phase_attn(l):
            lambda_init = 0.8 - 0.6 * math.exp(-0.3 * l)
            A.reset()
            qkT = A.get([16, S], BF16, parts=68)
            vaug = A.get([NT, 4 * 129], BF16)
            qkg = A.get([1024])
            sub_g = A.get([128])
            lamb = A.get([256])
            small = A.get([64])
            qk = [A.get([1024]) for _ in range(2)]
            vt = [A.get([512]) for _ in range(2)]
            sq = A.get([1024])
            qn = [A.get([1024], BF16) for _ in range(2)]
            PT = [A.get([512], BF16) for _ in range(3)]
            t0 = [A.get([128]) for _ in range(2)]
            ot = [A.get([128]) for _ in range(2)]
            oa = [A.get([512], BF16) for _ in range(2)]
            oT = [A.get([4, 128], BF16) for _ in range(2)]
            s.dma(qkg, bc_part(W['attn_qkg'][l:l + 1, :]))
            s.dma(sub_g, bc_part(W['attn_subln_g'][l:l + 1, :]))
            s.dma(lamb, bc_part(W['attn_lambda'][l:l + 1, :]))
            lp = small[:, 0:2]
            s.tt('dve', sq[:, 0:64], lamb[:, 0:64], lamb[:, 64:128], ALU.mult)
            s.tt('dve', sq[:, 64:128], lamb[:, 128:192], lamb[:, 192:256], ALU.mult)
            s.reduce('dve', lp, sq[:, 0:128].rearrange("p (a b) -> p a b", a=2), ALU.add)
            s.act(lp, lp, AF.Exp)
            nlam = small[:, 2:3]
            s.tt('dve', nlam, lp[:, 1:2], lp[:, 0:1], ALU.subtract)
            s.ts('dve', nlam, nlam, -lambda_init, None, ALU.add)
            for g in range(16):
                isk = g // 8
                h = (g % 8) // 2
                s.dma(qkT[64:68, g, :], aug_in[:, isk, h, :], q='pool')
            s.memset('dve', vaug, 1.0)
            ssq = small[:, 8:24]
            rs = small[:, 24:40]
            tm = small[:, 40:56]
            for tt in range(NT):
                q_ = qk[tt % 2]
                v_ = vt[tt % 2]
                s.dma(q_, P_scr[tt * 128:(tt + 1) * 128, 0:1024])
                s.dma(v_, P_scr[tt * 128:(tt + 1) * 128, 1024:1536])
                s.tt('dve', sq, q_, q_, ALU.mult)
                s.reduce('dve', ssq, sq.rearrange("p (a b) -> p a b", a=16), ALU.add)
                rstd_from_ssq(rs[:, 0:8], ssq[:, 0:8], 1.0, 64 * EPS, tm[:, 0:8])
                rstd_from_ssq(rs[:, 8:16], ssq[:, 8:16], 1.0 / 64, EPS, tm[:, 8:16])
                s.tt('dve', sq.rearrange("p (a b) -> p a b", a=16), q_.rearrange("p (a b) -> p a b", a=16),
                     bc_last(rs, 64), ALU.mult)
                qb_ = qn[tt % 2]
                s.tt('dve', qb_, sq, qkg, ALU.mult)
                for half in range(2):
                    pb = s.bank().bitcast(BF16)
                    for j in range(8):
                        g = half * 8 + j
                        s.tr(pb[0:64, j * 128:(j + 1) * 128], qb_[:, g * 64:(g + 1) * 64], idb)
                    evac(qkT[0:64, half * 8:half * 8 + 8, tt * 128:(tt + 1) * 128],
                         pb[0:64, :].rearrange("p (g t) -> p g t", g=8))
                s.copy('act', vaug[:, tt, :].rearrange("p (h d) -> p h d", h=4)[:, :, 0:128],
                       v_.rearrange("p (h d) -> p h d", h=4))
            import os
            stage = int(os.environ.get('ATT_STAGE', '9'))
            if stage < 2:
                return
            SB = banks[0:4]
            ACC = banks[4:8]
            sbi = 0
            pti = 0
            for qb in range(NT):
                for h in range(4):
                    accs = [ACC[(2 * (qb * 4 + h)) % 4], ACC[(2 * (qb * 4 + h) + 1) % 4]]
                    for c in range(2):
                        gq = h * 2 + c
                        gk = 8 + h * 2 + c
                        acc = accs[c]
                        for jg in range(qb // 4 + 1):
                            jbs = list(range(jg * 4, min(jg * 4 + 4, qb + 1)))
                            sbk = SB[sbi % 4]
                            sbi += 1
                            for jj, jb in enumerate(jbs):
                                diag = (jb == qb)
                                s.mm(sbk[:, jj * 128:(jj + 1) * 128], qkT[0:68, gk, jb * 128:(jb + 1) * 128],
                                     qkT[0:68, gq, qb * 128:(qb + 1) * 128], start=True, stop=not diag)
                                if diag:
                                    s.mm(sbk[:, jj * 128:(jj + 1) * 128], idb, mbb, start=False, stop=True)
                            pt = PT[pti % 3]
                            pti += 1
                            n = len(jbs) * 128
                            s.act(pt[:, 0:n], sbk[:, 0:n], AF.Exp)
                            for jj, jb in enumerate(jbs):
                                s.mm(acc[:, 0:129], pt[:, jj * 128:(jj + 1) * 128],
                                     vaug[:, jb, h * 129:(h + 1) * 129], start=(jb == 0), stop=(jb == qb))
                    if stage < 3:
                        continue
                    i2 = (qb * 4 + h) % 2
                    rd = small[:, 56 + 4 * i2:56 + 4 * i2 + 4]
                    s.op('dve', lambda g, a=rd[:, 0:1], b=accs[0][:, 128:129]: g.reciprocal(a, b),
                         [accs[0][:, 128:129]], [rd[:, 0:1]])
                    s.op('dve', lambda g, a=rd[:, 1:2], b=accs[1][:, 128:129]: g.reciprocal(a, b),
                         [accs[1][:, 128:129]], [rd[:, 1:2]])
                    s.tt('dve', rd[:, 1:2], rd[:, 1:2], nlam, ALU.mult)
                    s.ts('dve', t0[i2], accs[0][:, 0:128], rd[:, 0:1], None, ALU.mult)
                    s.stt('dve', ot[i2], accs[1][:, 0:128], rd[:, 1:2], t0[i2], ALU.mult, ALU.add)
                    s.memset('dve', rd[:, 2:3], 0.0)
                    s.act(t0[i2], ot[i2], AF.Square, accum_out=rd[:, 2:3])
                    rstd_from_ssq(rd[:, 2:3], rd[:, 2:3], 1.0 / 128, EPS, rd[:, 3:4])
                    s.ts('dve', rd[:, 2:3], rd[:, 2:3], 1.0 - lambda_init, None, ALU.mult)
                    oab = oa[qb % 2]
                    s.stt('dve', oab[:, h * 128:(h + 1) * 128], ot[i2], rd[:, 2:3], sub_g, ALU.mult, ALU.mult)
                if stage < 4:
                    continue
                oab = oa[qb % 2]
                pb = banks[(qb % 2)].bitcast(BF16)
                for k in range(4):
                    s.tr(pb[:, k * 128:(k + 1) * 128], oab[:, k * 128:(k + 1) * 128], idb)
                o_t = oT[qb % 2]
                evac(o_t, pb[:, 0:512].rearrange("p (k t) -> p k t", k=4))
                s.dma(OT_scr[0, :, qb * 128:(qb + 1) * 128].rearrange("(k p) t -> p k t", p=128), o_t)

        def chunk_core(H, Vd, Rb, Kb, Ab, Bb, Vt, epos, Hst, Y, wk, lowrank, KbM, BbM):
            G = wk['G']
            nq = 4 if lowrank else 2
            m_l2 = bass.AP(m_l.tensor, m_l.offset, [list(m_l.ap[0]), [0, 2], [1, 128]])
            m_sl2 = bass.AP(m_sl.tensor, m_sl.offset, [list(m_sl.ap[0]), [0, 2], [1, 128]])
            for g0 in range(0, H, G):
                hs = list(range(g0, min(g0 + G, H)))
                SL = {h: wk['slots'][j] for j, h in enumerate(hs)}
                ksl = lambda h: slice(h * 64, (h + 1) * 64)
                vsl = lambda h: slice(h * Vd, (h + 1) * Vd)
                pbs = {}
                for h in hs:
                    pb = s.bank()
                    pbs[h] = pb
                    for qi, src in enumerate([Rb, Kb, Ab, Bb][:nq]):
                        s.tr(pb[0:64, qi * 128:(qi + 1) * 128], src[:, ksl(h)], idf)
                for h in hs:
                    evac(SL[h]['FT'][:, 0:nq, :], pbs[h][0:64, 0:nq * 128].rearrange("p (q t) -> p q t", q=nq))
                for h in hs:
                    pb = s.bank()
                    pbs[h] = pb
                    s.tr(pb[0:64, 0:128], epos[:, ksl(h)], idf)
                for h in hs:
                    evac(SL[h]['ET'], pbs[h][0:64, 0:128])
                for h in hs:
                    FT = SL[h]['FT']
                    RT, KT = FT[:, 0, :], FT[:, 1, :]
                    pm = s.bank()
                    pbs[h] = pm
                    s.mm(pm[:, 0:128], KT, RT)
                    if lowrank:
                        AT, BT = FT[:, 2, :], FT[:, 3, :]
                        s.mm(pm[:, 128:256], BT, RT)
                        s.mm(pm[:, 256:384], KT, AT)
                        s.mm(pm[:, 384:512], BT, AT)
                for h in hs:
                    M1 = SL[h]['M1']
                    pm = pbs[h]
                    if lowrank:
                        s.tt('dve', M1[:, 0:2, :], pm[:, 0:256].rearrange("p (a b) -> p a b", a=2), m_l2, ALU.mult)
                        s.tt('dve', M1[:, 2:4, :], pm[:, 256:512].rearrange("p (a b) -> p a b", a=2), m_sl2, ALU.mult)
                    else:
                        s.tt('dve', M1[:, 0, :], pm[:, 0:128], m_l, ALU.mult)
                if lowrank:
                    cur = {}
                    for h in hs:
                        FT = SL[h]['FT']
                        pa = s.bank()
                        pbs[h] = pa
                        s.mm(pa[:, 0:128], FT[:, 2, :], FT[:, 3, :])
                    for h in hs:
                        sl = SL[h]
                        s.tt('dve', sl['P'][0], pbs[h][:, 0:128], m_slT, ALU.mult)
                        s.tt('dve', sl['W'][0], sl['M1'][:, 3, :], idf, ALU.add)
                        cur[h] = [sl['P'][0], sl['M1'][:, 3, :], sl['W'][0]]
                    for i in range(1, 6):
                        for h in hs:
                            Pc, Qc, Wc = cur[h]
                            pp = s.bank()
                            pbs[h] = pp
                            s.mm(pp[:, 0:128], Qc, Pc)
                            if i < 5:
                                s.mm(pp[:, 128:256], Pc, Qc)
                        for h in hs:
                            sl = SL[h]
                            Pn = sl['P'][i % 2]
                            s.copy('act', Pn, pbs[h][:, 0:128])
                            if i < 5:
                                Qn = sl['Q'][i % 2]
                                s.copy('dve', Qn, pbs[h][:, 128:256])
                                cur[h][1] = Qn
                            cur[h][0] = Pn
                        for h in hs:
                            pw = s.bank()
                            pbs[h] = pw
                            s.mm(pw[:, 0:128], cur[h][0], cur[h][2])
                        for h in hs:
                            Wn = SL[h]['W'][i % 2]
                            s.tt('dve', Wn, pbs[h][:, 0:128], cur[h][2], ALU.add)
                            cur[h][2] = Wn
                    for h in hs:
                        pz = s.bank()
                        pbs[h] = pz
                        s.mm(pz[:, 0:Vd], SL[h]['M1'][:, 2, :], Vt[:, vsl(h)])
                    for h in hs:
                        ZA = SL[h]['ZA']
                        s.copy('act', ZA[:, 0:Vd], pbs[h][:, 0:Vd])
                        s.copy('dve', ZA[:, Vd:Vd + 64], Ab[:, ksl(h)])
                    for h in hs:
                        pu = s.bank()
                        pbs[h] = pu
                        s.mm(pu[:, 0:Vd + 64], cur[h][2], SL[h]['ZA'][:, 0:Vd + 64])
                    for h in hs:
                        s.copy('act', SL[h]['UA'][:, 0:Vd + 64], pbs[h][:, 0:Vd + 64])
                    for h in hs:
                        pr = s.bank()
                        pbs[h] = pr
                        s.mm(pr[0:64, 0:128], SL[h]['UA'][:, Vd:Vd + 64], SL[h]['M1'][:, 1, :])
                    for h in hs:
                        s.tt('dve', SL[h]['RtT'], pbs[h][0:64, 0:128], SL[h]['FT'][:, 0, :], ALU.add)
                RtTs = {h: (SL[h]['RtT'] if lowrank else SL[h]['FT'][:, 0, :]) for h in hs}
                for c in range(2):
                    for h in hs:
                        s.copy('dve', SL[h]['Hc'][:, c, :], Hst[:, h, :])
                    if lowrank:
                        for h in hs:
                            pn = s.bank()
                            pbs[h] = pn
                            s.mm(pn[0:64, 0:64], SL[h]['UA'][:, Vd:Vd + 64], BbM[c][:, ksl(h)])
                            s.mm(pn[0:64, 64:64 + Vd], BbM[c][:, ksl(h)], SL[h]['UA'][:, 0:Vd], start=True, stop=False)
                            s.mm(pn[0:64, 64:64 + Vd], KbM[c][:, ksl(h)], Vt[:, vsl(h)], start=False, stop=True)
                        for h in hs:
                            s.tt('dve', SL[h]['BTI'], pbs[h][0:64, 0:64], idf[0:64, 0:64], ALU.add)
                        for h in hs:
                            s.mm(pbs[h][0:64, 256:256 + Vd], SL[h]['BTI'], SL[h]['Hc'][:, c, :], start=True, stop=True)
                        for h in hs:
                            gsc = SL[h]['ET'][:, c * 64 + 63:c * 64 + 64]
                            s.ts('dve', SL[h]['tmpH'], pbs[h][0:64, 256:256 + Vd], gsc, None, ALU.mult)
                            s.stt('dve', Hst[:, h, :], pbs[h][0:64, 64:64 + Vd], gsc, SL[h]['tmpH'], ALU.mult, ALU.add)
                    else:
                        for h in hs:
                            pn = s.bank()
                            pbs[h] = pn
                            s.mm(pn[0:64, 0:Vd], KbM[c][:, ksl(h)], Vt[:, vsl(h)], start=True, stop=True)
                        for h in hs:
                            gsc = SL[h]['ET'][:, c * 64 + 63:c * 64 + 64]
                            s.ts('dve', SL[h]['tmpH'], SL[h]['Hc'][:, c, :], gsc, None, ALU.mult)
                            s.stt('dve', Hst[:, h, :], pbs[h][0:64, 0:Vd], gsc, SL[h]['tmpH'], ALU.mult, ALU.add)
                for h in hs:
                    for c in range(2):
                        s.tt('pool', SL[h]['RtM'][:, c, :], RtTs[h], cst[0:64, C_CM + c * 128:C_CM + (c + 1) * 128], ALU.mult)
                pys = {}
                for h in hs:
                    py = s.bank()
                    pys[h] = py
                    if lowrank:
                        s.mm(py[:, 0:Vd], SL[h]['M1'][:, 1, :], SL[h]['UA'][:, 0:Vd], start=True, stop=False)
                        s.mm(py[:, 0:Vd], SL[h]['M1'][:, 0, :], Vt[:, vsl(h)], start=False, stop=True)
                    else:
                        s.mm(py[:, 0:Vd], SL[h]['M1'][:, 0, :], Vt[:, vsl(h)], start=True, stop=True)
                for h in hs:
                    s.copy('act', Y[:, vsl(h)], pys[h][:, 0:Vd])
                for h in hs:
                    pyh = s.bank()
                    pys[h] = pyh
                    for c in range(2):
                        s.mm(pyh[:, 0:Vd], SL[h]['RtM'][:, c, :], SL[h]['Hc'][:, c, :], start=(c == 0), stop=(c == 1))
                for h in hs:
                    s.tt('dve', Y[:, vsl(h)], pys[h][:, 0:Vd], Y[:, vsl(h)], ALU.add)

        def alloc_wk(Vd, lowrank, G=4):
            wk = {'G': G, 'slots': []}
            for j in range(G):
                sl = {}
                sl['FT'] = A.get([4 if lowrank else 2, 128], F32, parts=64)
                sl['ET'] = A.get([128], F32, parts=64)
                sl['M1'] = A.get([4 if lowrank else 1, 128])
                sl['Hc'] = A.get([2, Vd], F32, parts=64)
                sl['tmpH'] = A.get([Vd], F32, parts=64)
                sl['RtM'] = A.get([2, 128], F32, parts=64)
                if lowrank:
                    sl['P'] = [A.get([128]) for _ in range(2)]
                    sl['Q'] = [A.get([128]) for _ in range(2)]
                    sl['W'] = [A.get([128]) for _ in range(2)]
                    sl['ZA'] = A.get([Vd + 64])
                    sl['UA'] = A.get([Vd + 64])
                    sl['RtT'] = A.get([128], F32, parts=64)
                    sl['BTI'] = A.get([64], F32, parts=64)
                wk['slots'].append(sl)
            return wk

        def gammas(H, lw, gam):
            pg = s.bank()
            for h in range(H):
                s.mm(pg[0:64, h * 32:(h + 1) * 32], lw[:, h * 64:(h + 1) * 64], cst[:, C_CI:C_CI + 32])
            s.act(gam, pg[0:64, 0:32 * H].rearrange("p (h c) -> p h c", h=H)[:, :, 0:2], AF.Exp)

        def store_T(idx, src_bf, tt, oTb):
            pb = s.bank().bitcast(BF16)
            for k in range(4):
                s.tr(pb[:, k * 128:(k + 1) * 128], src_bf[:, k * 128:(k + 1) * 128], idb)
            evac(oTb, pb[:, 0:512].rearrange("p (k t) -> p k t", k=4))
            s.dma(OT_scr[idx, :, tt * 128:(tt + 1) * 128].rearrange("(k p) t -> p k t", p=128), oTb)

        def phase_rwkv(l):
            A.reset()
            wk = alloc_wk(64, True)
            mu = A.get([1792])
            wup = A.get([512], F32, parts=64)
            aup = A.get([512], F32, parts=64)
            gup = A.get([512])
            kkb = A.get([512])
            kab = A.get([512])
            rkb = A.get([512])
            lng = A.get([512])
            lnb = A.get([512])
            Hst = A.get([8, 64], F32, parts=64)
            cur = [A.get([1792]) for _ in range(2)]
            prv = A.get([1792])
            lora = A.get([256])
            sgT = A.get([128])
            lw = A.get([512])
            av = A.get([512])
            gv = A.get([512])
            epos = A.get([512])
            eneg = A.get([512])
            eprv = A.get([512])
            kkn = A.get([512])
            kmod = A.get([512])
            Rb = A.get([512])
            Ab = A.get([512])
            Bb = A.get([512])
            Kb = A.get([512])
            KbM = [A.get([512]) for _ in range(2)]
            BbM = [A.get([512]) for _ in range(2)]
            Y = A.get([512])
            t1 = A.get([512])
            t2 = A.get([512])
            st8 = A.get([64])
            orb = [A.get([512], BF16) for _ in range(2)]
            oTb = [A.get([4, 128], BF16) for _ in range(2)]
            s.dma(mu, bc_part(W['rwkv_mu'][l:l + 1, :]))
            s.dma(wup, W['rwkv_w_up'][l])
            s.dma(aup, W['rwkv_a_up'][l])
            s.dma(gup, W['rwkv_g_up'][l])
            s.dma(rows[0:1, 0:512], W['rwkv_w0'][l:l + 1, :])
            s.dma(rows[0:1, 512:1024], W['rwkv_a0'][l:l + 1, :])
            for dst, nm in ((kkb, 'rwkv_k_k'), (kab, 'rwkv_k_a'), (rkb, 'rwkv_r_k'), (lng, 'rwkv_lnx_g'),
                            (lnb, 'rwkv_lnx_b')):
                s.dma(dst, bc_part(W[nm][l:l + 1, :]))
            s.memset('dve', Hst, 0.0)
            v3 = lambda ap: ap.rearrange("p (h k) -> p h k", h=8)
            import os
            for tt in range(int(os.environ.get('RW_TILES', NT))):
                cu = cur[tt % 2]
                r0 = tt * 128
                s.dma(cu, P_scr[r0:r0 + 128, 1536:3328])
                if tt == 0:
                    s.memset('dve', prv[0:1, :], 0.0)
                    s.dma(prv[1:128, :], P_scr[0:127, 1536:3328])
                else:
                    s.dma(prv, P_scr[r0 - 1:r0 + 127, 1536:3328])
                s.tt('dve', prv, prv, cu, ALU.subtract)
                s.tt('dve', prv, prv, mu, ALU.mult)
                s.tt('dve', cu, cu, prv, ALU.add)
                r_, k_, v_ = cu[:, 0:512], cu[:, 512:1024], cu[:, 1024:1536]
                pl = s.bank()
                s.tr(pl[0:64, 0:128], cu[:, 1536:1600], idf)
                s.tr(pl[0:64, 128:256], cu[:, 1600:1664], idf)
                s.tr(pl[:, 256:384], cu[:, 1664:1792], idf)
                s.act(lora[0:64, 0:128], pl[0:64, 0:128], AF.Tanh)
                s.copy('dve', lora[0:64, 128:256], pl[0:64, 128:256])
                s.act(sgT, pl[:, 256:384], AF.Sigmoid)
                px = s.bank()
                s.mm(px, lora[0:64, 0:128], wup, start=True, stop=False)
                s.mm(px, ones[0:1, 0:128], rows[0:1, 0:512], start=False, stop=True)
                s.act(lw, px, AF.Sigmoid)
                s.ts('dve', lw, lw, -math.exp(-0.5), None, ALU.mult)
                pa = s.bank()
                s.mm(pa, lora[0:64, 128:256], aup, start=True, stop=False)
                s.mm(pa, ones[0:1, 0:128], rows[0:1, 512:1024], start=False, stop=True)
                s.act(av, pa, AF.Sigmoid)
                pg = s.bank()
                s.mm(pg, sgT, gup)
                s.copy('dve', gv, pg)
                pc = s.bank()
                s.mm(pc, m_l, lw)
                s.act(epos, pc, AF.Exp)
                s.act(eneg, pc, AF.Exp, scale=-1.0)
                s.tt('dve', t1, pc, lw, ALU.subtract)
                s.act(eprv, t1, AF.Exp)
                s.tt('dve', kkn, k_, kkb, ALU.mult)
                s.tt('dve', t1, kkn, kkn, ALU.mult)
                s.reduce('dve', st8[:, 0:8], v3(t1), ALU.add)
                s.act(st8[:, 8:16], st8[:, 0:8], AF.Sqrt)
                s.ts('dve', st8[:, 8:16], st8[:, 8:16], 1e-12, None, ALU.max)
                s.op('dve', lambda g: g.reciprocal(st8[:, 16:24], st8[:, 8:16]), [st8[:, 8:16]], [st8[:, 16:24]])
                s.tt('dve', v3(kkn), v3(kkn), bc_last(st8[:, 16:24], 64), ALU.mult)
                s.stt('dve', t2, av, -1.0, kab, ALU.add, ALU.mult)
                s.stt('dve', kmod, t2, 1.0, k_, ALU.add, ALU.mult)
                s.tt('dve', Rb, r_, epos, ALU.mult)
                s.stt('dve', Ab, kkn, -1.0, eprv, ALU.mult, ALU.mult)
                s.tt('dve', t2, kkn, av, ALU.mult)
                s.tt('dve', Bb, t2, eneg, ALU.mult)
                s.tt('dve', Kb, kmod, eneg, ALU.mult)
                import os
                rst = int(os.environ.get('RW_STAGE', '9'))
                if rst < 2:
                    continue
                if rst < 3:
                    continue
                for c in range(2):
                    s.ts('dve', KbM[c], Kb, cst[:, C_CI + c:C_CI + c + 1], None, ALU.mult)
                    s.ts('dve', BbM[c], Bb, cst[:, C_CI + c:C_CI + c + 1], None, ALU.mult)
                chunk_core(8, 64, Rb, Kb, Ab, Bb, v_, epos, Hst, Y, wk, True, KbM, BbM)
                if rst < 4:
                    continue
                s.reduce('dve', st8[:, 24:32], v3(Y), ALU.add)
                s.tt('dve', t1, Y, Y, ALU.mult)
                s.reduce('dve', st8[:, 32:40], v3(t1), ALU.add)
                s.ts('dve', st8[:, 24:32], st8[:, 24:32], 1.0 / 64, None, ALU.mult)
                s.tt('dve', st8[:, 40:48], st8[:, 24:32], st8[:, 24:32], ALU.mult)
                s.stt('dve', st8[:, 32:40], st8[:, 32:40], 1.0 / 64, st8[:, 40:48], ALU.mult, ALU.subtract)
                rstd_from_ssq(st8[:, 48:56], st8[:, 32:40], 1.0, 64e-5, st8[:, 56:64])
                s.tt('dve', v3(t1), v3(Y), bc_last(st8[:, 24:32], 64), ALU.subtract)
                s.tt('dve', v3(t1), v3(t1), bc_last(st8[:, 48:56], 64), ALU.mult)
                s.tt('dve', t1, t1, lng, ALU.mult)
                s.tt('dve', t1, t1, lnb, ALU.add)
                s.tt('dve', t2, r_, kmod, ALU.mult)
                s.tt('dve', t2, t2, rkb, ALU.mult)
                s.reduce('dve', st8[:, 0:8], v3(t2), ALU.add)
                s.tt('dve', v3(t2), v3(v_), bc_last(st8[:, 0:8], 64), ALU.mult)
                s.tt('dve', t1, t1, t2, ALU.add)
                ob = orb[tt % 2]
                s.tt('dve', ob, t1, gv, ALU.mult)
                store_T(1, ob, tt, oTb[tt % 2])

        def phase_gla(l):
            A.reset()
            wk = alloc_wk(128, False)
            aup = A.get([256], F32, parts=16)
            ng = A.get([512])
            Hst = A.get([4, 128], F32, parts=64)
            cur = [A.get([1552]) for _ in range(2)]
            adT = A.get([128], F32, parts=16)
            ll = A.get([256])
            epos = A.get([256])
            eneg = A.get([256])
            Rb = A.get([256])
            Kb = A.get([256])
            KbM = [A.get([256]) for _ in range(2)]
            Y = A.get([512])
            t1 = A.get([512])
            sg = A.get([512])
            st4 = A.get([16])
            ogb = [A.get([512], BF16) for _ in range(2)]
            oTb = [A.get([4, 128], BF16) for _ in range(2)]
            s.dma(aup, W['gla_alpha_up'][l])
            s.dma(rows[0:1, 1024:1280], W['gla_alpha_b'][l:l + 1, :])
            s.dma(ng, bc_part(W['gla_norm_g4'][l:l + 1, :]))
            s.memset('dve', Hst, 0.0)
            v4 = lambda ap: ap.rearrange("p (h k) -> p h k", h=4)
            for tt in range(NT):
                cu = cur[tt % 2]
                r0 = tt * 128
                s.dma(cu, P_scr[r0:r0 + 128, 3328:4880])
                q_, k_, v_, gate = cu[:, 0:256], cu[:, 256:512], cu[:, 512:1024], cu[:, 1040:1552]
                pl = s.bank()
                s.tr(pl[0:16, 0:128], cu[:, 1024:1040], idf)
                s.copy('dve', adT, pl[0:16, 0:128])
                pz = s.bank()
                s.mm(pz[:, 0:256], adT, aup, start=True, stop=False)
                s.mm(pz[:, 0:256], ones[0:1, 0:128], rows[0:1, 1024:1280], start=False, stop=True)
                s.act(ll, pz[:, 0:256], AF.Sigmoid)
                s.act(ll, ll, AF.Ln)
                s.ts('dve', ll, ll, 1.0 / 16, None, ALU.mult)
                pc = s.bank()
                s.mm(pc[:, 0:256], m_l, ll)
                s.act(epos, pc[:, 0:256], AF.Exp)
                s.act(eneg, pc[:, 0:256], AF.Exp, scale=-1.0)
                s.stt('dve', Rb, q_, 0.125, epos, ALU.mult, ALU.mult)
                s.tt('pool', Kb, k_, eneg, ALU.mult)
                for c in range(2):
                    s.ts('dve', KbM[c], Kb, cst[:, C_CI + c:C_CI + c + 1], None, ALU.mult)
                chunk_core(4, 128, Rb, Kb, None, None, v_, epos, Hst, Y, wk, False, KbM, None)
                s.tt('pool', t1, Y, Y, ALU.mult)
                s.reduce('dve', st4[:, 0:4], v4(t1), ALU.add)
                rstd_from_ssq(st4[:, 4:8], st4[:, 0:4], 1.0 / 128, EPS, st4[:, 8:12])
                s.tt('dve', v4(t1), v4(Y), bc_last(st4[:, 4:8], 128), ALU.mult)
                s.tt('pool', t1, t1, ng, ALU.mult)
                s.act(sg, gate, AF.Silu)
                ob = ogb[tt % 2]
                s.tt('dve', ob, t1, sg, ALU.mult)
                store_T(2, ob, tt, oTb[tt % 2])

        def phase_merge(l, xsrc):
            A.reset()
            gt1 = modbc[:, 2 * D:3 * D]
            gw = A.get([8, 3072], BF16)
            pw = A.get([12, 1024], BF16)
            ow = A.get([8, 1024], BF16)
            oTs = [A.get([12, 512], BF16) for _ in range(2)]
            mT = A.get([8, 512], BF16)
            macc = A.get([512])
            sgm = A.get([512])
            xt = [A.get([1024]) for _ in range(2)]
            tmp = A.get([512])
            for j in range(6):
                s.dma(gw[:, :, j * 512:(j + 1) * 512],
                      W['w_in'][l, :, NPROJ + j * 512:NPROJ + (j + 1) * 512].rearrange("(k p) n -> p k n", p=128),
                      q='pool')
            for bi, nm in enumerate(('proj_attn', 'proj_rwkv', 'proj_gla')):
                s.dma(pw[:, bi * 4:(bi + 1) * 4, :], W[nm][l].rearrange("(k p) n -> p k n", p=128), q='pool')
            s.dma(ow, W['w_out'][l].rearrange("(k p) n -> p k n", p=128), q='pool')
            for tg in range(4):
                ts_ = slice(tg * 512, (tg + 1) * 512)
                oT = oTs[tg % 2]
                for bi in range(3):
                    s.dma(oT[:, bi * 4:(bi + 1) * 4, :], OT_scr[bi, :, ts_].rearrange("(k p) t -> p k t", p=128))
                for dc in range(8):
                    for bi in range(3):
                        pg = s.bank()
                        for k in range(8):
                            s.mm(pg, gw[:, k, bi * 1024 + dc * 128:bi * 1024 + (dc + 1) * 128], hT[:, k, ts_],
                                 start=(k == 0), stop=(k == 7))
                        pp = s.bank()
                        for k in range(4):
                            s.mm(pp, pw[:, bi * 4 + k, dc * 128:(dc + 1) * 128], oT[:, bi * 4 + k, :],
                                 start=(k == 0), stop=(k == 3))
                        s.act(sgm, pg, AF.Sigmoid)
                        if bi == 0:
                            s.tt('dve', macc, pp, sgm, ALU.mult)
                        else:
                            s.tt('dve', sgm, pp, sgm, ALU.mult)
                            if bi == 1:
                                s.tt('dve', macc, macc, sgm, ALU.add)
                            else:
                                s.tt('dve', mT[:, dc, :], macc, sgm, ALU.add)
                for t4 in range(4):
                    tt = tg * 4 + t4
                    x_ = xt[tt % 2]
                    s.dma(x_, xsrc[tt * 128:(tt + 1) * 128, :])
                    for half in range(2):
                        hs = slice(half * 512, (half + 1) * 512)
                        po = s.bank()
                        for k in range(8):
                            s.mm(po, mT[:, k, t4 * 128:(t4 + 1) * 128], ow[:, k, hs], start=(k == 0), stop=(k == 7))
                        s.tt('dve', tmp, po, gt1[:, hs], ALU.mult)
                        s.tt('dve', x_[:, hs], x_[:, hs], tmp, ALU.add)
                    s.dma(out[tt * 128:(tt + 1) * 128, :], x_)

        def phase_moe(l):
            A.reset()
            gt2 = modbc[:, 5 * D:6 * D]
            slots_i = A.get([NT, 2], I32)
            wts2 = A.get([NT, 2])
            zt = A.get([D], BF16)
            mark = A.off
            s.memset('dve', zt, 0.0)
            XGv = XG.rearrange("(p r) d -> p r d", p=128)
            RP = NE * CAP // 128
            for r0 in range(0, RP, 8):
                s.dma(XGv[:, r0:r0 + 8, :], bass.AP(zt.tensor, zt.offset, [list(zt.ap[0]), [0, 8], [1, D]]))
            rw = A.get([8, 64])
            lg_all = A.get([NT, 36])
            hTf = [A.get([8, 128]) for _ in range(2)]
            s.memset('dve', rw, 0.0)
            s.memset('dve', rows[0:1, 1280:1344], 0.0)
            s.dma(rw[:, :, 0:36], W['router_w'][l].rearrange("(k p) n -> p k n", p=128))
            s.dma(rows[0:1, 1280:1316], W['router_b'][l:l + 1, :])
            base = A.off
            A2 = Arena(A.ap[:, base // 4:], A.nbytes - base)
            _norm_with(A2, l, rw, lg_all, hTf)
            G3 = lambda: A2.get([NT, 4])
            E3 = lambda: A2.get([NT, 32])
            T1 = lambda: A2.get([NT])
            oh, pen, ge = G3(), G3(), G3()
            em, m1, m2, am, pos, ovf = E3(), E3(), E3(), E3(), E3(), E3()
            carry_all = A2.get([NT + 1, 32])
            gmax, gs, gp, v1, v2, dd, ex, w1, w2 = T1(), T1(), T1(), T1(), T1(), T1(), T1(), T1(), T1()
            slots_f = A2.get([NT, 2])
            hrow = [A2.get([D], BF16) for _ in range(2)]
            gl = lg_all[:, :, 0:4]
            el = lg_all[:, :, 4:36]
            s.reduce('dve', gmax, gl, ALU.max)
            s.tt('dve', oh, gl, bc_last(gmax, 4), ALU.is_equal)
            s.tt('dve', ge, gl, bc_last(gmax, 4), ALU.subtract)
            s.act(ge, ge, AF.Exp)
            s.reduce('dve', gs, ge, ALU.add)
            s.op('dve', lambda g: g.reciprocal(gp, gs), [gs], [gp])
            s.ts('dve', pen, oh, 1e9, -1e9, ALU.mult, ALU.add)
            s.copy('dve', em, el)
            em64 = em.rearrange("p t (g e) -> p (t g) e", g=4)
            s.tt('dve', em64, em64, bc_last(pen.rearrange("p t g -> p (t g)"), 8), ALU.add)
            s.reduce('dve', v1, em, ALU.max)
            s.tt('dve', m1, em, bc_last(v1, 32), ALU.is_equal)
            s.stt('dve', em, m1, -1e9, em, ALU.mult, ALU.add)
            s.reduce('dve', v2, em, ALU.max)
            s.tt('dve', m2, em, bc_last(v2, 32), ALU.is_equal)
            s.tt('dve', dd, v2, v1, ALU.subtract)
            s.act(ex, dd, AF.Exp)
            s.ts('dve', w1, ex, 1.0, None, ALU.add)
            s.op('dve', lambda g: g.reciprocal(w1, w1), [w1], [w1])
            s.tt('dve', w2, ex, w1, ALU.mult)
            s.tt('dve', wts2[:, :, 0], w1, gp, ALU.mult)
            s.tt('dve', wts2[:, :, 1], w2, gp, ALU.mult)
            s.tt('dve', am, m1, m2, ALU.add)
            amf = am.rearrange("p t e -> p (t e)")
            pp = s.bank()
            s.mm(pp, cst[:, C_TRI:C_TRI + 128], amf)
            pt = s.bank()
            s.mm(pt, cst[:, C_ONE:C_ONE + 128], amf)
            s.memset('dve', carry_all[:, 0, :], 0.0)
            for tt in range(NT):
                s.tt('dve', carry_all[:, tt + 1, :], carry_all[:, tt, :], pt[:, tt * 32:(tt + 1) * 32], ALU.add)
            carry = carry_all[:, NT, :]
            s.tt('dve', pos.rearrange("p t e -> p (t e)"), pp, carry_all[:, 0:NT, :].rearrange("p t e -> p (t e)"), ALU.add)
            s.ts('dve', ovf, pos, float(CAP), 1e6, ALU.is_ge, ALU.mult)
            ecap = cst[:, C_ECAP:C_ECAP + 32]
            s.tt('dve', pos, pos, bass.AP(ecap.tensor, ecap.offset, [list(ecap.ap[0]), [0, NT], [1, 32]]), ALU.add)
            s.tt('dve', pos, pos, ovf, ALU.add)
            s.tt('dve', m1, m1, pos, ALU.mult)
            s.tt('dve', m2, m2, pos, ALU.mult)
            s.reduce('dve', slots_f[:, :, 0], m1, ALU.add)
            s.reduce('dve', slots_f[:, :, 1], m2, ALU.add)
            s.copy('dve', slots_i, slots_f)
            for tt in range(NT):
                hr = hrow[tt % 2]
                s.dma(hr, H2_scr[tt * 128:(tt + 1) * 128, :])
                for j in range(2):
                    s.idma(XG, hr, slots_i[:, tt, j:j + 1], True, NE * CAP)
            if 'cnt' in DBG:
                s.dma(DBG['cnt'][l], carry)
            A.off = mark
            NB = 2
            wg = [A.get([8, 512], BF16) for _ in range(NB)]
            wu = [A.get([8, 512], BF16) for _ in range(NB)]
            wd = [A.get([4, 1024], BF16) for _ in range(NB)]
            RT_ = CAP // 128
            xg = [A.get([RT_, D], BF16) for _ in range(2)]
            xT = [A.get([8, CAP], BF16) for _ in range(2)]
            hid = [A.get([4, CAP], BF16) for _ in range(2)]
            sg = [A.get([CAP], BF16) for _ in range(2)]
            yst = [A.get([D]) for _ in range(2)]
            y12 = [[A.get([D]) for _ in range(2)] for _ in range(2)]
            xts = [A.get([D]) for _ in range(2)]
            yi = 0
            for e in range(NE):
                b = e % NB
                s.dma(wg[b], W['exp_w_gate'][l, e].rearrange("(k p) n -> p k n", p=128), q='pool')
                s.dma(wu[b], W['exp_w_up'][l, e].rearrange("(k p) n -> p k n", p=128), q='pool')
                s.dma(wd[b], W['exp_w_down'][l, e].rearrange("(k p) n -> p k n", p=128), q='pool')
                xg_ = xg[e % 2]
                s.dma(xg_, XG[e * CAP:(e + 1) * CAP, :].rearrange("(r p) d -> p r d", p=128))
                xT_ = xT[e % 2]
                for r in range(RT_):
                    pb = s.bank().bitcast(BF16)
                    for k in range(8):
                        s.tr(pb[:, k * 128:(k + 1) * 128], xg_[:, r, k * 128:(k + 1) * 128], idb)
                    evac(xT_[:, :, r * 128:(r + 1) * 128], pb.rearrange("p (k t) -> p k t", k=8))
                hid_ = hid[e % 2]
                for fc in range(4):
                    fs = slice(fc * 128, (fc + 1) * 128)
                    pg = s.bank()
                    for k in range(8):
                        s.mm(pg[:, 0:CAP], wg[b][:, k, fs], xT_[:, k, :], start=(k == 0), stop=(k == 7))
                    pu = s.bank()
                    for k in range(8):
                        s.mm(pu[:, 0:CAP], wu[b][:, k, fs], xT_[:, k, :], start=(k == 0), stop=(k == 7))
                    sgb = sg[fc % 2]
                    s.act(sgb, pg[:, 0:CAP], AF.Silu)
                    s.tt('dve', hid_[:, fc, :], pu[:, 0:CAP], sgb, ALU.mult)
                for r in range(RT_):
                    ys = yst[yi % 2]
                    yi += 1
                    for half in range(2):
                        hs = slice(half * 512, (half + 1) * 512)
                        py = s.bank()
                        for k in range(4):
                            s.mm(py, hid_[:, k, r * 128:(r + 1) * 128], wd[b][:, k, hs], start=(k == 0), stop=(k == 3))
                        evac(ys[:, hs], py)
                    s.dma(YG[e * CAP + r * 128:e * CAP + (r + 1) * 128, :], ys)
            for tt in range(NT):
                ya, yb = y12[tt % 2]
                x_ = xts[tt % 2]
                s.dma(x_, out[tt * 128:(tt + 1) * 128, :])
                s.memset('pool', ya, 0.0)
                s.memset('pool', yb, 0.0)
                s.idma(ya, YG, slots_i[:, tt, 0:1], False, NE * CAP)
                s.idma(yb, YG, slots_i[:, tt, 1:2], False, NE * CAP)
                s.ts('dve', ya, ya, wts2[:, tt, 0:1], None, ALU.mult)
                s.stt('dve', ya, yb, wts2[:, tt, 1:2], ya, ALU.mult, ALU.add)
                s.tt('dve', ya, ya, gt2, ALU.mult)
                s.tt('dve', x_, x_, ya, ALU.add)
                s.dma(out[tt * 128:(tt + 1) * 128, :], x_)

        def _norm_with(A2, l, rw, lg_all, hTf):
            nonlocal A
            saved = A
            A = A2
            try:
                phase_norm(l, 1, out, router=(rw, lg_all, hTf))
            finally:
                A = saved

        order = ['mod', 'norm', 'proj', 'attn', 'rwkv', 'gla', 'merge', 'moe']
        stop_l, stop_p = (nlayers - 1, 'moe') if upto == 'all' else upto
        done = False
        for l in range(nlayers):
            for ph in order:
                if phases is not None and (l, ph) not in phases:
                    continue
                if ph == 'mod':
                    phase_mod(l)
                    dump('mod', modbc[0:1, :])
                elif ph == 'norm':
                    phase_norm(l, 0, x_in if l == 0 else out)
                elif ph == 'proj':
                    phase_proj(l)
                elif ph == 'attn':
                    phase_attn(l)
                elif ph == 'rwkv':
                    phase_rwkv(l)
                elif ph == 'gla':
                    phase_gla(l)
                elif ph == 'merge':
                    phase_merge(l, x_in if l == 0 else out)
                elif ph == 'moe':
                    phase_moe(l)
                if (l, ph) == (stop_l, stop_p):
                    done = True
                    break
            if done:
                break
        if 'P' in DBG:
            s.dma(DBG['P'], P_scr)
        if 'OT' in DBG:
            s.dma(DBG['OT'], OT_scr, q='pool')
        if 'hT' in DBG:
            hf = arena_t[:, 0:4096]
            for k in range(8):
                for g in range(0, S, 4096 // 1):
                    pass
        s.finish()
        print("instructions:", s.ninst, "sbuf left:", nc.sbuf_bytes_remaining, flush=True)
    return nc


def prep_weights(inp):
    f = lambda a: np.ascontiguousarray(np.asarray(a, dtype=np.float32))
    w = {}
    for k in ('ada_w', 'ada_b', 'norm1_g', 'norm2_g', 'w_in', 'attn_subln_g', 'rwkv_mu', 'rwkv_w_up', 'rwkv_w0',
              'rwkv_a_up', 'rwkv_a0', 'rwkv_g_up', 'rwkv_k_k', 'rwkv_k_a', 'rwkv_lnx_g', 'rwkv_lnx_b',
              'gla_alpha_up', 'gla_alpha_b', 'proj_attn', 'proj_rwkv', 'proj_gla', 'w_out', 'exp_w_gate',
              'exp_w_up', 'exp_w_down'):
        w[k] = f(inp[k])
    w['attn_qkg'] = f(np.concatenate([np.tile(inp['attn_qn_g'], (1, 8)), np.tile(inp['attn_kn_g'], (1, 8))], axis=1))
    w['attn_lambda'] = f(np.reshape(inp['attn_lambda'], (DEPTH, 256)))
    w['rwkv_r_k'] = f(np.reshape(inp['rwkv_r_k'], (DEPTH, 512)))
    w['gla_norm_g4'] = f(np.tile(inp['gla_norm_g'], (1, 4)))
    w['router_w'] = f(np.concatenate([inp['router_grp_w'], inp['router_exp_w']], axis=2))
    w['router_b'] = f(np.concatenate([inp['router_grp_b'], inp['router_exp_b']], axis=1))
    return w


def kernel(**inputs):
    x = np.asarray(inputs['x'], dtype=np.float32)
    c = np.asarray(inputs['c'], dtype=np.float32)
    w = prep_weights(inputs)
    cst, aug = make_consts()
    nc = build_program()
    in_maps = []
    for b in range(8):
        m = dict(w)
        m['x'] = np.ascontiguousarray(x[b])
        m['c'] = np.ascontiguousarray(c[b].reshape(8, 128).T)
        m['consts'] = cst
        m['aug'] = aug
        in_maps.append(m)
    res = run_bass_kernel_spmd(nc, in_maps, core_ids=list(range(8)))
    return np.stack([np.asarray(r['out'], dtype=np.float32) for r in res.results], axis=0)
```

```python
import contextlib
import math
import numpy as np
import concourse.bass as bass
import concourse.mybir as mybir
from concourse.bass_utils import run_bass_kernel_spmd

F32 = mybir.dt.float32
BF16 = mybir.dt.bfloat16
I32 = mybir.dt.int32
AF = mybir.ActivationFunctionType
ALU = mybir.AluOpType
AX = mybir.AxisListType

D = 1024
S = 2048
NT = S // 128
DEPTH = 2
N_IN = 7952
NPROJ = 4880
EPS = 1e-6
SAME_ENGINE_SYNC = True


def _region(ap):
    t = ap.tensor
    a = ap.ap
    off = int(ap.offset)
    es = mybir.dt.size(ap.dtype)
    tn = type(t).__name__
    if 'DRam' in tn:
        ext = sum(int(s) * (int(c) - 1) for s, c in a) + 1
        return (t.name, 0, 1, off * es, (off + ext) * es)
    if 'PSum' in tn:
        return (t.name, 0, 128, 0, 1 << 20)
    ps = int(a[0][0])
    npart = int(a[0][1])
    if ps == 0:
        ps = 1 << 40
    p0 = off // ps
    f0 = off % ps
    ext = sum(int(s) * (int(c) - 1) for s, c in a[1:]) + 1
    return (t.name, p0, p0 + npart, f0 * es, (f0 + ext) * es)


def _ovl(r, q):
    return r[1] < q[2] and q[1] < r[2] and r[3] < q[4] and q[3] < r[4]


def _contains(big, small):
    return big[1] <= small[1] and small[2] <= big[2] and big[3] <= small[3] and small[4] <= big[4]


class Sched:
    def __init__(self, nc, stack):
        self.nc = nc
        self.engs = {'pe': nc.tensor, 'act': nc.scalar, 'dve': nc.vector, 'pool': nc.gpsimd, 'sp': nc.sync}
        self.sem = {}
        self.cnt = {}
        for e in ['pe', 'act', 'dve', 'pool']:
            self.sem[e] = stack.enter_context(nc.semaphore('s_' + e))
            self.cnt[e] = 0
        self.ndsem = {'sp': 16, 'pool': 8}
        for q in ['sp', 'pool']:
            for j in range(self.ndsem[q]):
                k = ('d', q, j)
                self.sem[k] = stack.enter_context(nc.semaphore('d_%s_%d' % (q, j)))
                self.cnt[k] = 0
        self.drr = {'sp': 0, 'pool': 0}
        self.known = {e: {} for e in self.engs}
        self.recs = {}
        self.npsum = 0
        self.banks = []
        self.ninst = 0
        import os
        self.max_ops = int(os.environ.get('MAX_OPS', '1000000000'))
        self.force_dma = False
        self.log_ops = bool(os.environ.get('LOG_OPS'))

    def _deps(self, reads, writes):
        deps = {}
        for ap in reads:
            r = _region(ap)
            for rec in self.recs.get(r[0], ()):
                if rec[1] and _ovl(rec[0], r):
                    if deps.get(rec[2], 0) < rec[3]:
                        deps[rec[2]] = rec[3]
        for ap in writes:
            r = _region(ap)
            for rec in self.recs.get(r[0], ()):
                if _ovl(rec[0], r):
                    if deps.get(rec[2], 0) < rec[3]:
                        deps[rec[2]] = rec[3]
        return deps

    def _record(self, reads, writes, k, v, noprune=False):
        for ap in reads:
            r = _region(ap)
            lst = self.recs.setdefault(r[0], [])
            for rec in lst:
                if (not rec[1]) and rec[2] == k and rec[0] == r:
                    rec[3] = max(rec[3], v)
                    break
            else:
                lst.append([r, False, k, v])
        for ap in writes:
            r = _region(ap)
            lst = self.recs.setdefault(r[0], [])
            if not noprune:
                lst[:] = [rec for rec in lst if not _contains(r, rec[0])]
            lst.append([r, True, k, v])

    def _waits(self, e, deps):
        eng = self.engs[e]
        for k, v in deps.items():
            if self.known[e].get(k, 0) >= v:
                continue
            if k == e:
                if e == 'pe' or not SAME_ENGINE_SYNC or v > self.cnt[e]:
                    continue
            eng.wait_ge(self.sem[k], v)
            self.known[e][k] = v
            self.ninst += 1

    def op(self, e, fn, reads, writes, inc=True):
        if self.ninst >= self.max_ops:
            return None
        if self.log_ops:
            import inspect
            fr = inspect.stack()
            print('OP', self.ninst, e, [f.lineno for f in fr[1:4]], flush=True)
        pr = [r for r in reads if 'PSum' in type(r.tensor).__name__]
        if pr:
            reads = [r for r in reads if 'PSum' not in type(r.tensor).__name__]
            writes = list(writes) + pr
        self._waits(e, self._deps(reads, writes))
        ins = fn(self.engs[e])
        self.ninst += 1
        if inc:
            self.cnt[e] += 1
            ins.then_inc(self.sem[e], 1)
            val = self.cnt[e]
        else:
            val = self.cnt[e] + 1
        self._record(reads, writes, e, val)
        return ins

    def dma(self, out, in_, q='sp', **kw):
        if self.ninst >= self.max_ops and not self.force_dma:
            return
        if self.log_ops:
            import inspect
            fr = inspect.stack()
            print('DMA', self.ninst, q, [f.lineno for f in fr[1:3]], flush=True)
        self._waits(q, self._deps([in_], [out]))
        j = self.drr[q]
        self.drr[q] = (j + 1) % self.ndsem[q]
        k = ('d', q, j)
        if self.known[q].get(k, 0) < self.cnt[k]:
            self.engs[q].wait_ge(self.sem[k], self.cnt[k])
            self.known[q][k] = self.cnt[k]
            self.ninst += 1
        self.engs[q].dma_start(out=out, in_=in_, **kw).then_inc(self.sem[k], 16)
        self.ninst += 1
        self.cnt[k] += 16
        self._record([in_], [out], k, self.cnt[k])

    def idma(self, out, in_, idx, scatter, nrows):
        q = 'pool'
        deps = self._deps([in_, idx], [out])
        if scatter:
            deps = {k_: v_ for k_, v_ in deps.items() if not (isinstance(k_, tuple) and k_[1] == 'pool')}
        self._waits(q, deps)
        j = self.drr[q]
        self.drr[q] = (j + 1) % self.ndsem[q]
        k = ('d', q, j)
        if self.known[q].get(k, 0) < self.cnt[k]:
            self.engs[q].wait_ge(self.sem[k], self.cnt[k])
            self.known[q][k] = self.cnt[k]
        off = bass.IndirectOffsetOnAxis(ap=idx, axis=0)
        if not hasattr(self, 'bc_regs'):
            self.bc_regs = {}
        if nrows not in self.bc_regs:
            r = self.engs[q].alloc_register("bc%d" % nrows)
            self.engs[q].reg_mov(r, nrows - 1)
            self.bc_regs[nrows] = r
        bcr = self.bc_regs[nrows]
        if scatter:
            ins = self.engs[q].indirect_dma_start(out=out, out_offset=off, in_=in_, in_offset=None,
                                                  bounds_check=bcr, oob_is_err=False)
        else:
            ins = self.engs[q].indirect_dma_start(out=out, out_offset=None, in_=in_, in_offset=off,
                                                  bounds_check=bcr, oob_is_err=False)
        ins.then_inc(self.sem[k], 16)
        self.ninst += 1
        self.cnt[k] += 16
        self._record([in_, idx], [out], k, self.cnt[k], noprune=scatter)

    def finish(self):
        sp = self.engs['sp']
        for k, v in self.cnt.items():
            if v > 0:
                sp.wait_ge(self.sem[k], v)

    def mm(self, out, lhsT, rhs, start=True, stop=True):
        return self.op('pe', lambda e: e.matmul(out, lhsT, rhs, start=start, stop=stop),
                       [lhsT, rhs], [out], inc=stop)

    def tr(self, out, in_, ident):
        return self.op('pe', lambda e: e.transpose(out, in_, ident), [in_, ident], [out])

    def act(self, out, in_, func, bias=None, scale=None, accum_out=None, eng='act'):
        kw = {}
        reads = [in_]
        writes = [out]
        if bias is not None:
            kw['bias'] = bias
            if not isinstance(bias, (int, float)):
                reads.append(bias)
        if scale is not None:
            kw['scale'] = scale
            if not isinstance(scale, (int, float)):
                reads.append(scale)
        if accum_out is not None:
            kw['accum_out'] = accum_out
            writes.append(accum_out)
        return self.op('act', lambda e: e.activation(out, in_, func, **kw), reads, writes)

    def copy(self, e, out, in_):
        if e == 'act':
            return self.op('act', lambda g: g.copy(out, in_), [in_], [out])
        return self.op(e, lambda g: g.tensor_copy(out, in_), [in_], [out])

    def tt(self, e, out, in0, in1, op):
        return self.op(e, lambda g: g.tensor_tensor(out, in0, in1, op), [in0, in1], [out])

    def ts(self, e, out, in0, s1, s2, op0, op1=None, accum_out=None):
        reads = [in0] + [s for s in (s1, s2) if s is not None and not isinstance(s, (int, float))]
        writes = [out] + ([accum_out] if accum_out is not None else [])
        kw = {}
        if accum_out is not None:
            kw['accum_out'] = accum_out
        if op1 is None:
            return self.op(e, lambda g: g.tensor_scalar(out, in0, s1, s2, op0, **kw), reads, writes)
        return self.op(e, lambda g: g.tensor_scalar(out, in0, s1, s2, op0, op1, **kw), reads, writes)

    def stt(self, e, out, in0, scalar, in1, op0, op1):
        reads = [in0, in1] + ([scalar] if not isinstance(scalar, (int, float)) else [])
        return self.op(e, lambda g: g.scalar_tensor_tensor(out, in0, scalar, in1, op0, op1), reads, [out])

    def memset(self, e, ap, val):
        return self.op(e, lambda g: g.memset(ap, val), [], [ap])

    def reduce(self, e, out, in_, op, axis=None):
        axis = AX.X if axis is None else axis
        return self.op(e, lambda g: g.tensor_reduce(out, in_, axis, op), [in_], [out])

    def bank(self):
        b = self.banks[self.npsum % len(self.banks)]
        self.npsum += 1
        return b


def bc_last(ap, n):
    return bass.AP(ap.tensor, ap.offset, [list(p) for p in ap.ap] + [[0, n]])


def bc_part(ap, n=128):
    dims = [list(p) for p in ap.ap]
    if len(dims) == 2 and dims[0][1] == 1:
        dims = dims[1:]
    return bass.AP(ap.tensor, ap.offset, [[0, n]] + dims)


C_ID, C_ML, C_MSL, C_MSLT, C_MB, C_CI = 0, 128, 256, 384, 512, 640
C_CM = 704
C_TRI, C_ONE, C_ECAP = 960, 1088, 1216
NCONST = 1248
CAP = 512
NE = 32


def make_consts():
    c = np.zeros((128, NCONST), np.float32)
    i = np.arange(128)
    same = (i[:, None] // 64) == (i[None, :] // 64)
    c[:, C_ID:C_ID + 128] = np.eye(128)
    c[:, C_ML:C_ML + 128] = (same & (i[:, None] <= i[None, :]))
    c[:, C_MSL:C_MSL + 128] = (same & (i[:, None] < i[None, :]))
    c[:, C_MSLT:C_MSLT + 128] = (same & (i[:, None] > i[None, :]))
    c[:, C_MB:C_MB + 128] = np.where(i[:, None] > i[None, :], -30000.0, 0.0)
    c[:, C_CI] = (i < 64)
    c[:, C_CI + 1] = (i >= 64)
    c[:, C_CM:C_CM + 64] = 1.0
    c[:, C_CM + 128 + 64:C_CM + 256] = 1.0
    c[:, C_TRI:C_TRI + 128] = (i[:, None] < i[None, :])
    c[:, C_ONE:C_ONE + 128] = 1.0
    c[:, C_ECAP:C_ECAP + 32] = np.arange(32)[None, :] * CAP
    aug = np.zeros((4, 2, 4, S), np.float32)
    t = np.arange(S)
    for h in range(4):
        sl = 2.0 ** (-8.0 * (h + 1) / 4)
        aug[0, 0, h] = 1.0
        aug[1, 0, h] = -sl * (t % 128)
        aug[2, 0, h] = 1.0
        aug[3, 0, h] = -sl * 128 * (t // 128)
        aug[0, 1, h] = sl * (t % 128)
        aug[1, 1, h] = 1.0
        aug[2, 1, h] = sl * 128 * (t // 128)
        aug[3, 1, h] = 1.0
    return c, aug


WSPECS = [
    ('ada_w', [DEPTH, D, 6 * D]), ('ada_b', [DEPTH, 6 * D]), ('norm1_g', [DEPTH, D]), ('norm2_g', [DEPTH, D]),
    ('w_in', [DEPTH, D, N_IN]), ('attn_qkg', [DEPTH, 1024]), ('attn_lambda', [DEPTH, 256]),
    ('attn_subln_g', [DEPTH, 128]), ('rwkv_mu', [DEPTH, 1792]), ('rwkv_w_up', [DEPTH, 64, 512]),
    ('rwkv_w0', [DEPTH, 512]), ('rwkv_a_up', [DEPTH, 64, 512]), ('rwkv_a0', [DEPTH, 512]),
    ('rwkv_g_up', [DEPTH, 128, 512]), ('rwkv_k_k', [DEPTH, 512]), ('rwkv_k_a', [DEPTH, 512]),
    ('rwkv_r_k', [DEPTH, 512]), ('rwkv_lnx_g', [DEPTH, 512]), ('rwkv_lnx_b', [DEPTH, 512]),
    ('gla_alpha_up', [DEPTH, 16, 256]), ('gla_alpha_b', [DEPTH, 256]), ('gla_norm_g4', [DEPTH, 512]),
    ('proj_attn', [DEPTH, 512, D]), ('proj_rwkv', [DEPTH, 512, D]), ('proj_gla', [DEPTH, 512, D]),
    ('w_out', [DEPTH, D, D]), ('router_w', [DEPTH, D, 36]), ('router_b', [DEPTH, 36]),
    ('exp_w_gate', [DEPTH, 32, D, 512]), ('exp_w_up', [DEPTH, 32, D, 512]), ('exp_w_down', [DEPTH, 32, 512, D]),
]


class Arena:
    def __init__(self, ap, nbytes):
        self.ap = ap
        self.nbytes = nbytes
        self.off = 0

    def reset(self):
        self.off = 0

    def get(self, shape, dt=F32, parts=128):
        es = mybir.dt.size(dt)
        n = 1
        for v in shape:
            n *= v
        nb = (n * es + 31) // 32 * 32
        assert self.off + nb <= self.nbytes, ("arena overflow", self.off, nb)
        a = self.ap[:, self.off // 4:(self.off + nb) // 4]
        self.off += nb
        if dt != F32:
            a = a.bitcast(dt)
        a = a[0:parts, 0:n]
        if len(shape) == 2:
            a = a.rearrange("p (a b) -> p a b", a=shape[0])
        elif len(shape) == 3:
            a = a.rearrange("p (a b c) -> p a b c", a=shape[0], b=shape[1])
        return a


def build_program(nlayers=DEPTH, upto='all', dbg=None, phases=None, scr_io=False):
    dbg = dbg or {}
    nc = bass.Bass("TRN2", target_bir_lowering=False)
    dram = {}
    x_in = nc.dram_tensor("x", [S, D], F32, kind="ExternalInput").ap()
    c_in = nc.dram_tensor("c", [128, 8], F32, kind="ExternalInput").ap()
    cst_in = nc.dram_tensor("consts", [128, NCONST], F32, kind="ExternalInput").ap()
    aug_in = nc.dram_tensor("aug", [4, 2, 4, S], F32, kind="ExternalInput").ap()
    W = {}
    for name, shp in WSPECS:
        W[name] = nc.dram_tensor(name, shp, F32, kind="ExternalInput").ap()
    out = nc.dram_tensor("out", [S, D], F32, kind="ExternalOutput").ap()
    P_scr = nc.dram_tensor("P_scr", [S, NPROJ], F32, kind="ExternalInput" if scr_io else "Internal").ap()
    OT_scr = nc.dram_tensor("OT_scr", [3, 512, S], BF16, kind="ExternalOutput" if scr_io else "Internal").ap()
    H2_scr = nc.dram_tensor("H2_scr", [S, D], BF16, kind="Internal").ap()
    XG = nc.dram_tensor("XG", [NE * CAP, D], BF16, kind="Internal").ap()
    YG = nc.dram_tensor("YG", [NE * CAP, D], F32, kind="Internal").ap()
    DBG = {}
    for name, shp in dbg.items():
        DBG[name] = nc.dram_tensor("dbg_" + name, list(shp), F32, kind="ExternalOutput").ap()

    with contextlib.ExitStack() as stack:
        s = Sched(nc, stack)
        banks = [nc.alloc_psum_tensor("pb%d" % i, [128, 512], F32).ap() for i in range(8)]
        s.banks = banks
        hT = nc.alloc_sbuf_tensor("hT", [128, 8, S], BF16).ap()
        modbc = nc.alloc_sbuf_tensor("modbc", [128, 6 * D], F32).ap()
        cst = nc.alloc_sbuf_tensor("cst", [128, NCONST], F32).ap()
        idb = nc.alloc_sbuf_tensor("idb", [128, 128], BF16).ap()
        mbb = nc.alloc_sbuf_tensor("mbb", [128, 128], BF16).ap()
        ones = nc.alloc_sbuf_tensor("ones", [1, 128], F32).ap()
        rows = nc.alloc_sbuf_tensor("rows", [1, 1408], F32).ap()
        csb = nc.alloc_sbuf_tensor("csb", [128, 8], F32).ap()
        zt = nc.alloc_sbuf_tensor("zt", [128, D], BF16).ap()
        ARENA_BYTES = (int(nc.sbuf_bytes_remaining) - 2048) // 1024 * 1024
        print('arena bytes', ARENA_BYTES, flush=True)
        arena_t = nc.alloc_sbuf_tensor("arena", [128, ARENA_BYTES // 4], F32).ap()
        A = Arena(arena_t, ARENA_BYTES)
        idf = cst[:, C_ID:C_ID + 128]
        m_l = cst[:, C_ML:C_ML + 128]
        m_sl = cst[:, C_MSL:C_MSL + 128]
        m_slT = cst[:, C_MSLT:C_MSLT + 128]
        cind = cst[:, C_CI:C_CI + 2]

        rr = [0]

        def alt():
            rr[0] += 1
            return 'act' if rr[0] % 2 else 'dve'

        def evac(out_, in_):
            return s.copy(alt(), out_, in_)

        def dump(name, ap_sb, dst=None):
            if name in DBG:
                s.dma(DBG[name] if dst is None else dst, ap_sb)

        s.dma(cst, cst_in)
        s.dma(csb, c_in)
        s.memset('dve', ones, 1.0)
        s.copy('dve', idb, idf)
        s.copy('dve', mbb, cst[:, C_MB:C_MB + 128])
        s.act(csb, csb, AF.Silu)
        s.memset('dve', zt, 0.0)

        def zero_xg():
            XGv = XG.rearrange("(p r) d -> p r d", p=128)
            RP = NE * CAP // 128
            for r0 in range(0, RP, 8):
                s.dma(XGv[:, r0:r0 + 8, :], bass.AP(zt.tensor, zt.offset, [list(zt.ap[0]), [0, 8], [1, D]]), q='pool')

        def rstd_from_ssq(dst, ssq, scale, eps, tmp):
            s.ts('dve', tmp, ssq, scale, eps, ALU.mult, ALU.add)
            s.act(tmp, tmp, AF.Sqrt)
            s.op('dve', lambda g: g.reciprocal(dst, tmp), [tmp], [dst])

        def phase_mod(l):
            A.reset()
            adaw = [A.get([8, 512]) for _ in range(2)]
            adab = [A.get([512]) for _ in range(2)]
            cbc = A.get([8, 128])
            for k in range(8):
                s.copy('dve', cbc[:, k:k + 1, :], bc_last(csb[:, k:k + 1], 128))
            for cg in range(12):
                buf = adaw[cg % 2]
                bb = adab[cg % 2]
                s.dma(buf, W['ada_w'][l, :, cg * 512:(cg + 1) * 512].rearrange("(k p) n -> p k n", p=128))
                s.dma(bb, bc_part(W['ada_b'][l:l + 1, cg * 512:(cg + 1) * 512]))
                pb = s.bank()
                for k in range(8):
                    s.mm(pb, cbc[:, k, :], buf[:, k, :], start=(k == 0), stop=(k == 7))
                s.tt('dve', modbc[:, cg * 512:(cg + 1) * 512], pb, bb, ALU.add)

        def phase_norm(l, which, src, router=None):
            A.reset()
            sh = modbc[:, (3 * which) * D:(3 * which + 1) * D]
            sc = modbc[:, (3 * which + 1) * D:(3 * which + 2) * D]
            G1 = A.get([D])
            gb = A.get([D])
            ssq = A.get([NT])
            rstd = A.get([NT])
            tmpn = A.get([NT])
            junk = A.get([D])
            xts = [A.get([D]) for _ in range(3)]
            tmps = [A.get([D]) for _ in range(2)]
            hbs = [A.get([D], BF16 if router is None else F32) for _ in range(2)]
            s.dma(gb, bc_part(W['norm1_g' if which == 0 else 'norm2_g'][l:l + 1, :]))
            s.stt('dve', G1, sc, 1.0, gb, ALU.add, ALU.mult)
            s.memset('dve', ssq, 0.0)
            if router is not None:
                rw, lg_all, hTf = router
                hbbs = [A.get([D], BF16) for _ in range(2)]
            for tt in range(NT):
                xt = xts[tt % 3]
                s.dma(xt, src[tt * 128:(tt + 1) * 128, :])
                s.act(junk, xt, AF.Square, accum_out=ssq[:, tt:tt + 1])
                rstd_from_ssq(rstd[:, tt:tt + 1], ssq[:, tt:tt + 1], 1.0 / D, EPS, tmpn[:, tt:tt + 1])
                tmp = tmps[tt % 2]
                hb = hbs[tt % 2]
                s.stt('dve', tmp, xt, rstd[:, tt:tt + 1], G1, ALU.mult, ALU.mult)
                s.tt('dve', hb, tmp, sh, ALU.add)
                if router is None:
                    pb = s.bank().bitcast(BF16)
                    for k in range(8):
                        s.tr(pb[:, k * 128:(k + 1) * 128], hb[:, k * 128:(k + 1) * 128], idb)
                    evac(hT[:, :, tt * 128:(tt + 1) * 128], pb.rearrange("p (k t) -> p k t", k=8))
                else:
                    hf = hTf[tt % 2]
                    for half in range(2):
                        pb = s.bank()
                        for k in range(4):
                            kk = half * 4 + k
                            s.tr(pb[:, k * 128:(k + 1) * 128], hb[:, kk * 128:(kk + 1) * 128], idf)
                        s.copy('act', hT[:, half * 4:half * 4 + 4, tt * 128:(tt + 1) * 128],
                               pb.rearrange("p (k t) -> p k t", k=4))
                        s.copy('dve', hf[:, half * 4:half * 4 + 4, :], pb.rearrange("p (k t) -> p k t", k=4))
                    pl = s.bank()
                    for k in range(8):
                        s.mm(pl[:, 0:64], hf[:, k, :], rw[:, k, :], start=(k == 0), stop=False)
                    s.mm(pl[:, 0:64], ones[0:1, 0:128], rows[0:1, 1280:1344], start=False, stop=True)
                    s.copy('dve', lg_all[:, tt, :], pl[:, 0:36])
                    hbb = hbbs[tt % 2]
                    s.copy('act', hbb, hb)
                    s.dma(H2_scr[tt * 128:(tt + 1) * 128, :], hbb)

        def phase_proj(l):
            A.reset()
            wb = [A.get([8, 512], BF16) for _ in range(2)]
            stg = [A.get([512]) for _ in range(4)]
            nchunk = (NPROJ + 511) // 512
            si = 0
            for j in range(nchunk):
                c0 = j * 512
                ncol = min(512, NPROJ - c0)
                w = wb[j % 2]
                s.dma(w[:, :, 0:ncol], W['w_in'][l, :, c0:c0 + ncol].rearrange("(k p) n -> p k n", p=128), q='pool')
                for tt in range(NT):
                    pb = s.bank()
                    for k in range(8):
                        s.mm(pb[:, 0:ncol], hT[:, k, tt * 128:(tt + 1) * 128], w[:, k, 0:ncol],
                             start=(k == 0), stop=(k == 7))
                    st = stg[si % 4]
                    si += 1
                    evac(st[:, 0:ncol], pb[:, 0:ncol])
                    s.dma(P_scr[tt * 128:(tt + 1) * 128, c0:c0 + ncol], st[:, 0:ncol])

        def phase_attn(l):
            lambda_init = 0.8 - 0.6 * math.exp(-0.3 * l)
            A.reset()
            qkT = A.get([16, S], BF16, parts=68)
            vaug = A.get([NT, 4 * 129], BF16)
            qkg = A.get([1024])
            sub_g = A.get([128])
            lamb = A.get([256])
            small = A.get([64])
            qk = [A.get([1024]) for _ in range(2)]
            vt = [A.get([512]) for _ in range(2)]
            sq = A.get([1024])
            qn = [A.get([1024], BF16) for _ in range(2)]
            PT = [A.get([512], BF16) for _ in range(3)]
            t0 = [A.get([128]) for _ in range(2)]
            ot = [A.get([128]) for _ in range(2)]
            oa = [A.get([512], BF16) for _ in range(2)]
            oT = [A.get([4, 128], BF16) for _ in range(2)]
            s.dma(qkg, bc_part(W['attn_qkg'][l:l + 1, :]))
            s.dma(sub_g, bc_part(W['attn_subln_g'][l:l + 1, :]))
            s.dma(lamb, bc_part(W['attn_lambda'][l:l + 1, :]))
            lp = small[:, 0:2]
            s.tt('dve', sq[:, 0:64], lamb[:, 0:64], lamb[:, 64:128], ALU.mult)
            s.tt('dve', sq[:, 64:128], lamb[:, 128:192], lamb[:, 192:256], ALU.mult)
            s.reduce('dve', lp, sq[:, 0:128].rearrange("p (a b) -> p a b", a=2), ALU.add)
            s.act(lp, lp, AF.Exp)
            nlam = small[:, 2:3]
            s.tt('dve', nlam, lp[:, 1:2], lp[:, 0:1], ALU.subtract)
            s.ts('dve', nlam, nlam, -lambda_init, None, ALU.add)
            for g in range(16):
                isk = g // 8
                h = (g % 8) // 2
                s.dma(qkT[64:68, g, :], aug_in[:, isk, h, :], q='pool')
            s.memset('dve', vaug, 1.0)
            ssq = small[:, 8:24]
            rs = small[:, 24:40]
            tm = small[:, 40:56]
            for tt in range(NT):
                q_ = qk[tt % 2]
                v_ = vt[tt % 2]
                s.dma(q_, P_scr[tt * 128:(tt + 1) * 128, 0:1024])
                s.dma(v_, P_scr[tt * 128:(tt + 1) * 128, 1024:1536])
                s.tt('dve', sq, q_, q_, ALU.mult)
                s.reduce('dve', ssq, sq.rearrange("p (a b) -> p a b", a=16), ALU.add)
                rstd_from_ssq(rs[:, 0:8], ssq[:, 0:8], 1.0, 64 * EPS, tm[:, 0:8])
                rstd_from_ssq(rs[:, 8:16], ssq[:, 8:16], 1.0 / 64, EPS, tm[:, 8:16])
                s.tt('dve', sq.rearrange("p (a b) -> p a b", a=16), q_.rearrange("p (a b) -> p a b", a=16),
                     bc_last(rs, 64), ALU.mult)
                qb_ = qn[tt % 2]
                s.tt('dve', qb_, sq, qkg, ALU.mult)
                for half in range(2):
                    pb = s.bank().bitcast(BF16)
                    for j in range(8):
                        g = half * 8 + j
                        s.tr(pb[0:64, j * 128:(j + 1) * 128], qb_[:, g * 64:(g + 1) * 64], idb)
                    evac(qkT[0:64, half * 8:half * 8 + 8, tt * 128:(tt + 1) * 128],
                         pb[0:64, :].rearrange("p (g t) -> p g t", g=8))
                s.copy('act', vaug[:, tt, :].rearrange("p (h d) -> p h d", h=4)[:, :, 0:128],
                       v_.rearrange("p (h d) -> p h d", h=4))
            import os
            stage = int(os.environ.get('ATT_STAGE', '9'))
            if stage < 2:
                return
            SB = banks[0:4]
            ACC = banks[4:8]
            sbi = 0
            pti = 0
            for qb in range(NT):
                for h in range(4):
                    accs = [ACC[(2 * (qb * 4 + h)) % 4], ACC[(2 * (qb * 4 + h) + 1) % 4]]
                    for c in range(2):
                        gq = h * 2 + c
                        gk = 8 + h * 2 + c
                        acc = accs[c]
                        for jg in range(qb // 4 + 1):
                            jbs = list(range(jg * 4, min(jg * 4 + 4, qb + 1)))
                            sbk = SB[sbi % 4]
                            sbi += 1
                            for jj, jb in enumerate(jbs):
                                diag = (jb == qb)
                                s.mm(sbk[:, jj * 128:(jj + 1) * 128], qkT[0:68, gk, jb * 128:(jb + 1) * 128],
                                     qkT[0:68, gq, qb * 128:(qb + 1) * 128], start=True, stop=not diag)
                                if diag:
                                    s.mm(sbk[:, jj * 128:(jj + 1) * 128], idb, mbb, start=False, stop=True)
                            pt = PT[pti % 3]
                            pti += 1
                            n = len(jbs) * 128
                            s.act(pt[:, 0:n], sbk[:, 0:n], AF.Exp)
                            for jj, jb in enumerate(jbs):
                                s.mm(acc[:, 0:129], pt[:, jj * 128:(jj + 1) * 128],
                                     vaug[:, jb, h * 129:(h + 1) * 129], start=(jb == 0), stop=(jb == qb))
                    if stage < 3:
                        continue
                    i2 = (qb * 4 + h) % 2
                    rd = small[:, 56 + 4 * i2:56 + 4 * i2 + 4]
                    s.op('dve', lambda g, a=rd[:, 0:1], b=accs[0][:, 128:129]: g.reciprocal(a, b),
                         [accs[0][:, 128:129]], [rd[:, 0:1]])
                    s.op('dve', lambda g, a=rd[:, 1:2], b=accs[1][:, 128:129]: g.reciprocal(a, b),
                         [accs[1][:, 128:129]], [rd[:, 1:2]])
                    s.tt('dve', rd[:, 1:2], rd[:, 1:2], nlam, ALU.mult)
                    s.ts('dve', t0[i2], accs[0][:, 0:128], rd[:, 0:1], None, ALU.mult)
                    s.stt('dve', ot[i2], accs[1][:, 0:128], rd[:, 1:2], t0[i2], ALU.mult, ALU.add)
                    s.memset('dve', rd[:, 2:3], 0.0)
                    s.act(t0[i2], ot[i2], AF.Square, accum_out=rd[:, 2:3])
                    rstd_from_ssq(rd[:, 2:3], rd[:, 2:3], 1.0 / 128, EPS, rd[:, 3:4])
                    s.ts('dve', rd[:, 2:3], rd[:, 2:3], 1.0 - lambda_init, None, ALU.mult)
                    oab = oa[qb % 2]
                    s.stt('dve', oab[:, h * 128:(h + 1) * 128], ot[i2], rd[:, 2:3], sub_g, ALU.mult, ALU.mult)
                if stage < 4:
                    continue
                oab = oa[qb % 2]
                pb = banks[(qb % 2)].bitcast(BF16)
                for k in range(4):
                    s.tr(pb[:, k * 128:(k + 1) * 128], oab[:, k * 128:(k + 1) * 128], idb)
                o_t = oT[qb % 2]
                evac(o_t, pb[:, 0:512].rearrange("p (k t) -> p k t", k=4))
                s.dma(OT_scr[0, :, qb * 128:(qb + 1) * 128].rearrange("(k p) t -> p k t", p=128), o_t)

        def chunk_core(H, Vd, Rb, Kb, Ab, Bb, Vt, epos, Hst, Y, wk, lowrank, KbM, BbM):
            G = wk['G']
            nq = 4 if lowrank else 2
            m_l2 = bass.AP(m_l.tensor, m_l.offset, [list(m_l.ap[0]), [0, 2], [1, 128]])
            m_sl2 = bass.AP(m_sl.tensor, m_sl.offset, [list(m_sl.ap[0]), [0, 2], [1, 128]])
            for g0 in range(0, H, G):
                hs = list(range(g0, min(g0 + G, H)))
                SL = {h: wk['slots'][j] for j, h in enumerate(hs)}
                ksl = lambda h: slice(h * 64, (h + 1) * 64)
                vsl = lambda h: slice(h * Vd, (h + 1) * Vd)
                pbs = {}
                for h in hs:
                    pb = s.bank()
                    pbs[h] = pb
                    for qi, src in enumerate([Rb, Kb, Ab, Bb][:nq]):
                        s.tr(pb[0:64, qi * 128:(qi + 1) * 128], src[:, ksl(h)], idf)
                for h in hs:
                    evac(SL[h]['FT'][:, 0:nq, :], pbs[h][0:64, 0:nq * 128].rearrange("p (q t) -> p q t", q=nq))
                for h in hs:
                    pb = s.bank()
                    pbs[h] = pb
                    s.tr(pb[0:64, 0:128], epos[:, ksl(h)], idf)
                for h in hs:
                    evac(SL[h]['ET'], pbs[h][0:64, 0:128])
                for h in hs:
                    FT = SL[h]['FT']
                    RT, KT = FT[:, 0, :], FT[:, 1, :]
                    pm = s.bank()
                    pbs[h] = pm
                    s.mm(pm[:, 0:128], KT, RT)
                    if lowrank:
                        AT, BT = FT[:, 2, :], FT[:, 3, :]
                        s.mm(pm[:, 128:256], BT, RT)
                        s.mm(pm[:, 256:384], KT, AT)
                        s.mm(pm[:, 384:512], BT, AT)
                for h in hs:
                    M1 = SL[h]['M1']
                    pm = pbs[h]
                    if lowrank:
                        s.tt('dve', M1[:, 0:2, :], pm[:, 0:256].rearrange("p (a b) -> p a b", a=2), m_l2, ALU.mult)
                        s.tt('dve', M1[:, 2:4, :], pm[:, 256:512].rearrange("p (a b) -> p a b", a=2), m_sl2, ALU.mult)
                    else:
                        s.tt('dve', M1[:, 0, :], pm[:, 0:128], m_l, ALU.mult)
                if lowrank:
                    cur = {}
                    for h in hs:
                        FT = SL[h]['FT']
                        pa = s.bank()
                        pbs[h] = pa
                        s.mm(pa[:, 0:128], FT[:, 2, :], FT[:, 3, :])
                    for h in hs:
                        sl = SL[h]
                        s.tt('dve', sl['P'][0], pbs[h][:, 0:128], m_slT, ALU.mult)
                        s.tt('dve', sl['W'][0], sl['M1'][:, 3, :], idf, ALU.add)
                        cur[h] = [sl['P'][0], sl['M1'][:, 3, :], sl['W'][0]]
                    for i in range(1, 6):
                        for h in hs:
                            Pc, Qc, Wc = cur[h]
                            pp = s.bank()
                            pbs[h] = pp
                            s.mm(pp[:, 0:128], Qc, Pc)
                            if i < 5:
                                s.mm(pp[:, 128:256], Pc, Qc)
                        for h in hs:
                            sl = SL[h]
                            Pn = sl['P'][i % 2]
                            s.copy('act', Pn, pbs[h][:, 0:128])
                            if i < 5:
                                Qn = sl['Q'][i % 2]
                                s.copy('dve', Qn, pbs[h][:, 128:256])
                                cur[h][1] = Qn
                            cur[h][0] = Pn
                        for h in hs:
                            pw = s.bank()
                            pbs[h] = pw
                            s.mm(pw[:, 0:128], cur[h][0], cur[h][2])
                        for h in hs:
                            Wn = SL[h]['W'][i % 2]
                            s.tt('dve', Wn, pbs[h][:, 0:128], cur[h][2], ALU.add)
                            cur[h][2] = Wn
                    for h in hs:
                        pz = s.bank()
                        pbs[h] = pz
                        s.mm(pz[:, 0:Vd], SL[h]['M1'][:, 2, :], Vt[:, vsl(h)])
                    for h in hs:
                        ZA = SL[h]['ZA']
                        s.copy('act', ZA[:, 0:Vd], pbs[h][:, 0:Vd])
                        s.copy('dve', ZA[:, Vd:Vd + 64], Ab[:, ksl(h)])
                    for h in hs:
                        pu = s.bank()
                        pbs[h] = pu
                        s.mm(pu[:, 0:Vd + 64], cur[h][2], SL[h]['ZA'][:, 0:Vd + 64])
                    for h in hs:
                        s.copy('act', SL[h]['UA'][:, 0:Vd + 64], pbs[h][:, 0:Vd + 64])
                    for h in hs:
                        pr = s.bank()
                        pbs[h] = pr
                        s.mm(pr[0:64, 0:128], SL[h]['UA'][:, Vd:Vd + 64], SL[h]['M1'][:, 1, :])
                    for h in hs:
                        s.tt('dve', SL[h]['RtT'], pbs[h][0:64, 0:128], SL[h]['FT'][:, 0, :], ALU.add)
                RtTs = {h: (SL[h]['RtT'] if lowrank else SL[h]['FT'][:, 0, :]) for h in hs}
                for c in range(2):
                    for h in hs:
                        s.copy('dve', SL[h]['Hc'][:, c, :], Hst[:, h, :])
                    if lowrank:
                        for h in hs:
                            pn = s.bank()
                            pbs[h] = pn
                            s.mm(pn[0:64, 0:64], SL[h]['UA'][:, Vd:Vd + 64], BbM[c][:, ksl(h)])
                            s.mm(pn[0:64, 64:64 + Vd], BbM[c][:, ksl(h)], SL[h]['UA'][:, 0:Vd], start=True, stop=False)
                            s.mm(pn[0:64, 64:64 + Vd], KbM[c][:, ksl(h)], Vt[:, vsl(h)], start=False, stop=True)
                        for h in hs:
                            s.tt('dve', SL[h]['BTI'], pbs[h][0:64, 0:64], idf[0:64, 0:64], ALU.add)
                        for h in hs:
                            s.mm(pbs[h][0:64, 256:256 + Vd], SL[h]['BTI'], SL[h]['Hc'][:, c, :], start=True, stop=True)
                        for h in hs:
                            gsc = SL[h]['ET'][:, c * 64 + 63:c * 64 + 64]
                            s.ts('dve', SL[h]['tmpH'], pbs[h][0:64, 256:256 + Vd], gsc, None, ALU.mult)
                            s.stt('dve', Hst[:, h, :], pbs[h][0:64, 64:64 + Vd], gsc, SL[h]['tmpH'], ALU.mult, ALU.add)
                    else:
                        for h in hs:
                            pn = s.bank()
                            pbs[h] = pn
                            s.mm(pn[0:64, 0:Vd], KbM[c][:, ksl(h)], Vt[:, vsl(h)], start=True, stop=True)
                        for h in hs:
                            gsc = SL[h]['ET'][:, c * 64 + 63:c * 64 + 64]
                            s.ts('dve', SL[h]['tmpH'], SL[h]['Hc'][:, c, :], gsc, None, ALU.mult)
                            s.stt('dve', Hst[:, h, :], pbs[h][0:64, 0:Vd], gsc, SL[h]['tmpH'], ALU.mult, ALU.add)
                for h in hs:
                    for c in range(2):
                        s.tt('pool', SL[h]['RtM'][:, c, :], RtTs[h], cst[0:64, C_CM + c * 128:C_CM + (c + 1) * 128], ALU.mult)
                pys = {}
                for h in hs:
                    py = s.bank()
                    pys[h] = py
                    if lowrank:
                        s.mm(py[:, 0:Vd], SL[h]['M1'][:, 1, :], SL[h]['UA'][:, 0:Vd], start=True, stop=False)
                        s.mm(py[:, 0:Vd], SL[h]['M1'][:, 0, :], Vt[:, vsl(h)], start=False, stop=True)
                    else:
                        s.mm(py[:, 0:Vd], SL[h]['M1'][:, 0, :], Vt[:, vsl(h)], start=True, stop=True)
                for h in hs:
                    s.copy('act', Y[:, vsl(h)], pys[h][:, 0:Vd])
                for h in hs:
                    pyh = s.bank()
                    pys[h] = pyh
                    for c in range(2):
                        s.mm(pyh[:, 0:Vd], SL[h]['RtM'][:, c, :], SL[h]['Hc'][:, c, :], start=(c == 0), stop=(c == 1))
                for h in hs:
                    s.tt('dve', Y[:, vsl(h)], pys[h][:, 0:Vd], Y[:, vsl(h)], ALU.add)

        def alloc_wk(Vd, lowrank, G=4):
            wk = {'G': G, 'slots': []}
            for j in range(G):
                sl = {}
                sl['FT'] = A.get([4 if lowrank else 2, 128], F32, parts=64)
                sl['ET'] = A.get([128], F32, parts=64)
                sl['M1'] = A.get([4 if lowrank else 1, 128])
                sl['Hc'] = A.get([2, Vd], F32, parts=64)
                sl['tmpH'] = A.get([Vd], F32, parts=64)
                sl['RtM'] = A.get([2, 128], F32, parts=64)
                if lowrank:
                    sl['P'] = [A.get([128]) for _ in range(2)]
                    sl['Q'] = [A.get([128]) for _ in range(2)]
                    sl['W'] = [A.get([128]) for _ in range(2)]
                    sl['ZA'] = A.get([Vd + 64])
                    sl['UA'] = A.get([Vd + 64])
                    sl['RtT'] = A.get([128], F32, parts=64)
                    sl['BTI'] = A.get([64], F32, parts=64)
                wk['slots'].append(sl)
            return wk

        def gammas(H, lw, gam):
            pg = s.bank()
            for h in range(H):
                s.mm(pg[0:64, h * 32:(h + 1) * 32], lw[:, h * 64:(h + 1) * 64], cst[:, C_CI:C_CI + 32])
            s.act(gam, pg[0:64, 0:32 * H].rearrange("p (h c) -> p h c", h=H)[:, :, 0:2], AF.Exp)

        def store_T(idx, src_bf, tt, oTb):
            pb = s.bank().bitcast(BF16)
            for k in range(4):
                s.tr(pb[:, k * 128:(k + 1) * 128], src_bf[:, k * 128:(k + 1) * 128], idb)
            evac(oTb, pb[:, 0:512].rearrange("p (k t) -> p k t", k=4))
            s.dma(OT_scr[idx, :, tt * 128:(tt + 1) * 128].rearrange("(k p) t -> p k t", p=128), oTb)

        def phase_rwkv(l):
            A.reset()
            zero_xg()
            wk = alloc_wk(64, True)
            mu = A.get([1792])
            wup = A.get([512], F32, parts=64)
            aup = A.get([512], F32, parts=64)
            gup = A.get([512])
            kkb = A.get([512])
            kab = A.get([512])
            rkb = A.get([512])
            lng = A.get([512])
            lnb = A.get([512])
            Hst = A.get([8, 64], F32, parts=64)
            cur = [A.get([1792]) for _ in range(2)]
            prv = A.get([1792])
            lora = A.get([256])
            sgT = A.get([128])
            lw = A.get([512])
            av = A.get([512])
            gv = A.get([512])
            epos = A.get([512])
            eneg = A.get([512])
            eprv = A.get([512])
            kkn = A.get([512])
            kmod = A.get([512])
            Rb = A.get([512])
            Ab = A.get([512])
            Bb = A.get([512])
            Kb = A.get([512])
            KbM = [A.get([512]) for _ in range(2)]
            BbM = [A.get([512]) for _ in range(2)]
            Y = A.get([512])
            t1 = A.get([512])
            t2 = A.get([512])
            st8 = A.get([64])
            orb = [A.get([512], BF16) for _ in range(2)]
            oTb = [A.get([4, 128], BF16) for _ in range(2)]
            s.dma(mu, bc_part(W['rwkv_mu'][l:l + 1, :]))
            s.dma(wup, W['rwkv_w_up'][l])
            s.dma(aup, W['rwkv_a_up'][l])
            s.dma(gup, W['rwkv_g_up'][l])
            s.dma(rows[0:1, 0:512], W['rwkv_w0'][l:l + 1, :])
            s.dma(rows[0:1, 512:1024], W['rwkv_a0'][l:l + 1, :])
            for dst, nm in ((kkb, 'rwkv_k_k'), (kab, 'rwkv_k_a'), (rkb, 'rwkv_r_k'), (lng, 'rwkv_lnx_g'),
                            (lnb, 'rwkv_lnx_b')):
                s.dma(dst, bc_part(W[nm][l:l + 1, :]))
            s.memset('dve', Hst, 0.0)
            v3 = lambda ap: ap.rearrange("p (h k) -> p h k", h=8)
            import os
            for tt in range(int(os.environ.get('RW_TILES', NT))):
                cu = cur[tt % 2]
                r0 = tt * 128
                s.dma(cu, P_scr[r0:r0 + 128, 1536:3328])
                if tt == 0:
                    s.memset('dve', prv[0:1, :], 0.0)
                    s.dma(prv[1:128, :], P_scr[0:127, 1536:3328])
                else:
                    s.dma(prv, P_scr[r0 - 1:r0 + 127, 1536:3328])
                s.tt('dve', prv, prv, cu, ALU.subtract)
                s.tt('dve', prv, prv, mu, ALU.mult)
                s.tt('dve', cu, cu, prv, ALU.add)
                r_, k_, v_ = cu[:, 0:512], cu[:, 512:1024], cu[:, 1024:1536]
                pl = s.bank()
                s.tr(pl[0:64, 0:128], cu[:, 1536:1600], idf)
                s.tr(pl[0:64, 128:256], cu[:, 1600:1664], idf)
                s.tr(pl[:, 256:384], cu[:, 1664:1792], idf)
                s.act(lora[0:64, 0:128], pl[0:64, 0:128], AF.Tanh)
                s.copy('dve', lora[0:64, 128:256], pl[0:64, 128:256])
                s.act(sgT, pl[:, 256:384], AF.Sigmoid)
                px = s.bank()
                s.mm(px, lora[0:64, 0:128], wup, start=True, stop=False)
                s.mm(px, ones[0:1, 0:128], rows[0:1, 0:512], start=False, stop=True)
                s.act(lw, px, AF.Sigmoid)
                s.ts('dve', lw, lw, -math.exp(-0.5), None, ALU.mult)
                pa = s.bank()
                s.mm(pa, lora[0:64, 128:256], aup, start=True, stop=False)
                s.mm(pa, ones[0:1, 0:128], rows[0:1, 512:1024], start=False, stop=True)
                s.act(av, pa, AF.Sigmoid)
                pg = s.bank()
                s.mm(pg, sgT, gup)
                s.copy('dve', gv, pg)
                pc = s.bank()
                s.mm(pc, m_l, lw)
                s.act(epos, pc, AF.Exp)
                s.act(eneg, pc, AF.Exp, scale=-1.0)
                s.tt('dve', t1, pc, lw, ALU.subtract)
                s.act(eprv, t1, AF.Exp)
                s.tt('dve', kkn, k_, kkb, ALU.mult)
                s.tt('dve', t1, kkn, kkn, ALU.mult)
                s.reduce('dve', st8[:, 0:8], v3(t1), ALU.add)
                s.act(st8[:, 8:16], st8[:, 0:8], AF.Sqrt)
                s.ts('dve', st8[:, 8:16], st8[:, 8:16], 1e-12, None, ALU.max)
                s.op('dve', lambda g: g.reciprocal(st8[:, 16:24], st8[:, 8:16]), [st8[:, 8:16]], [st8[:, 16:24]])
                s.tt('dve', v3(kkn), v3(kkn), bc_last(st8[:, 16:24], 64), ALU.mult)
                s.stt('dve', t2, av, -1.0, kab, ALU.add, ALU.mult)
                s.stt('dve', kmod, t2, 1.0, k_, ALU.add, ALU.mult)
                s.tt('dve', Rb, r_, epos, ALU.mult)
                s.stt('dve', Ab, kkn, -1.0, eprv, ALU.mult, ALU.mult)
                s.tt('dve', t2, kkn, av, ALU.mult)
                s.tt('dve', Bb, t2, eneg, ALU.mult)
                s.tt('dve', Kb, kmod, eneg, ALU.mult)
                import os
                rst = int(os.environ.get('RW_STAGE', '9'))
                if rst < 2:
                    continue
                if rst < 3:
                    continue
                for c in range(2):
                    s.ts('dve', KbM[c], Kb, cst[:, C_CI + c:C_CI + c + 1], None, ALU.mult)
                    s.ts('dve', BbM[c], Bb, cst[:, C_CI + c:C_CI + c + 1], None, ALU.mult)
                chunk_core(8, 64, Rb, Kb, Ab, Bb, v_, epos, Hst, Y, wk, True, KbM, BbM)
                if rst < 4:
                    continue
                s.reduce('dve', st8[:, 24:32], v3(Y), ALU.add)
                s.tt('dve', t1, Y, Y, ALU.mult)
                s.reduce('dve', st8[:, 32:40], v3(t1), ALU.add)
                s.ts('dve', st8[:, 24:32], st8[:, 24:32], 1.0 / 64, None, ALU.mult)
                s.tt('dve', st8[:, 40:48], st8[:, 24:32], st8[:, 24:32], ALU.mult)
                s.stt('dve', st8[:, 32:40], st8[:, 32:40], 1.0 / 64, st8[:, 40:48], ALU.mult, ALU.subtract)
                rstd_from_ssq(st8[:, 48:56], st8[:, 32:40], 1.0, 64e-5, st8[:, 56:64])
                s.tt('dve', v3(t1), v3(Y), bc_last(st8[:, 24:32], 64), ALU.subtract)
                s.tt('dve', v3(t1), v3(t1), bc_last(st8[:, 48:56], 64), ALU.mult)
                s.tt('dve', t1, t1, lng, ALU.mult)
                s.tt('dve', t1, t1, lnb, ALU.add)
                s.tt('dve', t2, r_, kmod, ALU.mult)
                s.tt('dve', t2, t2, rkb, ALU.mult)
                s.reduce('dve', st8[:, 0:8], v3(t2), ALU.add)
                s.tt('dve', v3(t2), v3(v_), bc_last(st8[:, 0:8], 64), ALU.mult)
                s.tt('dve', t1, t1, t2, ALU.add)
                ob = orb[tt % 2]
                s.tt('dve', ob, t1, gv, ALU.mult)
                store_T(1, ob, tt, oTb[tt % 2])

        def phase_gla(l):
            A.reset()
            wk = alloc_wk(128, False)
            aup = A.get([256], F32, parts=16)
            ng = A.get([512])
            Hst = A.get([4, 128], F32, parts=64)
            cur = [A.get([1552]) for _ in range(2)]
            adT = A.get([128], F32, parts=16)
            ll = A.get([256])
            epos = A.get([256])
            eneg = A.get([256])
            Rb = A.get([256])
            Kb = A.get([256])
            KbM = [A.get([256]) for _ in range(2)]
            Y = A.get([512])
            t1 = A.get([512])
            sg = A.get([512])
            st4 = A.get([16])
            ogb = [A.get([512], BF16) for _ in range(2)]
            oTb = [A.get([4, 128], BF16) for _ in range(2)]
            s.dma(aup, W['gla_alpha_up'][l])
            s.dma(rows[0:1, 1024:1280], W['gla_alpha_b'][l:l + 1, :])
            s.dma(ng, bc_part(W['gla_norm_g4'][l:l + 1, :]))
            s.memset('dve', Hst, 0.0)
            v4 = lambda ap: ap.rearrange("p (h k) -> p h k", h=4)
            for tt in range(NT):
                cu = cur[tt % 2]
                r0 = tt * 128
                s.dma(cu, P_scr[r0:r0 + 128, 3328:4880])
                q_, k_, v_, gate = cu[:, 0:256], cu[:, 256:512], cu[:, 512:1024], cu[:, 1040:1552]
                pl = s.bank()
                s.tr(pl[0:16, 0:128], cu[:, 1024:1040], idf)
                s.copy('dve', adT, pl[0:16, 0:128])
                pz = s.bank()
                s.mm(pz[:, 0:256], adT, aup, start=True, stop=False)
                s.mm(pz[:, 0:256], ones[0:1, 0:128], rows[0:1, 1024:1280], start=False, stop=True)
                s.act(ll, pz[:, 0:256], AF.Sigmoid)
                s.act(ll, ll, AF.Ln)
                s.ts('dve', ll, ll, 1.0 / 16, None, ALU.mult)
                pc = s.bank()
                s.mm(pc[:, 0:256], m_l, ll)
                s.act(epos, pc[:, 0:256], AF.Exp)
                s.act(eneg, pc[:, 0:256], AF.Exp, scale=-1.0)
                s.stt('dve', Rb, q_, 0.125, epos, ALU.mult, ALU.mult)
                s.tt('pool', Kb, k_, eneg, ALU.mult)
                for c in range(2):
                    s.ts('dve', KbM[c], Kb, cst[:, C_CI + c:C_CI + c + 1], None, ALU.mult)
                chunk_core(4, 128, Rb, Kb, None, None, v_, epos, Hst, Y, wk, False, KbM, None)
                s.tt('pool', t1, Y, Y, ALU.mult)
                s.reduce('dve', st4[:, 0:4], v4(t1), ALU.add)
                rstd_from_ssq(st4[:, 4:8], st4[:, 0:4], 1.0 / 128, EPS, st4[:, 8:12])
                s.tt('dve', v4(t1), v4(Y), bc_last(st4[:, 4:8], 128), ALU.mult)
                s.tt('pool', t1, t1, ng, ALU.mult)
                s.act(sg, gate, AF.Silu)
                ob = ogb[tt % 2]
                s.tt('dve', ob, t1, sg, ALU.mult)
                store_T(2, ob, tt, oTb[tt % 2])

        def phase_merge(l, xsrc):
            A.reset()
            gt1 = modbc[:, 2 * D:3 * D]
            gw = A.get([8, 3072], BF16)
            pw = A.get([12, 1024], BF16)
            ow = A.get([8, 1024], BF16)
            oTs = [A.get([12, 512], BF16) for _ in range(2)]
            mT = A.get([8, 512], BF16)
            macc = A.get([512])
            sgm = A.get([512])
            xt = [A.get([1024]) for _ in range(2)]
            tmp = A.get([512])
            for j in range(6):
                s.dma(gw[:, :, j * 512:(j + 1) * 512],
                      W['w_in'][l, :, NPROJ + j * 512:NPROJ + (j + 1) * 512].rearrange("(k p) n -> p k n", p=128),
                      q='pool')
            for bi, nm in enumerate(('proj_attn', 'proj_rwkv', 'proj_gla')):
                s.dma(pw[:, bi * 4:(bi + 1) * 4, :], W[nm][l].rearrange("(k p) n -> p k n", p=128), q='pool')
            s.dma(ow, W['w_out'][l].rearrange("(k p) n -> p k n", p=128), q='pool')
            for tg in range(4):
                ts_ = slice(tg * 512, (tg + 1) * 512)
                oT = oTs[tg % 2]
                for bi in range(3):
                    s.dma(oT[:, bi * 4:(bi + 1) * 4, :], OT_scr[bi, :, ts_].rearrange("(k p) t -> p k t", p=128))
                for dc in range(8):
                    for bi in range(3):
                        pg = s.bank()
                        for k in range(8):
                            s.mm(pg, gw[:, k, bi * 1024 + dc * 128:bi * 1024 + (dc + 1) * 128], hT[:, k, ts_],
                                 start=(k == 0), stop=(k == 7))
                        pp = s.bank()
                        for k in range(4):
                            s.mm(pp, pw[:, bi * 4 + k, dc * 128:(dc + 1) * 128], oT[:, bi * 4 + k, :],
                                 start=(k == 0), stop=(k == 3))
                        s.act(sgm, pg, AF.Sigmoid)
                        if bi == 0:
                            s.tt('dve', macc, pp, sgm, ALU.mult)
                        else:
                            s.tt('dve', sgm, pp, sgm, ALU.mult)
                            if bi == 1:
                                s.tt('dve', macc, macc, sgm, ALU.add)
                            else:
                                s.tt('dve', mT[:, dc, :], macc, sgm, ALU.add)
                for t4 in range(4):
                    tt = tg * 4 + t4
                    x_ = xt[tt % 2]
                    s.dma(x_, xsrc[tt * 128:(tt + 1) * 128, :])
                    for half in range(2):
                        hs = slice(half * 512, (half + 1) * 512)
                        po = s.bank()
                        for k in range(8):
                            s.mm(po, mT[:, k, t4 * 128:(t4 + 1) * 128], ow[:, k, hs], start=(k == 0), stop=(k == 7))
                        s.tt('dve', tmp, po, gt1[:, hs], ALU.mult)
                        s.tt('dve', x_[:, hs], x_[:, hs], tmp, ALU.add)
                    s.dma(out[tt * 128:(tt + 1) * 128, :], x_)

        def phase_moe(l):
            A.reset()
            gt2 = modbc[:, 5 * D:6 * D]
            slots_i = A.get([NT, 2], I32)
            wts2 = A.get([NT, 2])
            mark = A.off
            rw = A.get([8, 64])
            lg_all = A.get([NT, 36])
            hTf = [A.get([8, 128]) for _ in range(2)]
            s.memset('dve', rw, 0.0)
            s.memset('dve', rows[0:1, 1280:1344], 0.0)
            s.dma(rw[:, :, 0:36], W['router_w'][l].rearrange("(k p) n -> p k n", p=128))
            s.dma(rows[0:1, 1280:1316], W['router_b'][l:l + 1, :])
            base = A.off
            A2 = Arena(A.ap[:, base // 4:], A.nbytes - base)
            _norm_with(A2, l, rw, lg_all, hTf)
            G3 = lambda: A2.get([NT, 4])
            E3 = lambda: A2.get([NT, 32])
            T1 = lambda: A2.get([NT])
            oh, pen, ge = G3(), G3(), G3()
            em, m1, m2, am, pos, ovf = E3(), E3(), E3(), E3(), E3(), E3()
            carry_all = A2.get([NT + 1, 32])
            gmax, gs, gp, v1, v2, dd, ex, w1, w2 = T1(), T1(), T1(), T1(), T1(), T1(), T1(), T1(), T1()
            slots_f = A2.get([NT, 2])
            hrow = [A2.get([D], BF16) for _ in range(2)]
            gl = lg_all[:, :, 0:4]
            el = lg_all[:, :, 4:36]
            s.reduce('dve', gmax, gl, ALU.max)
            s.tt('dve', oh, gl, bc_last(gmax, 4), ALU.is_equal)
            s.tt('dve', ge, gl, bc_last(gmax, 4), ALU.subtract)
            s.act(ge, ge, AF.Exp)
            s.reduce('dve', gs, ge, ALU.add)
            s.op('dve', lambda g: g.reciprocal(gp, gs), [gs], [gp])
            s.ts('dve', pen, oh, 1e9, -1e9, ALU.mult, ALU.add)
            s.copy('dve', em, el)
            em64 = em.rearrange("p t (g e) -> p (t g) e", g=4)
            s.tt('dve', em64, em64, bc_last(pen.rearrange("p t g -> p (t g)"), 8), ALU.add)
            s.reduce('dve', v1, em, ALU.max)
            s.tt('dve', m1, em, bc_last(v1, 32), ALU.is_equal)
            s.stt('dve', em, m1, -1e9, em, ALU.mult, ALU.add)
            s.reduce('dve', v2, em, ALU.max)
            s.tt('dve', m2, em, bc_last(v2, 32), ALU.is_equal)
            s.tt('dve', dd, v2, v1, ALU.subtract)
            s.act(ex, dd, AF.Exp)
            s.ts('dve', w1, ex, 1.0, None, ALU.add)
            s.op('dve', lambda g: g.reciprocal(w1, w1), [w1], [w1])
            s.tt('dve', w2, ex, w1, ALU.mult)
            s.tt('dve', wts2[:, :, 0], w1, gp, ALU.mult)
            s.tt('dve', wts2[:, :, 1], w2, gp, ALU.mult)
            s.tt('dve', am, m1, m2, ALU.add)
            amf = am.rearrange("p t e -> p (t e)")
            pp = s.bank()
            s.mm(pp, cst[:, C_TRI:C_TRI + 128], amf)
            pt = s.bank()
            s.mm(pt, cst[:, C_ONE:C_ONE + 128], amf)
            s.memset('dve', carry_all[:, 0, :], 0.0)
            for tt in range(NT):
                s.tt('dve', carry_all[:, tt + 1, :], carry_all[:, tt, :], pt[:, tt * 32:(tt + 1) * 32], ALU.add)
            carry = carry_all[:, NT, :]
            s.tt('dve', pos.rearrange("p t e -> p (t e)"), pp, carry_all[:, 0:NT, :].rearrange("p t e -> p (t e)"), ALU.add)
            s.ts('dve', ovf, pos, float(CAP), 1e6, ALU.is_ge, ALU.mult)
            ecap = cst[:, C_ECAP:C_ECAP + 32]
            s.tt('dve', pos, pos, bass.AP(ecap.tensor, ecap.offset, [list(ecap.ap[0]), [0, NT], [1, 32]]), ALU.add)
            s.tt('dve', pos, pos, ovf, ALU.add)
            s.tt('dve', m1, m1, pos, ALU.mult)
            s.tt('dve', m2, m2, pos, ALU.mult)
            s.reduce('dve', slots_f[:, :, 0], m1, ALU.add)
            s.reduce('dve', slots_f[:, :, 1], m2, ALU.add)
            s.copy('dve', slots_i, slots_f)
            for tt in range(NT):
                hr = hrow[tt % 2]
                s.dma(hr, H2_scr[tt * 128:(tt + 1) * 128, :])
                for j in range(2):
                    s.idma(XG, hr, slots_i[:, tt, j:j + 1], True, NE * CAP)
            if 'cnt' in DBG:
                s.dma(DBG['cnt'][l], carry)
            A.off = mark
            NB = 2
            wg = [A.get([8, 512], BF16) for _ in range(NB)]
            wu = [A.get([8, 512], BF16) for _ in range(NB)]
            wd = [A.get([4, 1024], BF16) for _ in range(NB)]
            RT_ = CAP // 128
            xg = [A.get([RT_, D], BF16) for _ in range(2)]
            xT = [A.get([8, CAP], BF16) for _ in range(2)]
            hid = [A.get([4, CAP], BF16) for _ in range(2)]
            sg = [A.get([CAP], BF16) for _ in range(2)]
            yst = [A.get([D]) for _ in range(2)]
            y12 = [[A.get([D]) for _ in range(2)] for _ in range(2)]
            xts = [A.get([D]) for _ in range(2)]
            yi = 0
            for e in range(NE):
                b = e % NB
                s.dma(wg[b], W['exp_w_gate'][l, e].rearrange("(k p) n -> p k n", p=128), q='pool')
                s.dma(wu[b], W['exp_w_up'][l, e].rearrange("(k p) n -> p k n", p=128), q='pool')
                s.dma(wd[b], W['exp_w_down'][l, e].rearrange("(k p) n -> p k n", p=128), q='pool')
                xg_ = xg[e % 2]
                s.dma(xg_, XG[e * CAP:(e + 1) * CAP, :].rearrange("(r p) d -> p r d", p=128))
                xT_ = xT[e % 2]
                for r in range(RT_):
                    pb = s.bank().bitcast(BF16)
                    for k in range(8):
                        s.tr(pb[:, k * 128:(k + 1) * 128], xg_[:, r, k * 128:(k + 1) * 128], idb)
                    evac(xT_[:, :, r * 128:(r + 1) * 128], pb.rearrange("p (k t) -> p k t", k=8))
                hid_ = hid[e % 2]
                for fc in range(4):
                    fs = slice(fc * 128, (fc + 1) * 128)
                    pg = s.bank()
                    for k in range(8):
                        s.mm(pg[:, 0:CAP], wg[b][:, k, fs], xT_[:, k, :], start=(k == 0), stop=(k == 7))
                    pu = s.bank()
                    for k in range(8):
                        s.mm(pu[:, 0:CAP], wu[b][:, k, fs], xT_[:, k, :], start=(k == 0), stop=(k == 7))
                    sgb = sg[fc % 2]
                    s.act(sgb, pg[:, 0:CAP], AF.Silu)
                    s.tt('dve', hid_[:, fc, :], pu[:, 0:CAP], sgb, ALU.mult)
                for r in range(RT_):
                    ys = yst[yi % 2]
                    yi += 1
                    for half in range(2):
                        hs = slice(half * 512, (half + 1) * 512)
                        py = s.bank()
                        for k in range(4):
                            s.mm(py, hid_[:, k, r * 128:(r + 1) * 128], wd[b][:, k, hs], start=(k == 0), stop=(k == 3))
                        evac(ys[:, hs], py)
                    s.dma(YG[e * CAP + r * 128:e * CAP + (r + 1) * 128, :], ys)
            for tt in range(NT):
                ya, yb = y12[tt % 2]
                x_ = xts[tt % 2]
                s.dma(x_, out[tt * 128:(tt + 1) * 128, :])
                s.memset('pool', ya, 0.0)
                s.memset('pool', yb, 0.0)
                s.idma(ya, YG, slots_i[:, tt, 0:1], False, NE * CAP)
                s.idma(yb, YG, slots_i[:, tt, 1:2], False, NE * CAP)
                s.ts('dve', ya, ya, wts2[:, tt, 0:1], None, ALU.mult)
                s.stt('dve', ya, yb, wts2[:, tt, 1:2], ya, ALU.mult, ALU.add)
                s.tt('dve', ya, ya, gt2, ALU.mult)
                s.tt('dve', x_, x_, ya, ALU.add)
                s.dma(out[tt * 128:(tt + 1) * 128, :], x_)

        def _norm_with(A2, l, rw, lg_all, hTf):
            nonlocal A
            saved = A
            A = A2
            try:
                phase_norm(l, 1, out, router=(rw, lg_all, hTf))
            finally:
                A = saved

        order = ['mod', 'norm', 'proj', 'attn', 'rwkv', 'gla', 'merge', 'moe']
        stop_l, stop_p = (nlayers - 1, 'moe') if upto == 'all' else upto
        done = False
        for l in range(nlayers):
            for ph in order:
                if phases is not None and (l, ph) not in phases:
                    continue
                if ph == 'mod':
                    phase_mod(l)
                    dump('mod', modbc[0:1, :])
                elif ph == 'norm':
                    phase_norm(l, 0, x_in if l == 0 else out)
                elif ph == 'proj':
                    phase_proj(l)
                elif ph == 'attn':
                    phase_attn(l)
                elif ph == 'rwkv':
                    phase_rwkv(l)
                elif ph == 'gla':
                    phase_gla(l)
                elif ph == 'merge':
                    phase_merge(l, x_in if l == 0 else out)
                elif ph == 'moe':
                    phase_moe(l)
                if (l, ph) == (stop_l, stop_p):
                    done = True
                    break
            if done:
                break
        if 'P' in DBG:
            s.dma(DBG['P'], P_scr)
        if 'OT' in DBG:
            s.dma(DBG['OT'], OT_scr, q='pool')
        if 'hT' in DBG:
            hf = arena_t[:, 0:4096]
            for k in range(8):
                for g in range(0, S, 4096 // 1):
                    pass
        s.finish()
        print("instructions:", s.ninst, "sbuf left:", nc.sbuf_bytes_remaining, flush=True)
    return nc


def prep_weights(inp):
    f = lambda a: np.ascontiguousarray(np.asarray(a, dtype=np.float32))
    w = {}
    for k in ('ada_w', 'ada_b', 'norm1_g', 'norm2_g', 'w_in', 'attn_subln_g', 'rwkv_mu', 'rwkv_w_up', 'rwkv_w0',
              'rwkv_a_up', 'rwkv_a0', 'rwkv_g_up', 'rwkv_k_k', 'rwkv_k_a', 'rwkv_lnx_g', 'rwkv_lnx_b',
              'gla_alpha_up', 'gla_alpha_b', 'proj_attn', 'proj_rwkv', 'proj_gla', 'w_out', 'exp_w_gate',
              'exp_w_up', 'exp_w_down'):
        w[k] = f(inp[k])
    w['attn_qkg'] = f(np.concatenate([np.tile(inp['attn_qn_g'], (1, 8)), np.tile(inp['attn_kn_g'], (1, 8))], axis=1))
    w['attn_lambda'] = f(np.reshape(inp['attn_lambda'], (DEPTH, 256)))
    w['rwkv_r_k'] = f(np.reshape(inp['rwkv_r_k'], (DEPTH, 512)))
    w['gla_norm_g4'] = f(np.tile(inp['gla_norm_g'], (1, 4)))
    w['router_w'] = f(np.concatenate([inp['router_grp_w'], inp['router_exp_w']], axis=2))
    w['router_b'] = f(np.concatenate([inp['router_grp_b'], inp['router_exp_b']], axis=1))
    return w


def kernel(**inputs):
    x = np.asarray(inputs['x'], dtype=np.float32)
    c = np.asarray(inputs['c'], dtype=np.float32)
    w = prep_weights(inputs)
    cst, aug = make_consts()
    nc = build_program()
    in_maps = []
    for b in range(8):
        m = dict(w)
        m['x'] = np.ascontiguousarray(x[b])
        m['c'] = np.ascontiguousarray(c[b].reshape(8, 128).T)
        m['consts'] = cst
        m['aug'] = aug
        in_maps.append(m)
    res = run_bass_kernel_spmd(nc, in_maps, core_ids=list(range(8)))
    return np.stack([np.asarray(r['out'], dtype=np.float32) for r in res.results], axis=0)
```
